# Optimizing a Trainium2 kernel written in Bass

```python
import math
import jax, jax.numpy as jnp
from jax import lax
import numpy as np

D_MODEL = 1024
BATCH = 4
SEQ = 4096
DEPTH = 1

SSM_HEAD_DIM = 64
D_SSM = D_MODEL
SSM_HEADS = D_SSM // SSM_HEAD_DIM
SSM_GROUPS = 4
D_STATE = 128
CONV_K = 4
CONV_CH = D_SSM + 2 * SSM_GROUPS * D_STATE
CHUNK = 128
ATT_HEAD_DIM = 64
D_ATT = D_MODEL
ATT_HEADS = D_ATT // ATT_HEAD_DIM
KV_HEADS = ATT_HEADS // 4
Q_PER_KV = ATT_HEADS // KV_HEADS
D_KV = KV_HEADS * ATT_HEAD_DIM
D_MIX = D_SSM + D_ATT
WINDOW = 128
ATT_BLOCK = 128
REL_BUCKETS = 32
REL_MAX_DIST = 128
IN_COLS = D_SSM + CONV_CH + SSM_HEADS + D_ATT + 2 * D_KV
N_EXPERTS = 64
TOP_K = 8
EXPERT_DIM = D_MODEL // 4
SHARED_DIM = D_MODEL // 4
ROUTE_GROUPS = 8
ROUTE_TOPK_GROUPS = 4
ROUTED_SCALE = 2.5
MOE_BLOCK = 128
EPS = 1e-6

kernel_name = 'hybrid_ssd_swa_sink_moe_block'


def rms_norm(x, g):
    xf = x.astype(jnp.float32)
    y = xf * lax.rsqrt(jnp.mean(xf * xf, axis=-1, keepdims=True) + EPS)
    return (y * g.astype(jnp.float32)).astype(x.dtype)


def causal_depthwise_conv(u, w, b):
    out = lax.conv_general_dilated(u, w[:, None, :].astype(u.dtype), window_strides=(1,),
                                   padding=[(CONV_K - 1, 0)], dimension_numbers=('NWC', 'WIO', 'NWC'),
                                   feature_group_count=u.shape[-1])
    return out + b.astype(u.dtype)


def ssd_scan(xs, Bm, Cm, dt_raw, dt_bias, a_log, d_skip):
    f32 = jnp.float32
    Bsz, L, _ = xs.shape
    nc = L // CHUNK
    R = SSM_HEADS // SSM_GROUPS
    dt = jax.nn.softplus(dt_raw.astype(f32) + dt_bias.astype(f32))
    A = -jnp.exp(a_log.astype(f32))
    x = xs.astype(f32).reshape(Bsz, L, SSM_HEADS, SSM_HEAD_DIM)
    Xd = x.reshape(Bsz, nc, CHUNK, SSM_GROUPS, R, SSM_HEAD_DIM) * dt.reshape(Bsz, nc, CHUNK, SSM_GROUPS, R)[..., None]
    a = jnp.moveaxis((dt * A).reshape(Bsz, nc, CHUNK, SSM_GROUPS, R), 2, -1)
    cs = jnp.cumsum(a, axis=-1)
    Bc = Bm.astype(f32).reshape(Bsz, nc, CHUNK, SSM_GROUPS, D_STATE)
    Cc = Cm.astype(f32).reshape(Bsz, nc, CHUNK, SSM_GROUPS, D_STATE)
    causal = jnp.tril(jnp.ones((CHUNK, CHUNK), dtype=bool))
    Lmat = jnp.exp(jnp.where(causal, cs[..., :, None] - cs[..., None, :], -jnp.inf))
    CB = jnp.einsum('bclgn,bcsgn->bcgls', Cc, Bc)
    y_diag = jnp.einsum('bcgls,bcgrls,bcsgrp->bclgrp', CB, Lmat, Xd)
    decay_states = jnp.exp(cs[..., -1:] - cs)
    chunk_states = jnp.einsum('bclgn,bcgrl,bclgrp->bcgrpn', Bc, decay_states, Xd)
    chunk_decay = jnp.exp(cs[..., -1])

    def step(state, inp):
        s_c, d_c = inp
        return state * d_c[..., None, None] + s_c, state

    init = jnp.zeros((Bsz, SSM_GROUPS, R, SSM_HEAD_DIM, D_STATE), f32)
    _, states_in = lax.scan(step, init, (jnp.moveaxis(chunk_states, 1, 0), jnp.moveaxis(chunk_decay, 1, 0)))
    states_in = jnp.moveaxis(states_in, 0, 1)
    y_off = jnp.einsum('bclgn,bcgrpn,bcgrl->bclgrp', Cc, states_in, jnp.exp(cs))
    y = (y_diag + y_off).reshape(Bsz, L, SSM_HEADS, SSM_HEAD_DIM) + x * d_skip.astype(f32)[:, None]
    return y.reshape(Bsz, L, D_SSM)


def t5_causal_bucket(dist):
    max_exact = REL_BUCKETS // 2
    d = jnp.maximum(dist, 1).astype(jnp.float32)
    large = max_exact + (jnp.log(d / max_exact) / math.log(REL_MAX_DIST / max_exact)
                         * (REL_BUCKETS - max_exact)).astype(jnp.int32)
    large = jnp.minimum(large, REL_BUCKETS - 1)
    return jnp.where(dist < max_exact, dist, large)


def sliding_window_attention(q, k, v, sinks, rel_bias):
    f32 = jnp.float32
    Bsz, L, _ = q.shape
    nb = L // ATT_BLOCK
    qb = q.reshape(Bsz, nb, ATT_BLOCK, KV_HEADS, Q_PER_KV, ATT_HEAD_DIM)

    def band(t):
        tp = jnp.pad(t.reshape(Bsz, L, KV_HEADS, ATT_HEAD_DIM), ((0, 0), (ATT_BLOCK, 0), (0, 0), (0, 0)))
        tp = tp.reshape(Bsz, nb + 1, ATT_BLOCK, KV_HEADS, ATT_HEAD_DIM)
        return jnp.concatenate([tp[:, :-1], tp[:, 1:]], axis=2)

    kb, vb = band(k), band(v)
    qi = jnp.arange(ATT_BLOCK)[:, None]
    sj = jnp.arange(2 * ATT_BLOCK)[None, :]
    dist = qi + ATT_BLOCK - sj
    in_win = (dist >= 0) & (dist < WINDOW)
    key_abs = jnp.arange(nb)[:, None, None] * ATT_BLOCK - ATT_BLOCK + sj[None]
    mask = in_win[None] & (key_abs >= 0)
    bias = rel_bias.astype(f32)[t5_causal_bucket(jnp.maximum(dist, 0))]
    bias = bias.transpose(2, 0, 1).reshape(KV_HEADS, Q_PER_KV, ATT_BLOCK, 2 * ATT_BLOCK)
    s = jnp.einsum('bnqkgd,bnskd->bnkgqs', qb, kb).astype(f32) * (ATT_HEAD_DIM ** -0.5) + bias
    s = jnp.where(mask[None, :, None, None], s, -jnp.inf)
    sink = sinks.astype(f32).reshape(KV_HEADS, Q_PER_KV)[None, None, :, :, None, None]
    m = jnp.maximum(jnp.max(s, axis=-1, keepdims=True), sink)
    p = jnp.exp(s - m)
    p = p / (jnp.sum(p, axis=-1, keepdims=True) + jnp.exp(sink - m))
    o = jnp.einsum('bnkgqs,bnskd->bnqkgd', p.astype(v.dtype), vb)
    return o.reshape(Bsz, L, D_ATT)


def hybrid_mixer(h, w_in, conv_w, conv_b, dt_bias, a_log, d_skip, ssm_norm_g, att_norm_g, sinks, rel_bias, w_out):
    Bsz, L, _ = h.shape
    proj = h @ w_in
    i1 = D_SSM
    i2 = i1 + CONV_CH
    i3 = i2 + SSM_HEADS
    i4 = i3 + D_ATT
    i5 = i4 + D_KV
    z, xbc, dt_raw, q, k, v = jnp.split(proj, [i1, i2, i3, i4, i5], axis=-1)
    xbc = jax.nn.silu(causal_depthwise_conv(xbc, conv_w, conv_b))
    xs, Bm, Cm = jnp.split(xbc, [D_SSM, D_SSM + SSM_GROUPS * D_STATE], axis=-1)
    y = ssd_scan(xs, Bm, Cm, dt_raw, dt_bias, a_log, d_skip)
    yz = (y * jax.nn.silu(z.astype(jnp.float32))).reshape(Bsz, L, SSM_GROUPS, D_SSM // SSM_GROUPS)
    yz = yz * lax.rsqrt(jnp.mean(yz * yz, axis=-1, keepdims=True) + EPS)
    y_ssm = (yz.reshape(Bsz, L, D_SSM) * ssm_norm_g.astype(jnp.float32)).astype(h.dtype)
    y_att = rms_norm(sliding_window_attention(q, k, v, sinks, rel_bias), att_norm_g)
    return jnp.concatenate([y_ssm, y_att], axis=-1) @ w_out


def moe_ffn(h, router_w, router_bias, exp_w_gate, exp_w_up, exp_w_down, sh_w_gate, sh_w_up, sh_w_down):
    f32 = jnp.float32
    N = h.shape[0]
    scores = jax.nn.sigmoid(h.astype(f32) @ router_w.astype(f32))
    sel = scores + router_bias.astype(f32)
    per_group = N_EXPERTS // ROUTE_GROUPS
    grp_score = lax.top_k(sel.reshape(N, ROUTE_GROUPS, per_group), 2)[0].sum(-1)
    _, gidx = lax.top_k(grp_score, ROUTE_TOPK_GROUPS)
    gmask = jax.nn.one_hot(gidx, ROUTE_GROUPS, dtype=f32).sum(1)
    allowed = jnp.repeat(gmask, per_group, axis=1) > 0
    _, idx = lax.top_k(jnp.where(allowed, sel, -jnp.inf), TOP_K)
    w = jnp.take_along_axis(scores, idx, axis=1)
    gates = w / jnp.sum(w, axis=-1, keepdims=True) * ROUTED_SCALE

    A = N * TOP_K
    NB = (A + N_EXPERTS * (MOE_BLOCK - 1) + MOE_BLOCK - 1) // MOE_BLOCK
    P = NB * MOE_BLOCK
    flat_e = idx.reshape(-1)
    flat_tok = jnp.arange(A, dtype=jnp.int32) // TOP_K
    order = jnp.argsort(flat_e)
    se, stok, sg = flat_e[order], flat_tok[order], gates.reshape(-1)[order]
    counts = jnp.bincount(flat_e, length=N_EXPERTS)
    padded = (counts + MOE_BLOCK - 1) // MOE_BLOCK * MOE_BLOCK
    pad_end = jnp.cumsum(padded)
    pad_start = pad_end - padded
    grp_start = jnp.cumsum(counts) - counts
    dest = pad_start[se] + (jnp.arange(A) - grp_start[se])
    row_tok = jnp.zeros((P,), jnp.int32).at[dest].set(stok)
    row_gate = jnp.zeros((P,), f32).at[dest].set(sg)
    block_expert = jnp.minimum(jnp.searchsorted(pad_end, jnp.arange(NB) * MOE_BLOCK, side='right'), N_EXPERTS - 1)

    def expert_block(args):
        toks, e, g = args
        xb = h[toks]
        u = jax.nn.silu(xb @ exp_w_gate[e]) * (xb @ exp_w_up[e])
        return (u @ exp_w_down[e]).astype(f32) * g[:, None]

    y_rows = lax.map(expert_block, (row_tok.reshape(NB, MOE_BLOCK), block_expert, row_gate.reshape(NB, MOE_BLOCK)))
    routed = jax.ops.segment_sum(y_rows.reshape(P, -1), row_tok, num_segments=N)
    shared = (jax.nn.silu(h @ sh_w_gate) * (h @ sh_w_up)) @ sh_w_down
    return (routed + shared.astype(f32)).astype(h.dtype)


def setup_inputs(seed: int = 0) -> dict:
    key = jax.random.key(seed)
    ks = jax.random.split(key, 26)
    f32 = jnp.float32

    def nrm(k, shape, scale):
        return jax.random.normal(k, shape, f32) * scale

    dt0 = jnp.exp(jax.random.uniform(ks[9], (DEPTH, SSM_HEADS), f32, math.log(1e-3), math.log(1e-1)))
    return {
        'x': nrm(ks[0], (BATCH, SEQ, D_MODEL), 1.0),
        'c': nrm(ks[1], (BATCH, D_MODEL), 1.0),
        'mod_w': nrm(ks[2], (DEPTH, D_MODEL, 6 * D_MODEL), 0.5 * D_MODEL ** -0.5),
        'mod_b': nrm(ks[3], (DEPTH, 6 * D_MODEL), 0.02),
        'norm1_g': 1.0 + nrm(ks[4], (DEPTH, D_MODEL), 0.02),
        'norm2_g': 1.0 + nrm(ks[5], (DEPTH, D_MODEL), 0.02),
        'w_in': nrm(ks[6], (DEPTH, D_MODEL, IN_COLS), D_MODEL ** -0.5),
        'conv_w': nrm(ks[7], (DEPTH, CONV_K, CONV_CH), CONV_K ** -0.5),
        'conv_b': nrm(ks[8], (DEPTH, CONV_CH), 0.02),
        'dt_bias': dt0 + jnp.log(-jnp.expm1(-dt0)),
        'a_log': jnp.log(jax.random.uniform(ks[10], (DEPTH, SSM_HEADS), f32, 1.0, 16.0)),
        'd_skip': 1.0 + nrm(ks[11], (DEPTH, SSM_HEADS), 0.1),
        'ssm_norm_g': 1.0 + nrm(ks[12], (DEPTH, D_SSM), 0.02),
        'att_norm_g': 1.0 + nrm(ks[13], (DEPTH, D_ATT), 0.02),
        'sinks': nrm(ks[14], (DEPTH, ATT_HEADS), 0.5),
        'rel_bias': nrm(ks[15], (REL_BUCKETS, ATT_HEADS), 0.5),
        'w_out': nrm(ks[16], (DEPTH, D_MIX, D_MODEL), D_MIX ** -0.5),
        'router_w': nrm(ks[17], (DEPTH, D_MODEL, N_EXPERTS), D_MODEL ** -0.5),
        'router_bias': nrm(ks[18], (DEPTH, N_EXPERTS), 0.01),
        'exp_w_gate': nrm(ks[19], (DEPTH, N_EXPERTS, D_MODEL, EXPERT_DIM), D_MODEL ** -0.5),
        'exp_w_up': nrm(ks[20], (DEPTH, N_EXPERTS, D_MODEL, EXPERT_DIM), D_MODEL ** -0.5),
        'exp_w_down': nrm(ks[21], (DEPTH, N_EXPERTS, EXPERT_DIM, D_MODEL), EXPERT_DIM ** -0.5),
        'sh_w_gate': nrm(ks[22], (DEPTH, D_MODEL, SHARED_DIM), D_MODEL ** -0.5),
        'sh_w_up': nrm(ks[23], (DEPTH, D_MODEL, SHARED_DIM), D_MODEL ** -0.5),
        'sh_w_down': nrm(ks[24], (DEPTH, SHARED_DIM, D_MODEL), SHARED_DIM ** -0.5),
        'final_g': 1.0 + nrm(ks[25], (D_MODEL,), 0.02),
    }


def reference(x, c, mod_w, mod_b, norm1_g, norm2_g, w_in, conv_w, conv_b, dt_bias, a_log, d_skip,
              ssm_norm_g, att_norm_g, sinks, rel_bias, w_out, router_w, router_bias, exp_w_gate, exp_w_up,
              exp_w_down, sh_w_gate, sh_w_up, sh_w_down, final_g):
    Bsz, L, D = x.shape
    for layer in range(DEPTH):
        mod = jax.nn.silu(c) @ mod_w[layer] + mod_b[layer]
        sh1, sc1, g1, sh2, sc2, g2 = [m[:, None, :] for m in jnp.split(mod, 6, axis=-1)]
        h = rms_norm(x, norm1_g[layer]) * (1.0 + sc1) + sh1
        x = x + g1 * hybrid_mixer(h, w_in[layer], conv_w[layer], conv_b[layer], dt_bias[layer], a_log[layer],
                                  d_skip[layer], ssm_norm_g[layer], att_norm_g[layer], sinks[layer], rel_bias,
                                  w_out[layer])
        h = rms_norm(x, norm2_g[layer]) * (1.0 + sc2) + sh2
        y = moe_ffn(h.reshape(Bsz * L, D), router_w[layer], router_bias[layer], exp_w_gate[layer], exp_w_up[layer],
                    exp_w_down[layer], sh_w_gate[layer], sh_w_up[layer], sh_w_down[layer])
        x = x + g2 * y.reshape(Bsz, L, D)
    return rms_norm(x, final_g)
```

```python
import numpy as np
from contextlib import ExitStack
import concourse.bass as bass
import concourse.mybir as mybir
from concourse.bass_utils import run_bass_kernel_spmd

F32, BF16 = mybir.dt.float32, mybir.dt.bfloat16
AF = mybir.ActivationFunctionType
ALU = mybir.AluOpType
AX = mybir.AxisListType
EPS = 1e-6
NT = 2048
NCH = NT // 128
NEXP = 65

_VOFF = {}
_o = 0
for _n, _w in (("n1g", 8), ("n2g", 8), ("fg", 8), ("modb", 48), ("convw", 64), ("convb", 16), ("dtb", 16), ("alog", 16),
               ("dskip", 16), ("sinks", 16), ("rbias", 64), ("wog", 16), ("flag", 1), ("cT", 8)):
    _VOFF[_n] = (_o, _w)
    _o += _w
NV = _o


class _Rec:
    def __init__(self):
        self.call = None

    def __getattr__(self, name):
        def f(*a, **k):
            self.call = (name, a, k)
            return self
        return f


class Prog:
    def __init__(self, nc, es):
        self.nc, self.es = nc, es
        self.names = ("pe", "dve", "act", "pool", "sp")
        self.semh, self.cnt = {}, {}
        for k in self.names:
            self.semh[k] = es.enter_context(nc.semaphore("c_" + k))
            self.cnt[k] = 0
        self.seen = {k: {} for k in self.names}
        self.lastw, self.readers = {}, {}
        self.ops = {k: [] for k in self.names}

    def _deps(self, eng, r, w):
        deps = {}

        def add(d):
            if d is None:
                return
            s, v = d
            if s == eng and eng in ("pe", "sp"):
                return
            if deps.get(s, 0) < v:
                deps[s] = v
        for k in r:
            add(self.lastw.get(k))
        for k in w:
            add(self.lastw.get(k))
            for s, v in self.readers.get(k, {}).items():
                add((s, v))
        out = []
        for s, v in deps.items():
            if self.seen[eng].get(s, 0) < v:
                self.seen[eng][s] = v
                out.append((s, v))
        return out

    def _mark(self, me, r, w):
        for k in r:
            self.readers.setdefault(k, {})[me[0]] = me[1]
        for k in w:
            self.lastw[k] = me
            self.readers[k] = {}

    def op(self, eng, fn, r=(), w=()):
        rec = _Rec()
        fn(rec)
        name_, a_, k_ = rec.call
        fn = (lambda e, name_=name_, a_=a_, k_=k_: getattr(e, name_)(*a_, **k_))
        waits = self._deps(eng, r, w)
        self.cnt[eng] += 1
        self._mark((eng, self.cnt[eng]), r, w)
        self.ops[eng].append((waits, fn, (eng, 1)))

    def dma(self, q, out, in_, r=(), w=(), sem="d"):
        if sem not in self.semh:
            self.semh[sem] = self.es.enter_context(self.nc.semaphore("d_" + sem))
            self.cnt[sem] = 0
        waits = self._deps(q, r, w)
        self.cnt[sem] += 16
        self._mark((sem, self.cnt[sem]), r, w)
        self.ops[q].append((waits, (lambda e, o=out, i=in_: e.dma_start(out=o, in_=i)), (sem, 16)))

    def dma_group(self, q, pairs, r=(), w=(), sem="d"):
        if sem not in self.semh:
            self.semh[sem] = self.es.enter_context(self.nc.semaphore("d_" + sem))
            self.cnt[sem] = 0
        waits = self._deps(q, r, w)
        for i, (o, i_) in enumerate(pairs):
            self.cnt[sem] += 16
            self.ops[q].append((waits if i == 0 else [], (lambda e, o=o, i_=i_: e.dma_start(out=o, in_=i_)), (sem, 16)))
        self._mark((sem, self.cnt[sem]), r, w)

    def barrier(self):
        tgt = dict(self.cnt)
        for eng in self.names:
            waits = []
            for s, v in tgt.items():
                if v > 0 and self.seen[eng].get(s, 0) < v and not (s == eng):
                    self.seen[eng][s] = v
                    waits.append((s, v))
            if waits:
                self.ops[eng].append((waits, None, None))
        self.lastw, self.readers = {}, {}

    def final_wait(self, eng, sem):
        self.ops[eng].append(([(sem, self.cnt[sem])], None, None))

    def emit(self):
        block = self.es.enter_context(self.nc.Block())
        decs = dict(pe=block.tensor, dve=block.vector, act=block.scalar, pool=block.gpsimd, sp=block.sync)
        for name in self.names:
            ops = self.ops[name]

            def body(e, ops=ops):
                for waits, fn, inc in ops:
                    for ws, wv in waits:
                        e.wait_ge(self.semh[ws], wv)
                    if fn is not None:
                        fn(e).then_inc(self.semh[inc[0]], inc[1])
            decs[name](body)


def build_nc(stage="full"):
    nc = bass.Bass("TRN2", target_bir_lowering=False)
    D = lambda n, sh: nc.dram_tensor(n, sh, F32, kind="ExternalInput").ap()
    xoT = D("xoT", [1024, NT])
    xpT = D("xpT", [1024, NT])
    vec = D("vec", [128, NV])
    modw = D("modw", [128, 8, 6144])
    cbTd = D("cbT", [16, 128])
    w1d = D("w1", [128, 8, 3088])
    w2d = D("w2", [128, 8, 1792])
    wod = D("wo", [128, 16, 1024])
    wrd = D("wr", [128, 8, 64])
    biasd = D("biasT", [128, 16 * 256])
    egd = D("eg", [NEXP, 128, 8 * 256])
    eud = D("eu", [NEXP, 128, 8 * 256])
    edd = D("ed", [NEXP, 128, 2 * 1024])
    outT = nc.dram_tensor("outT", [1024, NT], F32, kind="ExternalOutput").ap()
    xo3 = xoT.rearrange("(c p) t -> p c t", p=128)
    xp3 = xpT.rearrange("(c p) t -> p c t", p=128)
    out3 = outT.rearrange("(c p) t -> p c t", p=128)

    with ExitStack() as es:
        P = Prog(nc, es)
        SB = lambda n, sh, dt, st=es: st.enter_context(nc.sbuf_tensor(n, sh, dt))
        PS = es.enter_context(nc.psum_tensor("PS", [128, 4096], F32))
        PSB = PS[:].bitcast(BF16)

        def bank(b, n=512, off=0):
            return PS[:, b * 512 + off: b * 512 + off + n]

        def bankb(b, n=1024):
            return PSB[:, b * 1024: b * 1024 + n]
        pk = lambda *bs: [("ps", b) for b in bs]

        R = SB("R", [128, 8, NT], F32)
        V = SB("V", [128, NV], F32)
        U = SB("U", [128, 128], F32)
        SL = SB("SL", [128, 128], F32)
        idf = SB("idf", [128, 128], F32)
        idb = SB("idb", [128, 128], BF16)
        onf = SB("onf", [128, 128], F32)
        onb = SB("onb", [128, 128], BF16)
        MOD = SB("MOD", [128, 48], F32)
        A1 = SB("A1", [128, 8], F32)
        A2 = SB("A2", [128, 8], F32)
        g1bc = SB("g1bc", [128, 1024], F32)
        Abc = SB("Abc", [128, 16], F32)
        esink = SB("esink", [128, 16], F32)
        vs = lambda n, i=None: (V[:, _VOFF[n][0]:_VOFF[n][0] + _VOFF[n][1]] if i is None
                                else V[:, _VOFF[n][0] + i:_VOFF[n][0] + i + 1])

        P.dma("sp", V[:], vec[:, :], w=["V"], sem="v")
        P.op("pool", lambda e: e.memset(U[:], 1.0), w=["U"])
        P.op("pool", lambda e: e.affine_select(out=U[:], in_=U[:], pattern=[[1, 128]], compare_op=ALU.is_ge,
                                               fill=0.0, base=0, channel_multiplier=-1), r=["U"], w=["U"])
        P.op("pool", lambda e: e.memset(SL[:], 1.0), w=["SL"])
        P.op("pool", lambda e: e.affine_select(out=SL[:], in_=SL[:], pattern=[[-1, 128]], compare_op=ALU.is_ge,
                                               fill=0.0, base=-1, channel_multiplier=1), r=["SL"], w=["SL"])
        P.op("pool", lambda e: e.memset(idf[:], 1.0), w=["idf"])
        P.op("pool", lambda e: e.affine_select(out=idf[:], in_=idf[:], pattern=[[1, 128]], compare_op=ALU.is_equal,
                                               fill=0.0, base=0, channel_multiplier=-1), r=["idf"], w=["idf"])
        P.op("pool", lambda e: e.tensor_copy(out=idb[:], in_=idf[:]), r=["idf"], w=["idb"])
        P.op("pool", lambda e: e.memset(onf[:], 1.0), w=["onf"])
        P.op("pool", lambda e: e.memset(onb[:], 1.0), w=["onb"])

        def norm_h(xsrc, xkey, hT, hkey, Ag, sh_off, wk, n, ssb):
            sq, rs, hn = wk["sq"], wk["rs"], wk["hn"]
            hk = list(xkey) if wk.get("inplace") else ["hn"]
            P.op("act", lambda e: e.activation(out=sq[:, :, :n], in_=xsrc, func=AF.Square), r=xkey, w=["sq"])
            if wk.get("inplace"):
                P.op("pool", lambda e: e.tensor_tensor(out=hn[:, :, :n], in0=hn[:, :, :n],
                                                       in1=Ag[:].unsqueeze(2).broadcast_to([128, 8, n]), op=ALU.mult),
                     r=hk + ["A1", "A2"], w=hk)
            for c in range(8):
                P.op("pe", lambda e, c=c: e.matmul(bank(ssb, n), onb[:], sq[:, c, :n], start=(c == 0), stop=(c == 7)),
                     r=["sq", "onb"], w=pk(ssb))
            P.op("act", lambda e: e.activation(out=rs[:, :n], in_=bank(ssb, n), func=AF.Ln, bias=wk["eps"][:, 0:1],
                                               scale=1.0 / 1024), r=pk(ssb) + ["eps"], w=["rs"])
            P.op("act", lambda e: e.activation(out=rs[:, :n], in_=rs[:, :n], func=AF.Exp, scale=-0.5), r=["rs"], w=["rs"])
            if wk.get("inplace"):
                P.op("dve", lambda e: e.tensor_tensor(out=hn[:, :, :n], in0=hn[:, :, :n],
                                                      in1=rs[:, :n].unsqueeze(1).broadcast_to([128, 8, n]), op=ALU.mult),
                     r=hk + ["rs"], w=hk)
            else:
                P.op("dve", lambda e: e.tensor_tensor(out=hn[:, :, :n], in0=xsrc,
                                                      in1=rs[:, :n].unsqueeze(1).broadcast_to([128, 8, n]), op=ALU.mult),
                     r=xkey + ["rs"], w=hk)
                P.op("pool", lambda e: e.tensor_tensor(out=hn[:, :, :n], in0=hn[:, :, :n],
                                                       in1=Ag[:].unsqueeze(2).broadcast_to([128, 8, n]), op=ALU.mult),
                     r=hk + ["A1", "A2"], w=hk)
            P.op("dve", lambda e: e.tensor_tensor(out=hT, in0=hn[:, :, :n],
                                                  in1=MOD[:, sh_off:sh_off + 8].unsqueeze(2).broadcast_to([128, 8, n]),
                                                  op=ALU.add), r=hk + ["MOD"], w=hkey)

        with ExitStack() as e1:
            w1 = SB("w1s", [128, 8, 3088], BF16, e1)
            wo1 = SB("wo1", [128, 8, 1024], BF16, e1)
            with ExitStack() as esA:
                sc = SB("sc", [128, 8], BF16, esA)
                mw = [SB("mw%d" % i, [128, 8, 1024], BF16, esA) for i in range(4)]
                dg = SB("dg", [128, 128], F32, esA)
                P.op("act", lambda e: e.activation(out=sc[:], in_=vs("cT"), func=AF.Silu), r=["V"], w=["sc"])

                def load_mw(m):
                    P.dma_group("pool", [(mw[m % 4][:, c, :], modw[:, c, m * 1024:(m + 1) * 1024]) for c in range(8)],
                                w=[("mw", m % 4)], sem="mw%d" % (m % 4))
                for m in range(4):
                    load_mw(m)
                for m in range(6):
                    s = m % 4
                    for j in range(8):
                        col = m * 8 + j
                        for c in range(8):
                            P.op("pe", lambda e, s=s, j=j, c=c, col=col: e.matmul(
                                bank(0, 1, col), mw[s][:, c, j * 128:(j + 1) * 128], sc[:, c:c + 1],
                                start=(c == 0), stop=(c == 7)), r=[("mw", s), "sc"], w=pk(0))
                    if m + 4 < 6:
                        load_mw(m + 4)
                    if m == 1:
                        P.dma_group("pool", [(w1[:, c, :], w1d[:, c, :]) for c in range(8)], w=["w1"], sem="w1")
                        P.dma("pool", wo1[:], wod[:, 0:8, :], w=["wo1"], sem="wo1")
                P.op("dve", lambda e: e.tensor_tensor(out=MOD[:], in0=bank(0, 48), in1=vs("modb"), op=ALU.add),
                     r=pk(0) + ["V"], w=["MOD"])
                P.op("dve", lambda e: e.scalar_tensor_tensor(out=A1[:], in0=MOD[:, 8:16], scalar=1.0, in1=vs("n1g"),
                                                             op0=ALU.add, op1=ALU.mult), r=["MOD", "V"], w=["A1"])
                P.op("dve", lambda e: e.scalar_tensor_tensor(out=A2[:], in0=MOD[:, 32:40], scalar=1.0, in1=vs("n2g"),
                                                             op0=ALU.add, op1=ALU.mult), r=["MOD", "V"], w=["A2"])
                for gi, (gb, o) in enumerate(((g1bc, 16),)):
                    for c in range(8):
                        P.op("dve", lambda e, c=c, o=o: e.tensor_scalar(out=dg[:], in0=idf[:], scalar1=MOD[:, o + c:o + c + 1],
                                                                        scalar2=None, op0=ALU.mult),
                             r=["MOD", "idf"], w=["dg"])
                        P.op("pe", lambda e, c=c, gi=gi: e.matmul(bank(1 + 2 * gi + c // 4, 128, (c % 4) * 128), onf[:], dg[:],
                                                                  start=True, stop=True),
                             r=["onf", "dg"], w=pk(1 + 2 * gi + c // 4))
                    P.op("dve", lambda e, gb=gb, gi=gi: e.tensor_copy(out=gb[:], in_=PS[:, (1 + 2 * gi) * 512:(3 + 2 * gi) * 512]),
                         r=pk(1 + 2 * gi, 2 + 2 * gi), w=["gbc%d" % gi])
                P.op("act", lambda e: e.activation(out=Abc[:], in_=vs("alog"), func=AF.Exp), r=["V"], w=["Abc"])
                P.op("dve", lambda e: e.tensor_scalar(out=Abc[:], in0=Abc[:], scalar1=-1.0, scalar2=None, op0=ALU.mult),
                     r=["Abc"], w=["Abc"])
                P.op("act", lambda e: e.activation(out=esink[:], in_=vs("sinks"), func=AF.Exp), r=["V"], w=["esink"])
                P.barrier()
            P.dma_group("sp", [(R[:, c, :], xo3[:, c, :]) for c in range(8)], w=[("R", c) for c in range(8)], sem="r")

            xc = [SB("xc%d" % i, [128, 8, 128], F32, e1) for i in range(2)]
            wk = dict(sq=SB("sq1", [128, 8, 128], BF16, e1), rs=SB("rs1", [128, 128], F32, e1),
                      eps=SB("eps1", [128, 1], F32, e1), inplace=True)
            hTs = [SB("hT1%d" % i, [128, 8, 128], BF16, e1) for i in range(2)]
            pre = SB("pre", [128, 16, 131], BF16, e1)
            cdiag = SB("cdiag", [128, 16, 4, 128], BF16, e1)
            cbT = SB("cbTs", [16, 128], BF16, e1)
            P.dma("pool", cbT[:], cbTd[:, :], w=["cbT"], sem="cbT")
            xbcT = SB("xbcT", [128, 16, 128], BF16, e1)
            sm = SB("sm1", [128, 16, 12], F32, e1)
            S32 = SB("S32", [128, 1024], F32, e1)
            Sb = SB("Sb", [128, 1024], BF16, e1)
            Xd = SB("Xd", [128, 16, 64], BF16, e1)
            Xdd = SB("Xdd", [128, 16, 64], BF16, e1)
            Btok = SB("Btok", [128, 4, 128], BF16, e1)
            rhsA = SB("rhsA", [128, 16, 128], F32, e1)
            Lt = SB("Lt", [128, 16, 128], BF16, e1)
            CBm = SB("CBm", [128, 4, 128], BF16, e1)
            _rf = rhsA[:].rearrange("p h l -> p (h l)")
            y1 = _rf[:, 0:1024].rearrange("p (h d) -> p h d", h=16)
            y2 = _rf[:, 1024:2048].rearrange("p (h d) -> p h d", h=16)
            szt = SB("szt", [128, 1024], BF16, e1)
            ssq = SB("ssq1", [128, 4], F32, e1)
            yn = SB("yn", [128, 1024], BF16, e1)
            ymT = SB("ymT", [128, 8, 128], BF16, e1)
            SMK = lambda i: ("sm", i)
            smv = lambda i: sm[:, :, i]

            P.op("pool", lambda e: e.memset(wk["eps"][:], EPS), w=["eps"])
            for c in range(8):
                P.op("dve", lambda e, c=c: e.scalar_tensor_tensor(out=wo1[:, c, :], in0=wo1[:, c, :], scalar=vs("wog", c),
                                                                  in1=g1bc[:], op0=ALU.mult, op1=ALU.mult),
                     r=["wo1", "V", "gbc0"], w=["wo1"])
            for tl in range(16):
                for k in range(4):
                    P.op("dve", lambda e, tl=tl, k=k: e.tensor_scalar(out=cdiag[:, tl, k, :], in0=idf[:],
                                                                     scalar1=vs("convw", tl * 4 + k), scalar2=None,
                                                                     op0=ALU.mult), r=["idf", "V"], w=["cdiag"])
            P.op("pool", lambda e: e.memset(S32[:], 0.0), w=["S32"])
            P.op("pool", lambda e: e.memset(Sb[:], 0.0), w=["Sb"])
            P.op("pool", lambda e: e.memset(pre[:], 0.0), w=["pre"])

            def LOADX(t_):
                src_ = xo3 if t_ >= 16 else xp3
                tc_ = (t_ % 16) * 128
                P.dma("sp", xc[t_ % 2][:], src_[:, :, tc_:tc_ + 128], w=[("xc", t_ % 2)], sem="xc%d" % (t_ % 2))

            def NORM(t_):
                wk["hn"] = xc[t_ % 2]
                norm_h(xc[t_ % 2][:], [("xc", t_ % 2)], hTs[t_ % 2][:], [("hT", t_ % 2)], A1, 0, wk, 128, 0)
            LOADX(0)
            NORM(0)
            for t in range(32):
                own = t >= 16
                tc = (t % 16) * 128
                par = t % 2
                src = xo3 if own else xp3
                if t + 1 < 32:
                    LOADX(t + 1)
                hT, HK = hTs[par], ("hT", par)
                for c in range(8):
                    P.op("pe", lambda e, c=c: e.matmul(bank(5, 16), hT[:, c, :], w1[:, c, 3072:3088],
                                                       start=(c == 0), stop=(c == 7)), r=[HK, "w1"], w=pk(5))
                P.op("dve", lambda e: e.tensor_tensor(out=smv(0), in0=bank(5, 16), in1=vs("dtb"), op=ALU.add),
                     r=pk(5) + ["V"], w=[SMK(0)])
                P.op("dve", lambda e: e.tensor_scalar(out=smv(1), in0=smv(0), scalar1=-1.0, scalar2=None, op0=ALU.mult),
                     r=[SMK(0)], w=[SMK(1)])
                P.op("dve", lambda e: e.tensor_tensor(out=smv(1), in0=smv(1), in1=smv(0), op=ALU.max),
                     r=[SMK(0), SMK(1)], w=[SMK(1)])
                P.op("act", lambda e: e.activation(out=smv(2), in_=smv(1), func=AF.Exp, scale=-1.0),
                     r=[SMK(1)], w=[SMK(2)])
                P.op("act", lambda e: e.activation(out=smv(3), in_=smv(2), func=AF.Ln, bias=onf[:, 0:1]),
                     r=[SMK(2), "onf"], w=[SMK(3)])
                P.op("dve", lambda e: e.scalar_tensor_tensor(out=smv(4), in0=smv(0), scalar=0.0, in1=smv(3),
                                                             op0=ALU.max, op1=ALU.add), r=[SMK(0), SMK(3)], w=[SMK(4)])
                P.op("dve", lambda e: e.tensor_tensor(out=smv(5), in0=smv(4), in1=Abc[:], op=ALU.mult),
                     r=[SMK(4), "Abc"], w=[SMK(5)])
                cur = pre
                if t == 16:
                    P.op("pool", lambda e: e.tensor_scalar(out=pre[:, :, 0:3], in0=pre[:, :, 128:131],
                                                           scalar1=vs("flag", 0), scalar2=None, op0=ALU.mult),
                         r=["pre", "V"], w=["pre"])
                else:
                    P.op("pool", lambda e: e.tensor_copy(out=pre[:, :, 0:3], in_=pre[:, :, 128:131]),
                         r=["pre"], w=["pre"])
                for bg in range(4 if t >= 15 else 3):
                    b = 1 + bg % 2
                    for ti in range(4):
                        tl = bg * 4 + ti
                        for c in range(8):
                            P.op("pe", lambda e, b=b, ti=ti, tl=tl, c=c: e.matmul(
                                bank(b, 128, ti * 128), w1[:, c, tl * 128:(tl + 1) * 128], hT[:, c, :],
                                start=(c == 0), stop=(c == 7)), r=["w1", HK], w=pk(b))
                    eng = "act" if bg % 2 == 0 else "dve"
                    if eng == "act":
                        P.op("act", lambda e, b=b, bg=bg, cur=cur: e.activation(
                            out=cur[:, bg * 4:(bg + 1) * 4, 3:131], in_=bank(b).rearrange("p (a n) -> p a n", a=4),
                            func=AF.Copy), r=pk(b), w=["pre"])
                    else:
                        P.op("dve", lambda e, b=b, bg=bg, cur=cur: e.tensor_copy(
                            out=cur[:, bg * 4:(bg + 1) * 4, 3:131], in_=bank(b).rearrange("p (a n) -> p a n", a=4)),
                            r=pk(b), w=["pre"])
                if t + 1 < 32:
                    NORM(t + 1)
                P.op("pe", lambda e: e.matmul(bank(5, 16, 16), U[:], smv(5), start=True, stop=True),
                     r=["U", SMK(5)], w=pk(5))
                P.op("pe", lambda e: e.matmul(bank(5, 16, 32), onf[:], smv(5), start=True, stop=True),
                     r=["onf", SMK(5)], w=pk(5))
                P.op("dve", lambda e: e.tensor_copy(out=smv(6), in_=bank(5, 16, 16)), r=pk(5), w=[SMK(6)])
                P.op("act", lambda e: e.activation(out=smv(7), in_=bank(5, 16, 16), func=AF.Exp), r=pk(5), w=[SMK(7)])
                P.op("dve", lambda e: e.tensor_tensor(out=smv(10), in0=bank(5, 16, 32), in1=smv(6), op=ALU.subtract),
                     r=pk(5) + [SMK(6)], w=[SMK(10)])
                P.op("act", lambda e: e.activation(out=smv(8), in_=smv(10), func=AF.Exp), r=[SMK(10)], w=[SMK(8)])
                P.op("act", lambda e: e.activation(out=smv(9), in_=bank(5, 16, 32), func=AF.Exp), r=pk(5), w=[SMK(9)])
                if own:
                    for hf in range(2):
                        for c in range(8):
                            P.op("pe", lambda e, hf=hf, c=c: e.matmul(bank(hf), hT[:, c, :],
                                                                      w1[:, c, 2048 + hf * 512: 2048 + (hf + 1) * 512],
                                                                      start=(c == 0), stop=(c == 7)), r=[HK, "w1"], w=pk(hf))
                    P.op("act", lambda e: e.activation(out=szt[:], in_=PS[:, 0:1024], func=AF.Silu), r=pk(0, 1), w=["szt"])
                ntl = 16 if t >= 15 else 12
                for bg in range(ntl // 4):
                    b = 3 + bg % 2
                    for ti in range(4):
                        tl = bg * 4 + ti
                        P.op("pe", lambda e, b=b, ti=ti, tl=tl: e.matmul(
                            bank(b, 128, ti * 128), cbT[:], idb[0:16, tl:tl + 1].broadcast_to([16, 128]),
                            start=True, stop=False), r=["cbT", "idb"], w=pk(b))
                        for k in range(4):
                            P.op("pe", lambda e, b=b, ti=ti, tl=tl, k=k: e.matmul(
                                bank(b, 128, ti * 128), cdiag[:, tl, k, :], pre[:, tl, k:k + 128],
                                start=False, stop=(k == 3)), r=["cdiag", "pre"], w=pk(b))
                    P.op("act", lambda e, b=b, bg=bg: e.activation(
                        out=xbcT[:, bg * 4:(bg + 1) * 4, :], in_=bank(b).rearrange("p (a n) -> p a n", a=4),
                        func=AF.Silu), r=pk(b), w=["xbcT"])
                for c in range(8):
                    P.op("pe", lambda e, c=c: e.transpose(out=bankb(6, 128 * (c + 1))[:, c * 128:(c + 1) * 128],
                                                          in_=xbcT[:, c, :], identity=idb[:]),
                         r=["xbcT", "idb"], w=pk(6))
                for g in range(4):
                    P.op("pe", lambda e, g=g: e.transpose(out=bankb(7, 128 * (g + 1))[:, g * 128:(g + 1) * 128],
                                                          in_=xbcT[:, 8 + g, :], identity=idb[:]),
                         r=["xbcT", "idb"], w=pk(7))
                P.op("dve", lambda e: e.tensor_tensor(out=Xd[:], in0=bankb(6).rearrange("p (h d) -> p h d", h=16),
                                                      in1=smv(4).unsqueeze(2).broadcast_to([128, 16, 64]), op=ALU.mult),
                     r=pk(6) + [SMK(4)], w=["Xd"])
                P.op("act", lambda e: e.activation(out=Btok[:], in_=bankb(7, 512).rearrange("p (g n) -> p g n", g=4),
                                                   func=AF.Copy), r=pk(7), w=["Btok"])
                P.op("pool", lambda e: e.tensor_tensor(out=Xdd[:], in0=Xd[:],
                                                       in1=smv(8).unsqueeze(2).broadcast_to([128, 16, 64]), op=ALU.mult),
                     r=["Xd", SMK(8)], w=["Xdd"])
                if own:
                    P.op("dve", lambda e: e.tensor_tensor(out=rhsA[:], in0=U[:].unsqueeze(1).broadcast_to([128, 16, 128]),
                                                          in1=smv(5).unsqueeze(2).broadcast_to([128, 16, 128]),
                                                          op=ALU.mult), r=["U", SMK(5)], w=["rhsA"])
                    for q4 in range(4):
                        P.op("pe", lambda e, q4=q4: e.matmul(bank(q4), SL[:], rhsA[:, q4 * 4:(q4 + 1) * 4, :],
                                                             start=True, stop=True), r=["SL", "rhsA"], w=pk(q4))
                    P.op("dve", lambda e: e.tensor_tensor(out=y2, in0=bankb(6).rearrange("p (h d) -> p h d", h=16),
                                                          in1=vs("dskip").unsqueeze(2).broadcast_to([128, 16, 64]),
                                                          op=ALU.mult), r=pk(6) + ["V"], w=["rhsA"])
                    P.op("act", lambda e: e.activation(out=Lt[:], in_=PS[:, 0:2048].rearrange("p (h l) -> p h l", h=16),
                                                       func=AF.Exp), r=pk(0, 1, 2, 3), w=["Lt"])
                    for g in range(4):
                        P.op("pe", lambda e, g=g: e.matmul(bank(7, 128, g * 128), xbcT[:, 8 + g, :], xbcT[:, 12 + g, :],
                                                           start=True, stop=True), r=["xbcT"], w=pk(7))
                    P.op("dve", lambda e: e.tensor_tensor(out=CBm[:], in0=bank(7).rearrange("p (g l) -> p g l", g=4),
                                                          in1=U[:].unsqueeze(1).broadcast_to([128, 4, 128]), op=ALU.mult),
                         r=pk(7) + ["U"], w=["CBm"])
                    P.op("dve", lambda e: e.tensor_tensor(
                        out=Lt[:].rearrange("p (g r) l -> p g r l", g=4), in0=Lt[:].rearrange("p (g r) l -> p g r l", g=4),
                        in1=CBm[:].unsqueeze(2).broadcast_to([128, 4, 4, 128]), op=ALU.mult), r=["Lt", "CBm"], w=["Lt"])
                    for h in range(16):
                        P.op("pe", lambda e, h=h: e.matmul(PS[:, 2048 + h * 64: 2048 + (h + 1) * 64], Lt[:, h, :], Xd[:, h, :],
                                                           start=True, stop=True), r=["Lt", "Xd"], w=pk(4 + h // 8))
                    for h in range(16):
                        P.op("pe", lambda e, h=h: e.matmul(PS[:, 3072 + h * 64: 3072 + (h + 1) * 64], xbcT[:, 12 + h // 4, :],
                                                           Sb[:, h * 64:(h + 1) * 64], start=True, stop=True),
                             r=["xbcT", "Sb"], w=pk(6 + h // 8))
                    P.op("dve", lambda e: e.tensor_tensor(out=y1, in0=PS[:, 3072:4096].rearrange("p (h d) -> p h d", h=16),
                                                          in1=smv(7).unsqueeze(2).broadcast_to([128, 16, 64]), op=ALU.mult),
                         r=pk(6, 7) + [SMK(7)], w=["rhsA"])
                    P.op("dve", lambda e: e.tensor_tensor(out=y1, in0=PS[:, 2048:3072].rearrange("p (h d) -> p h d", h=16),
                                                          in1=y1, op=ALU.add), r=pk(4, 5) + ["rhsA"], w=["rhsA"])
                    P.op("dve", lambda e: e.tensor_tensor(out=y2, in0=y2, in1=y1, op=ALU.add),
                         r=["rhsA"], w=["rhsA"])
                    y1f = _rf[:, 0:1024]
                    P.op("dve", lambda e, y1f=y1f: e.tensor_tensor(out=y1f, in0=szt[:], in1=_rf[:, 1024:2048],
                                                                   op=ALU.mult), r=["szt", "rhsA"], w=["rhsA"])
                    P.op("pool", lambda e: e.memset(ssq[:], 0.0), w=["ssq"])
                    for g in range(4):
                        P.op("act", lambda e, g=g, y1f=y1f: e.activation(
                            out=Lt[:, 0:2, :].rearrange("p a l -> p (a l)"), in_=y1f[:, g * 256:(g + 1) * 256],
                            func=AF.Square, accum_out=ssq[:, g:g + 1]), r=["rhsA", "ssq"], w=["Lt", "ssq"])
                    P.op("act", lambda e: e.activation(out=ssq[:], in_=ssq[:], func=AF.Ln, bias=wk["eps"][:, 0:1],
                                                       scale=1.0 / 256), r=["ssq", "eps"], w=["ssq"])
                    P.op("act", lambda e: e.activation(out=ssq[:], in_=ssq[:], func=AF.Exp, scale=-0.5), r=["ssq"], w=["ssq"])
                    P.op("dve", lambda e: e.tensor_tensor(out=yn[:].rearrange("p (g f) -> p g f", g=4),
                                                          in0=y1f.rearrange("p (g f) -> p g f", g=4),
                                                          in1=ssq[:].unsqueeze(2).broadcast_to([128, 4, 256]), op=ALU.mult),
                         r=["rhsA", "ssq"], w=["yn"])
                    for c in range(8):
                        P.op("pe", lambda e, c=c: e.transpose(out=bankb(2, 128 * (c + 1))[:, c * 128:(c + 1) * 128],
                                                              in_=yn[:, c * 128:(c + 1) * 128], identity=idb[:]),
                             r=["yn", "idb"], w=pk(2))
                    P.op("act", lambda e: e.activation(out=ymT[:], in_=bankb(2).rearrange("p (c l) -> p c l", c=8),
                                                       func=AF.Copy), r=pk(2), w=["ymT"])
                    out_proj(P, bank, wo1, "wo1", ymT, "ymT", R, t - 16, (3, 4))
                if t < 31:
                    for h in range(16):
                        P.op("pe", lambda e, h=h: e.matmul(PS[:, 2560 + h * 64: 2560 + (h + 1) * 64], Btok[:, h // 4, :],
                                                           Xdd[:, h, :], start=True, stop=True),
                             r=["Btok", "Xdd"], w=pk(5 + h // 8))
                    P.op("pool", lambda e: e.tensor_tensor(out=S32[:].rearrange("p (h d) -> p h d", h=16),
                                                           in0=S32[:].rearrange("p (h d) -> p h d", h=16),
                                                           in1=smv(9).unsqueeze(2).broadcast_to([128, 16, 64]),
                                                           op=ALU.mult), r=["S32", SMK(9)], w=["S32"])
                    P.op("dve", lambda e: e.tensor_tensor(out=S32[:], in0=PS[:, 2560:3584], in1=S32[:], op=ALU.add),
                         r=pk(5, 6) + ["S32"], w=["S32"])
                    if t == 15:
                        P.op("dve", lambda e: e.tensor_scalar(out=S32[:], in0=S32[:], scalar1=vs("flag", 0), scalar2=None,
                                                              op0=ALU.mult), r=["S32", "V"], w=["S32"])
                    P.op("act", lambda e: e.activation(out=Sb[:], in_=S32[:], func=AF.Copy), r=["S32"], w=["Sb"])
            P.barrier()

        if stage == "ssd":
            return finish(nc, P, R, out3, es)

        with ExitStack() as eW:
            wg = [SB("wg%d" % i, [128, 8, 256], BF16, eW) for i in range(3)]
            wu = [SB("wu%d" % i, [128, 8, 256], BF16, eW) for i in range(3)]
            wd = [SB("wd%d" % i, [128, 2, 1024], BF16, eW) for i in range(3)]
            wr = SB("wrs", [128, 8, 64], F32, eW)

            def load_expert_dma(e_):
                s = e_ % 3
                P.dma_group("pool", [(wg[s][:].rearrange("p c n -> p (c n)"), egd[e_, :, :]),
                                     (wu[s][:].rearrange("p c n -> p (c n)"), eud[e_, :, :]),
                                     (wd[s][:].rearrange("p c n -> p (c n)"), edd[e_, :, :])],
                            w=[("wg", s), ("wu", s), ("wd", s)], sem="ew%d" % s)
            with ExitStack() as e2:
                w2 = SB("w2s", [128, 8, 1792], BF16, e2)
                wo2 = SB("wo2", [128, 8, 1024], BF16, e2)
                bT = SB("bT", [128, 16, 256], BF16, e2)
                xc = [SB("xd%d" % i, [128, 8, 128], F32, e2) for i in range(2)]
                wk = dict(sq=SB("sq2", [128, 8, 128], BF16, e2), rs=SB("rs2", [128, 128], F32, e2),
                          eps=SB("eps2", [128, 1], F32, e2), inplace=True)
                hT = SB("hT2", [128, 8, 128], BF16, e2)
                kT = [SB("kT%d" % i, [128, 4, 128], BF16, e2) for i in range(3)]
                qT = [SB("qT%d" % i, [128, 8, 128], BF16, e2) for i in range(2)]
                Va = [SB("Va%d" % i, [128, 4, 65], BF16, e2) for i in range(3)]
                PT = SB("PT", [128, 16, 2, 128], BF16, e2)
                den = SB("den", [128, 16], F32, e2)
                on = SB("on", [128, 16, 64], F32, e2)
                junk = SB("junk2", [128, 1024], F32, e2)
                ss1 = SB("ss1", [128, 1], F32, e2)
                ya = SB("ya", [128, 1024], BF16, e2)
                yaT = SB("yaT", [128, 8, 128], BF16, e2)

                P.op("pool", lambda e: e.memset(wk["eps"][:], EPS), w=["eps"])
                P.dma_group("pool", [(w2[:, c, :], w2d[:, c, :]) for c in range(8)], w=["w2"], sem="w2")
                P.dma("pool", wo2[:], wod[:, 8:16, :], w=["wo2"], sem="wo2")
                P.dma("pool", bT[:].rearrange("p h n -> p (h n)"), biasd[:, :], w=["bT"], sem="bT")
                load_expert_dma(0)
                load_expert_dma(1)
                P.dma("sp", wr[:], wrd[:, :, :], w=["wr"], sem="wr")
                for c in range(8):
                    P.op("dve", lambda e, c=c: e.scalar_tensor_tensor(out=wo2[:, c, :], in0=wo2[:, c, :], scalar=vs("wog", 8 + c),
                                                                      in1=g1bc[:], op0=ALU.mult, op1=ALU.mult),
                         r=["wo2", "V", "gbc0"], w=["wo2"])
                for i in range(3):
                    P.op("pool", lambda e, i=i: e.memset(Va[i][:], 1.0), w=[("Va", i)])

                def F(t):
                    own = t >= 0
                    par = t % 2
                    p3 = t % 3
                    if own:
                        P.dma("sp", xc[par][:], xo3[:, :, t * 128:(t + 1) * 128], w=[("xc", par)], sem="xc%d" % par)
                    else:
                        P.dma("sp", xc[par][:], xp3[:, :, NT - 128:NT], w=[("xc", par)], sem="xc%d" % par)
                    wk["hn"] = xc[par]
                    norm_h(xc[par][:], [("xc", par)], hT[:], ["hT"], A1, 0, wk, 128, 0)
                    for kh in range(4):
                        for c in range(8):
                            P.op("pe", lambda e, kh=kh, c=c: e.matmul(bank(1, 128, kh * 128), w2[:, c, 1024 + kh * 128:1024 + (kh + 1) * 128],
                                                                      hT[:, c, :], start=(c == 0), stop=(c == 7)),
                                 r=["w2", "hT"], w=pk(1))
                    P.op("act", lambda e, p3=p3: e.activation(out=kT[p3][:], in_=bank(1).rearrange("p (a n) -> p a n", a=4),
                                                                func=AF.Copy), r=pk(1), w=[("kT", p3)])
                    for c in range(8):
                        P.op("pe", lambda e, c=c: e.matmul(bank(0, 256, 128), hT[:, c, :], w2[:, c, 1536:1792],
                                                           start=(c == 0), stop=(c == 7)), r=["w2", "hT"], w=pk(0))
                    P.op("dve", lambda e, p3=p3: e.tensor_copy(out=Va[p3][:, :, 0:64],
                                                                in_=bank(0, 256, 128).rearrange("p (k d) -> p k d", k=4)),
                         r=pk(0), w=[("Va", p3)])
                    if not own:
                        return
                    for bg in range(2):
                        b = 2
                        for ti in range(4):
                            tl = bg * 4 + ti
                            for c in range(8):
                                P.op("pe", lambda e, b=b, ti=ti, tl=tl, c=c: e.matmul(
                                    bank(b, 128, ti * 128), w2[:, c, tl * 128:(tl + 1) * 128], hT[:, c, :],
                                    start=(c == 0), stop=(c == 7)), r=["w2", "hT"], w=pk(b))
                        P.op("act", lambda e, b=b, bg=bg, par=par: e.activation(out=qT[par][:, bg * 4:(bg + 1) * 4, :],
                                                                       in_=bank(b).rearrange("p (a n) -> p a n", a=4),
                                                                       func=AF.Copy, scale=0.125), r=pk(b), w=[("qT", par)])
                def B(t):
                    par = t % 2
                    p3, q3 = t % 3, (t - 1) % 3
                    for hp in range(8):
                        b = 5 + hp % 2
                        for hh in range(2):
                            h = 2 * hp + hh
                            kh = h // 4
                            lo, hi = hh * 64, (hh + 1) * 64
                            P.op("pe", lambda e, b=b, hh=hh, h=h: e.matmul(bank(b, 256, hh * 256), idb[:], bT[:, h, :],
                                                                           start=True, stop=False), r=["idb", "bT"], w=pk(b))
                            P.op("pe", lambda e, b=b, hh=hh, kh=kh, lo=lo, hi=hi, hp=hp: e.matmul(
                                bank(b, 128, hh * 256), kT[q3][lo:hi, kh, :], qT[par][lo:hi, hp, :], start=False, stop=False),
                                r=[("kT", q3), ("qT", par)], w=pk(b))
                            P.op("pe", lambda e, b=b, hh=hh, kh=kh, lo=lo, hi=hi, hp=hp: e.matmul(
                                bank(b, 128, hh * 256 + 128), kT[p3][lo:hi, kh, :], qT[par][lo:hi, hp, :], start=False, stop=True),
                                r=[("kT", p3), ("qT", par)], w=pk(b))
                        P.op("act", lambda e, b=b, hp=hp: e.activation(
                            out=PT[:, 2 * hp:2 * hp + 2, :, :].rearrange("p h b q -> p (h b q)"), in_=bank(b), func=AF.Exp),
                            r=pk(b), w=["PT"])
                    if t == 0:
                        P.op("pool", lambda e: e.tensor_scalar(out=PT[:, :, 0, :], in0=PT[:, :, 0, :], scalar1=vs("flag", 0),
                                                               scalar2=None, op0=ALU.mult), r=["PT", "V"], w=["PT"])
                    hb = ((0, 6, 3), (6, 12, 4), (12, 16, 7))
                    for h0, h1, b in hb:
                        for h in range(h0, h1):
                            kh = h // 4
                            o_ = (h - h0) * 65
                            P.op("pe", lambda e, b=b, o_=o_, h=h, kh=kh: e.matmul(bank(b, 65, o_), PT[:, h, 0, :], Va[q3][:, kh, :],
                                                                                  start=True, stop=False),
                                 r=["PT", ("Va", q3)], w=pk(b))
                            P.op("pe", lambda e, b=b, o_=o_, h=h, kh=kh: e.matmul(bank(b, 65, o_), PT[:, h, 1, :], Va[p3][:, kh, :],
                                                                                  start=False, stop=True),
                                 r=["PT", ("Va", p3)], w=pk(b))
                    for h0, h1, b in hb:
                        n = h1 - h0
                        pv = bank(b, n * 65).rearrange("p (h d) -> p h d", h=n)
                        P.op("dve", lambda e, pv=pv, h0=h0, h1=h1: e.tensor_tensor(out=den[:, h0:h1], in0=pv[:, :, 64],
                                                                                   in1=esink[:, h0:h1], op=ALU.add),
                             r=pk(b) + ["esink"], w=["den"])
                    P.op("dve", lambda e: e.reciprocal(out=den[:], in_=den[:]), r=["den"], w=["den"])
                    for h0, h1, b in hb:
                        n = h1 - h0
                        pv = bank(b, n * 65).rearrange("p (h d) -> p h d", h=n)
                        P.op("dve", lambda e, pv=pv, h0=h0, h1=h1, n=n: e.tensor_tensor(
                            out=on[:, h0:h1, :], in0=pv[:, :, 0:64], in1=den[:, h0:h1].unsqueeze(2).broadcast_to([128, n, 64]),
                            op=ALU.mult), r=pk(b) + ["den"], w=["on"])
                    P.op("pool", lambda e: e.memset(ss1[:], 0.0), w=["ss1"])
                    P.op("act", lambda e: e.activation(out=junk[:], in_=on[:].rearrange("p h d -> p (h d)"), func=AF.Square,
                                                       accum_out=ss1[:, 0:1]), r=["on", "ss1"], w=["junk", "ss1"])
                    P.op("act", lambda e: e.activation(out=ss1[:], in_=ss1[:], func=AF.Ln, bias=wk["eps"][:, 0:1],
                                                       scale=1.0 / 1024), r=["ss1", "eps"], w=["ss1"])
                    P.op("act", lambda e: e.activation(out=ss1[:], in_=ss1[:], func=AF.Exp, scale=-0.5), r=["ss1"], w=["ss1"])
                    P.op("dve", lambda e: e.tensor_scalar(out=ya[:], in0=on[:].rearrange("p h d -> p (h d)"), scalar1=ss1[:, 0:1],
                                                          scalar2=None, op0=ALU.mult), r=["on", "ss1"], w=["ya"])
                    for c in range(8):
                        P.op("pe", lambda e, c=c: e.transpose(out=bankb(5, 128 * (c + 1))[:, c * 128:(c + 1) * 128],
                                                              in_=ya[:, c * 128:(c + 1) * 128], identity=idb[:]),
                             r=["ya", "idb"], w=pk(5))
                    P.op("act", lambda e: e.activation(out=yaT[:], in_=bankb(5).rearrange("p (c l) -> p c l", c=8),
                                                       func=AF.Copy), r=pk(5), w=["yaT"])
                    out_proj(P, bank, wo2, "wo2", yaT, "yaT", R, t, (6, 7))
                F(-1)
                F(0)
                for t in range(16):
                    if t + 1 < 16:
                        F(t + 1)
                    B(t)
                P.barrier()

            if stage == "mixer":
                return finish(nc, P, R, out3, es)

            with ExitStack() as e3:
                h2T = SB("h2T", [128, 8, NT], BF16, e3)
                gT = SB("gT", [128, NT], BF16, e3)
                selE = SB("selE", [128, NEXP, 128], BF16, e3)
                wk = dict(sq=SB("sq3", [128, 8, 512], BF16, e3), rs=SB("rs3", [128, 512], F32, e3),
                          hn=SB("hn3", [128, 8, 512], F32, e3), eps=SB("eps3", [128, 1], F32, e3))
                rt = SB("rt", [128, 12, 64], F32, e3)
                gsb = SB("gsb", [128, 512], F32, e3)
                sg = [SB("sg%d" % i, [128, 512], BF16, e3) for i in range(2)]
                tt = [SB("tt%d" % i, [128, 512], BF16, e3) for i in range(2)]
                uu = [[SB("uu%d%d" % (i, f), [128, 512], BF16, e3) for f in range(2)] for i in range(2)]

                g2bc = SB("g2bc", [128, 1024], F32, e3)
                dg2 = SB("dg2", [128, 128], F32, e3)
                for c in range(8):
                    P.op("dve", lambda e, c=c: e.tensor_scalar(out=dg2[:], in0=idf[:], scalar1=MOD[:, 40 + c:41 + c],
                                                               scalar2=None, op0=ALU.mult), r=["MOD", "idf"], w=["dg2"])
                    P.op("pe", lambda e, c=c: e.matmul(bank(3 + c // 4, 128, (c % 4) * 128), onf[:], dg2[:], start=True, stop=True),
                         r=["onf", "dg2"], w=pk(3 + c // 4))
                P.op("dve", lambda e: e.tensor_copy(out=g2bc[:], in_=PS[:, 3 * 512:5 * 512]), r=pk(3, 4), w=["gbc1"])
                P.op("pool", lambda e: e.memset(wk["eps"][:], EPS), w=["eps"])
                P.op("pool", lambda e: e.memset(selE[:], 1.0), w=["selE"])
                P.op("pool", lambda e: e.affine_select(out=selE[:], in_=selE[:], pattern=[[1, NEXP], [0, 128]],
                                                       compare_op=ALU.is_equal, fill=0.0, base=0, channel_multiplier=-1),
                     r=["selE"], w=["selE"])
                P.op("pool", lambda e: e.memset(gT[:], 0.0), w=[("gT", g_) for g_ in range(4)])
                P.op("pool", lambda e: e.memset(gT[64:65, :], 1.0), w=[("gT", g_) for g_ in range(4)])

                def scale_expert(e_):
                    s = e_ % 3
                    P.op("pool", lambda e, s=s: e.tensor_tensor(out=wd[s][:], in0=wd[s][:],
                                                                in1=g2bc[:].unsqueeze(1).broadcast_to([128, 2, 1024]), op=ALU.mult),
                         r=[("wd", s), "gbc1"], w=[("wd", s)])

                def load_expert(e_):
                    load_expert_dma(e_)
                    scale_expert(e_)
                scale_expert(0)
                scale_expert(1)

                rv = lambda i, n=64: rt[:, i, 0:n]
                RK = lambda i: ("rt", i)
                def PRO(tg):
                    ts_ = slice(tg * 512, (tg + 1) * 512)
                    norm_h(R[:, :, ts_], [("R", c, tg) for c in range(8)], h2T[:, :, ts_], [("h2T", tg)], A2, 24, wk, 512, 5)
                    P.op("dve", lambda e: e.tensor_tensor(out=wk["hn"][:], in0=wk["hn"][:],
                                                          in1=MOD[:, 24:32].unsqueeze(2).broadcast_to([128, 8, 512]), op=ALU.add),
                         r=["hn", "MOD"], w=["hn"])
                    for tl in range(4):
                        for c in range(8):
                            P.op("pe", lambda e, tl=tl, c=c: e.matmul(bank(6, 64), wk["hn"][:, c, tl * 128:(tl + 1) * 128], wr[:, c, :],
                                                                      start=(c == 0), stop=(c == 7)), r=["hn", "wr"], w=pk(6))
                        P.op("act", lambda e: e.activation(out=rv(0), in_=bank(6, 64), func=AF.Sigmoid), r=pk(6), w=[RK(0)])
                        P.op("dve", lambda e: e.tensor_tensor(out=rv(1), in0=rv(0), in1=vs("rbias"), op=ALU.add),
                             r=[RK(0), "V"], w=[RK(1)])
                        g3 = lambda ap: ap.rearrange("p (g e) -> p g e", e=8)
                        b3 = lambda ap: ap.unsqueeze(2).broadcast_to([128, 8, 8])
                        P.op("dve", lambda e: e.tensor_reduce(out=rv(6, 8), in_=g3(rv(1)), axis=AX.X, op=ALU.max), r=[RK(1)], w=[RK(6)])
                        P.op("dve", lambda e: e.tensor_tensor(out=g3(rv(2)), in0=g3(rv(1)), in1=b3(rv(6, 8)), op=ALU.is_equal),
                             r=[RK(1), RK(6)], w=[RK(2)])
                        P.op("dve", lambda e: e.scalar_tensor_tensor(out=rv(2), in0=rv(2), scalar=-1e9, in1=rv(1), op0=ALU.mult,
                                                                     op1=ALU.add), r=[RK(2), RK(1)], w=[RK(2)])
                        P.op("dve", lambda e: e.tensor_reduce(out=rv(7, 8), in_=g3(rv(2)), axis=AX.X, op=ALU.max), r=[RK(2)], w=[RK(7)])
                        P.op("dve", lambda e: e.tensor_tensor(out=rv(7, 8), in0=rv(7, 8), in1=rv(6, 8), op=ALU.add),
                             r=[RK(7), RK(6)], w=[RK(7)])
                        P.op("dve", lambda e: e.max(out=rv(8, 8), in_=rv(7, 8)), r=[RK(7)], w=[RK(8)])
                        P.op("dve", lambda e: e.tensor_scalar(out=rv(9, 8), in0=rv(7, 8), scalar1=rt[:, 8, 3:4], scalar2=None,
                                                              op0=ALU.is_ge), r=[RK(7), RK(8)], w=[RK(9)])
                        P.op("dve", lambda e: e.scalar_tensor_tensor(out=g3(rv(3)), in0=g3(rv(1)), scalar=2.0, in1=b3(rv(9, 8)),
                                                                     op0=ALU.add, op1=ALU.mult), r=[RK(1), RK(9)], w=[RK(3)])
                        P.op("dve", lambda e: e.max(out=rv(10, 8), in_=rv(3)), r=[RK(3)], w=[RK(10)])
                        P.op("dve", lambda e: e.tensor_scalar(out=rv(4), in0=rv(3), scalar1=rt[:, 10, 7:8], scalar2=None,
                                                              op0=ALU.is_ge), r=[RK(3), RK(10)], w=[RK(4)])
                        P.op("dve", lambda e: e.tensor_tensor(out=rv(4), in0=rv(4), in1=rv(0), op=ALU.mult),
                             r=[RK(4), RK(0)], w=[RK(4)])
                        P.op("dve", lambda e: e.tensor_reduce(out=rv(11, 1), in_=rv(4), axis=AX.X, op=ALU.add), r=[RK(4)], w=[RK(11)])
                        P.op("dve", lambda e: e.reciprocal(out=rv(11, 1), in_=rv(11, 1)), r=[RK(11)], w=[RK(11)])
                        P.op("dve", lambda e: e.tensor_scalar(out=rv(5), in0=rv(4), scalar1=rt[:, 11, 0:1], scalar2=2.5,
                                                              op0=ALU.mult, op1=ALU.mult), r=[RK(4), RK(11)], w=[RK(5)])
                        P.op("pe", lambda e: e.transpose(out=bank(7, 128)[0:64, :], in_=rv(5), identity=idf[:]),
                             r=[RK(5), "idf"], w=pk(7))
                        col = tg * 512 + tl * 128
                        P.op("act", lambda e, col=col: e.activation(out=gT[0:64, col:col + 128], in_=bank(7, 128)[0:64, :],
                                                                    func=AF.Copy), r=pk(7), w=[("gT", tg)])

                PRO(0)
                if stage != "norouted":
                    experts = list(range(NEXP))
                else:
                    experts = [64]
                steps = [(e_, tg) for e_ in experts for tg in range(4)]

                def down(i):
                    e_, tg = steps[i]
                    s = e_ % 3
                    for dtl in range(8):
                        b = 5 + dtl % 3
                        for f in range(2):
                            P.op("pe", lambda e, b=b, s=s, f=f, dtl=dtl, i=i: e.matmul(
                                bank(b), wd[s][:, f, dtl * 128:(dtl + 1) * 128], uu[i % 2][f][:], start=(f == 0), stop=(f == 1)),
                                r=[("wd", s), ("uu", i % 2, f)], w=pk(b))
                        P.op("dve", lambda e, b=b, dtl=dtl, tg=tg: e.tensor_tensor(
                            out=R[:, dtl, tg * 512:(tg + 1) * 512], in0=bank(b), in1=R[:, dtl, tg * 512:(tg + 1) * 512], op=ALU.add),
                            r=pk(b) + [("R", dtl, tg)], w=[("R", dtl, tg)])

                for i, (e_, tg) in enumerate(steps):
                    s = e_ % 3
                    if stage == "norouted" and i == 0:
                        P.barrier()
                        load_expert(64)
                    P.op("pe", lambda e, e_=e_, tg=tg: e.matmul(bank(0), selE[:, e_, :], gT[:, tg * 512:(tg + 1) * 512],
                                                                start=True, stop=True), r=["selE", ("gT", tg)], w=pk(0))
                    P.op("act", lambda e: e.activation(out=gsb[:], in_=bank(0), func=AF.Copy), r=pk(0), w=["gsb"])
                    for f in range(2):
                        bG, bU = 1 + 2 * f, 2 + 2 * f
                        for c in range(8):
                            P.op("pe", lambda e, bG=bG, s=s, c=c, f=f, tg=tg: e.matmul(
                                bank(bG), wg[s][:, c, f * 128:(f + 1) * 128], h2T[:, c, tg * 512:(tg + 1) * 512],
                                start=(c == 0), stop=(c == 7)), r=[("wg", s), ("h2T", tg)], w=pk(bG))
                        for c in range(8):
                            P.op("pe", lambda e, bU=bU, s=s, c=c, f=f, tg=tg: e.matmul(
                                bank(bU), wu[s][:, c, f * 128:(f + 1) * 128], h2T[:, c, tg * 512:(tg + 1) * 512],
                                start=(c == 0), stop=(c == 7)), r=[("wu", s), ("h2T", tg)], w=pk(bU))
                        P.op("act", lambda e, bG=bG, f=f: e.activation(out=sg[f][:], in_=bank(bG), func=AF.Silu),
                             r=pk(bG), w=[("sg", f)])
                        P.op("dve", lambda e, bU=bU, f=f: e.tensor_tensor(out=tt[f][:], in0=bank(bU), in1=gsb[:], op=ALU.mult),
                             r=pk(bU) + ["gsb"], w=[("tt", f)])
                        P.op("dve", lambda e, f=f, i=i: e.tensor_tensor(out=uu[i % 2][f][:], in0=sg[f][:], in1=tt[f][:], op=ALU.mult),
                             r=[("sg", f), ("tt", f)], w=[("uu", i % 2, f)])
                    if i > 0:
                        down(i - 1)
                    if e_ == experts[0] and tg + 1 < 4:
                        PRO(tg + 1)
                    if tg == 0 and stage != "norouted" and e_ + 2 < NEXP:
                        load_expert(e_ + 2)
                down(len(steps) - 1)

                for tg in range(4):
                    ts_ = slice(tg * 512, (tg + 1) * 512)
                    sq, rs, hn = wk["sq"], wk["rs"], wk["hn"]
                    P.op("act", lambda e, ts_=ts_: e.activation(out=sq[:], in_=R[:, :, ts_], func=AF.Square),
                         r=[("R", c, tg) for c in range(8)], w=["sq"])
                    for c in range(8):
                        P.op("pe", lambda e, c=c: e.matmul(bank(0), onb[:], sq[:, c, :], start=(c == 0), stop=(c == 7)),
                             r=["sq", "onb"], w=pk(0))
                    P.op("act", lambda e: e.activation(out=rs[:], in_=bank(0), func=AF.Ln, bias=wk["eps"][:, 0:1], scale=1.0 / 1024),
                         r=pk(0) + ["eps"], w=["rs"])
                    P.op("act", lambda e: e.activation(out=rs[:], in_=rs[:], func=AF.Exp, scale=-0.5), r=["rs"], w=["rs"])
                    P.op("dve", lambda e, ts_=ts_: e.tensor_tensor(out=hn[:], in0=R[:, :, ts_],
                                                                   in1=rs[:].unsqueeze(1).broadcast_to([128, 8, 512]), op=ALU.mult),
                         r=[("R", c, tg) for c in range(8)] + ["rs"], w=["hn"])
                    P.op("pool", lambda e: e.tensor_tensor(out=hn[:], in0=hn[:], in1=vs("fg").unsqueeze(2).broadcast_to([128, 8, 512]),
                                                           op=ALU.mult), r=["hn", "V"], w=["hn"])
                    P.dma_group("sp", [(out3[:, c, ts_], hn[:, c, :]) for c in range(8)], r=["hn"], sem="out")
                P.final_wait("sp", "out")
                P.emit()
    return nc


def out_proj(P, bank, wo, wokey, yT, ykey, R, t, banks):
    for half in range(2):
        b = banks[half]
        for di in range(4):
            dtl = half * 4 + di
            for fc in range(8):
                P.op("pe", lambda e, b=b, di=di, dtl=dtl, fc=fc: e.matmul(
                    bank(b, 128, di * 128), wo[:, fc, dtl * 128:(dtl + 1) * 128], yT[:, fc, :],
                    start=(fc == 0), stop=(fc == 7)), r=[wokey, ykey], w=[("ps", b)])
        P.op("dve", lambda e, b=b, half=half: e.tensor_tensor(
            out=R[:, half * 4:(half + 1) * 4, t * 128:(t + 1) * 128], in0=bank(b).rearrange("p (a n) -> p a n", a=4),
            in1=R[:, half * 4:(half + 1) * 4, t * 128:(t + 1) * 128], op=ALU.add),
            r=[("ps", b)] + [("R", half * 4 + i) for i in range(4)], w=[("R", half * 4 + i) for i in range(4)])


def finish(nc, P, R, out3, es):
    for c in range(8):
        P.dma("sp", out3[:, c, :], R[:, c, :], r=[("R", c)], sem="out")
    P.final_wait("sp", "out")
    P.emit()
    return nc


def _fm(v, n):
    return np.ascontiguousarray(np.asarray(v, np.float32).reshape(n, 128).T)


def _wl(w):
    K, N = w.shape
    return np.ascontiguousarray(np.asarray(w, np.float32).reshape(K // 128, 128, N).transpose(1, 0, 2))


def _bucket_table():
    import math
    q = np.arange(128)[None, :]
    j = np.arange(128)[:, None]
    tabs, valid = [], []
    for blk in range(2):
        dist = q + 128 - (blk * 128 + j)
        ok = (dist >= 0) & (dist < 128)
        d = np.maximum(dist, 0)
        dd = np.maximum(d, 1).astype(np.float32)
        large = 16 + (np.log(dd / np.float32(16)) / np.float32(math.log(128 / 16)) * np.float32(16)).astype(np.int32)
        large = np.minimum(large, 31)
        tabs.append(np.where(d < 16, d, large))
        valid.append(ok)
    return np.stack(tabs, 1), np.stack(valid, 1)


def make_in_maps(inp):
    f = lambda k: np.asarray(inp[k], np.float32)
    x, c = f("x"), f("c")
    W = f("w_in")[0]
    z_, xbc_, dt_, q_, k_, v_ = (W[:, 0:1024], W[:, 1024:3072], W[:, 3072:3088], W[:, 3088:4112], W[:, 4112:4368],
                                 W[:, 4368:4624])
    w1 = _wl(np.concatenate([xbc_, z_, dt_], 1))
    kd = np.concatenate([np.concatenate([k_[:, h * 64:(h + 1) * 64]] * 2, 1) for h in range(4)], 1)
    w2 = _wl(np.concatenate([q_, kd, v_], 1))
    wo = _wl(f("w_out")[0])
    wr = _wl(f("router_w")[0])
    modw = _wl(f("mod_w")[0])
    tab, ok = _bucket_table()
    rb = f("rel_bias")
    bias = rb[tab]
    bias = np.where(ok[..., None], bias, np.float32(-30000.0)).transpose(0, 3, 1, 2)
    biasT = np.ascontiguousarray(bias.reshape(128, 16 * 256), np.float32)
    eg = np.concatenate([f("exp_w_gate")[0], f("sh_w_gate")], 0)
    eu = np.concatenate([f("exp_w_up")[0], f("sh_w_up")], 0)
    ed = np.concatenate([f("exp_w_down")[0], f("sh_w_down")], 0)
    eg = np.ascontiguousarray(eg.reshape(NEXP, 8, 128, 256).transpose(0, 2, 1, 3).reshape(NEXP, 128, 2048))
    eu = np.ascontiguousarray(eu.reshape(NEXP, 8, 128, 256).transpose(0, 2, 1, 3).reshape(NEXP, 128, 2048))
    ed = np.ascontiguousarray(ed.reshape(NEXP, 2, 128, 1024).transpose(0, 2, 1, 3).reshape(NEXP, 128, 2048))
    bc = lambda v: np.broadcast_to(np.asarray(v, np.float32).reshape(1, -1), (128, np.asarray(v).size))
    cbT = np.ascontiguousarray(f("conv_b")[0].reshape(16, 128))
    maps = []
    for core in range(8):
        b, hf = core // 2, core % 2
        vec = np.zeros((128, NV), np.float32)

        def put(n, a):
            o, w = _VOFF[n]
            vec[:, o:o + w] = a
        put("n1g", _fm(f("norm1_g")[0], 8))
        put("n2g", _fm(f("norm2_g")[0], 8))
        put("fg", _fm(f("final_g"), 8))
        put("modb", _fm(f("mod_b")[0], 48))
        put("convw", f("conv_w")[0].T.reshape(16, 128, 4).transpose(1, 0, 2).reshape(128, 64))
        put("convb", _fm(f("conv_b")[0], 16))
        put("dtb", bc(f("dt_bias")[0]))
        put("alog", bc(f("a_log")[0]))
        put("dskip", bc(f("d_skip")[0]))
        put("sinks", bc(f("sinks")[0]))
        put("rbias", bc(f("router_bias")[0]))
        put("wog", _fm(np.concatenate([f("ssm_norm_g")[0], f("att_norm_g")[0]]), 16))
        put("flag", np.full((128, 1), float(hf), np.float32))
        put("cT", _fm(c[b], 8))
        xo = np.ascontiguousarray(x[b, hf * NT:(hf + 1) * NT, :].T)
        xp = np.ascontiguousarray(x[b, 0:NT, :].T) if hf == 1 else np.zeros((1024, NT), np.float32)
        maps.append(dict(xoT=xo, xpT=xp, vec=vec, modw=modw, cbT=cbT, w1=w1, w2=w2, wo=wo, wr=wr, biasT=biasT,
                         eg=eg, eu=eu, ed=ed))
    return maps


_NC_CACHE = {}


def run(inp, stage="full"):
    if stage not in _NC_CACHE:
        _NC_CACHE[stage] = build_nc(stage)
    nc = _NC_CACHE[stage]
    maps = make_in_maps(inp)
    res = run_bass_kernel_spmd(nc, maps, core_ids=list(range(8)))
    out = np.empty((4, 4096, 1024), np.float32)
    for core in range(8):
        b, hf = core // 2, core % 2
        out[b, hf * NT:(hf + 1) * NT, :] = res.results[core]["outT"].T
    return out


def kernel(**inputs):
    return run(inputs, "full")
```

```python
import numpy as np
from contextlib import ExitStack
import concourse.bass as bass
import concourse.mybir as mybir
from concourse.bass_utils import run_bass_kernel_spmd

F32, BF16 = mybir.dt.float32, mybir.dt.bfloat16
AF = mybir.ActivationFunctionType
ALU = mybir.AluOpType
AX = mybir.AxisListType
EPS = 1e-6
NT = 2048
NCH = NT // 128
NEXP = 65

_VOFF = {}
_o = 0
for _n, _w in (("n1g", 8), ("n2g", 8), ("fg", 8), ("modb", 48), ("convw", 64), ("convb", 16), ("dtb", 16), ("alog", 16),
               ("dskip", 16), ("sinks", 16), ("rbias", 64), ("wog", 16), ("flag", 1), ("cT", 8)):
    _VOFF[_n] = (_o, _w)
    _o += _w
NV = _o


class _Rec:
    def __init__(self):
        self.call = None

    def __getattr__(self, name):
        def f(*a, **k):
            self.call = (name, a, k)
            return self
        return f


class Prog:
    def __init__(self, nc, es):
        self.nc, self.es = nc, es
        self.names = ("pe", "dve", "act", "pool", "sp")
        self.semh, self.cnt = {}, {}
        for k in self.names:
            self.semh[k] = es.enter_context(nc.semaphore("c_" + k))
            self.cnt[k] = 0
        self.seen = {k: {} for k in self.names}
        self.lastw, self.readers = {}, {}
        self.ops = {k: [] for k in self.names}

    def _deps(self, eng, r, w):
        deps = {}

        def add(d):
            if d is None:
                return
            s, v = d
            if s == eng and eng in ("pe", "sp"):
                return
            if deps.get(s, 0) < v:
                deps[s] = v
        for k in r:
            add(self.lastw.get(k))
        for k in w:
            add(self.lastw.get(k))
            for s, v in self.readers.get(k, {}).items():
                add((s, v))
        out = []
        for s, v in deps.items():
            if self.seen[eng].get(s, 0) < v:
                self.seen[eng][s] = v
                out.append((s, v))
        return out

    def _mark(self, me, r, w):
        for k in r:
            self.readers.setdefault(k, {})[me[0]] = me[1]
        for k in w:
            self.lastw[k] = me
            self.readers[k] = {}

    def op(self, eng, fn, r=(), w=()):
        rec = _Rec()
        fn(rec)
        name_, a_, k_ = rec.call
        fn = (lambda e, name_=name_, a_=a_, k_=k_: getattr(e, name_)(*a_, **k_))
        waits = self._deps(eng, r, w)
        self.cnt[eng] += 1
        self._mark((eng, self.cnt[eng]), r, w)
        self.ops[eng].append((waits, fn, (eng, 1)))

    def dma(self, q, out, in_, r=(), w=(), sem="d"):
        if sem not in self.semh:
            self.semh[sem] = self.es.enter_context(self.nc.semaphore("d_" + sem))
            self.cnt[sem] = 0
        waits = self._deps(q, r, w)
        self.cnt[sem] += 16
        self._mark((sem, self.cnt[sem]), r, w)
        self.ops[q].append((waits, (lambda e, o=out, i=in_: e.dma_start(out=o, in_=i)), (sem, 16)))

    def dma_group(self, q, pairs, r=(), w=(), sem="d"):
        if sem not in self.semh:
            self.semh[sem] = self.es.enter_context(self.nc.semaphore("d_" + sem))
            self.cnt[sem] = 0
        waits = self._deps(q, r, w)
        for i, (o, i_) in enumerate(pairs):
            self.cnt[sem] += 16
            self.ops[q].append((waits if i == 0 else [], (lambda e, o=o, i_=i_: e.dma_start(out=o, in_=i_)), (sem, 16)))
        self._mark((sem, self.cnt[sem]), r, w)

    def barrier(self):
        tgt = dict(self.cnt)
        for eng in self.names:
            waits = []
            for s, v in tgt.items():
                if v > 0 and self.seen[eng].get(s, 0) < v and not (s == eng):
                    self.seen[eng][s] = v
                    waits.append((s, v))
            if waits:
                self.ops[eng].append((waits, None, None))
        self.lastw, self.readers = {}, {}

    def final_wait(self, eng, sem):
        self.ops[eng].append(([(sem, self.cnt[sem])], None, None))

    def emit(self):
        block = self.es.enter_context(self.nc.Block())
        decs = dict(pe=block.tensor, dve=block.vector, act=block.scalar, pool=block.gpsimd, sp=block.sync)
        for name in self.names:
            ops = self.ops[name]

            def body(e, ops=ops):
                for waits, fn, inc in ops:
                    for ws, wv in waits:
                        e.wait_ge(self.semh[ws], wv)
                    if fn is not None:
                        fn(e).then_inc(self.semh[inc[0]], inc[1])
            decs[name](body)


def build_nc(stage="full"):
    nc = bass.Bass("TRN2", target_bir_lowering=False)
    D = lambda n, sh: nc.dram_tensor(n, sh, F32, kind="ExternalInput").ap()
    xoT = D("xoT", [1024, NT])
    xpT = D("xpT", [1024, NT])
    vec = D("vec", [128, NV])
    modw = D("modw", [128, 8, 6144])
    cbTd = D("cbT", [16, 128])
    w1d = D("w1", [128, 8, 3088])
    w2d = D("w2", [128, 8, 1792])
    wod = D("wo", [128, 16, 1024])
    wrd = D("wr", [128, 8, 64])
    biasd = D("biasT", [128, 16 * 256])
    egd = D("eg", [NEXP, 128, 8 * 256])
    eud = D("eu", [NEXP, 128, 8 * 256])
    edd = D("ed", [NEXP, 128, 2 * 1024])
    outT = nc.dram_tensor("outT", [1024, NT], F32, kind="ExternalOutput").ap()
    xo3 = xoT.rearrange("(c p) t -> p c t", p=128)
    xp3 = xpT.rearrange("(c p) t -> p c t", p=128)
    out3 = outT.rearrange("(c p) t -> p c t", p=128)

    with ExitStack() as es:
        P = Prog(nc, es)
        SB = lambda n, sh, dt, st=es: st.enter_context(nc.sbuf_tensor(n, sh, dt))
        PS = es.enter_context(nc.psum_tensor("PS", [128, 4096], F32))
        PSB = PS[:].bitcast(BF16)

        def bank(b, n=512, off=0):
            return PS[:, b * 512 + off: b * 512 + off + n]

        def bankb(b, n=1024):
            return PSB[:, b * 1024: b * 1024 + n]
        pk = lambda *bs: [("ps", b) for b in bs]

        R = SB("R", [128, 8, NT], F32)
        V = SB("V", [128, NV], F32)
        U = SB("U", [128, 128], F32)
        SL = SB("SL", [128, 128], F32)
        idf = SB("idf", [128, 128], F32)
        idb = SB("idb", [128, 128], BF16)
        onf = SB("onf", [128, 128], F32)
        onb = SB("onb", [128, 128], BF16)
        MOD = SB("MOD", [128, 48], F32)
        A1 = SB("A1", [128, 8], F32)
        A2 = SB("A2", [128, 8], F32)
        g1bc = SB("g1bc", [128, 1024], F32)
        Abc = SB("Abc", [128, 16], F32)
        esink = SB("esink", [128, 16], F32)
        vs = lambda n, i=None: (V[:, _VOFF[n][0]:_VOFF[n][0] + _VOFF[n][1]] if i is None
                                else V[:, _VOFF[n][0] + i:_VOFF[n][0] + i + 1])

        P.dma("sp", V[:], vec[:, :], w=["V"], sem="v")
        P.op("pool", lambda e: e.memset(U[:], 1.0), w=["U"])
        P.op("pool", lambda e: e.affine_select(out=U[:], in_=U[:], pattern=[[1, 128]], compare_op=ALU.is_ge,
                                               fill=0.0, base=0, channel_multiplier=-1), r=["U"], w=["U"])
        P.op("pool", lambda e: e.memset(SL[:], 1.0), w=["SL"])
        P.op("pool", lambda e: e.affine_select(out=SL[:], in_=SL[:], pattern=[[-1, 128]], compare_op=ALU.is_ge,
                                               fill=0.0, base=-1, channel_multiplier=1), r=["SL"], w=["SL"])
        P.op("pool", lambda e: e.memset(idf[:], 1.0), w=["idf"])
        P.op("pool", lambda e: e.affine_select(out=idf[:], in_=idf[:], pattern=[[1, 128]], compare_op=ALU.is_equal,
                                               fill=0.0, base=0, channel_multiplier=-1), r=["idf"], w=["idf"])
        P.op("pool", lambda e: e.tensor_copy(out=idb[:], in_=idf[:]), r=["idf"], w=["idb"])
        P.op("pool", lambda e: e.memset(onf[:], 1.0), w=["onf"])
        P.op("pool", lambda e: e.memset(onb[:], 1.0), w=["onb"])

        def norm_h(xsrc, xkey, hT, hkey, Ag, sh_off, wk, n, ssb):
            sq, rs, hn = wk["sq"], wk["rs"], wk["hn"]
            hk = list(xkey) if wk.get("inplace") else ["hn"]
            P.op("act", lambda e: e.activation(out=sq[:, :, :n], in_=xsrc, func=AF.Square), r=xkey, w=["sq"])
            if wk.get("inplace"):
                P.op("pool", lambda e: e.tensor_tensor(out=hn[:, :, :n], in0=hn[:, :, :n],
                                                       in1=Ag[:].unsqueeze(2).broadcast_to([128, 8, n]), op=ALU.mult),
                     r=hk + ["A1", "A2"], w=hk)
            for c in range(8):
                P.op("pe", lambda e, c=c: e.matmul(bank(ssb, n), onb[:], sq[:, c, :n], start=(c == 0), stop=(c == 7)),
                     r=["sq", "onb"], w=pk(ssb))
            P.op("act", lambda e: e.activation(out=rs[:, :n], in_=bank(ssb, n), func=AF.Ln, bias=wk["eps"][:, 0:1],
                                               scale=1.0 / 1024), r=pk(ssb) + ["eps"], w=["rs"])
            P.op("act", lambda e: e.activation(out=rs[:, :n], in_=rs[:, :n], func=AF.Exp, scale=-0.5), r=["rs"], w=["rs"])
            if wk.get("inplace"):
                P.op("dve", lambda e: e.tensor_tensor(out=hn[:, :, :n], in0=hn[:, :, :n],
                                                      in1=rs[:, :n].unsqueeze(1).broadcast_to([128, 8, n]), op=ALU.mult),
                     r=hk + ["rs"], w=hk)
            else:
                P.op("dve", lambda e: e.tensor_tensor(out=hn[:, :, :n], in0=xsrc,
                                                      in1=rs[:, :n].unsqueeze(1).broadcast_to([128, 8, n]), op=ALU.mult),
                     r=xkey + ["rs"], w=hk)
                P.op("pool", lambda e: e.tensor_tensor(out=hn[:, :, :n], in0=hn[:, :, :n],
                                                       in1=Ag[:].unsqueeze(2).broadcast_to([128, 8, n]), op=ALU.mult),
                     r=hk + ["A1", "A2"], w=hk)
            P.op("dve", lambda e: e.tensor_tensor(out=hT, in0=hn[:, :, :n],
                                                  in1=MOD[:, sh_off:sh_off + 8].unsqueeze(2).broadcast_to([128, 8, n]),
                                                  op=ALU.add), r=hk + ["MOD"], w=hkey)

        with ExitStack() as e1:
            w1 = SB("w1s", [128, 8, 3088], BF16, e1)
            wo1 = SB("wo1", [128, 8, 1024], BF16, e1)
            with ExitStack() as esA:
                sc = SB("sc", [128, 8], BF16, esA)
                mw = [SB("mw%d" % i, [128, 8, 1024], BF16, esA) for i in range(4)]
                dg = SB("dg", [128, 128], F32, esA)
                P.op("act", lambda e: e.activation(out=sc[:], in_=vs("cT"), func=AF.Silu), r=["V"], w=["sc"])

                def load_mw(m):
                    P.dma_group("pool", [(mw[m % 4][:, c, :], modw[:, c, m * 1024:(m + 1) * 1024]) for c in range(8)],
                                w=[("mw", m % 4)], sem="mw%d" % (m % 4))
                for m in range(4):
                    load_mw(m)
                for m in range(6):
                    s = m % 4
                    for j in range(8):
                        col = m * 8 + j
                        for c in range(8):
                            P.op("pe", lambda e, s=s, j=j, c=c, col=col: e.matmul(
                                bank(0, 1, col), mw[s][:, c, j * 128:(j + 1) * 128], sc[:, c:c + 1],
                                start=(c == 0), stop=(c == 7)), r=[("mw", s), "sc"], w=pk(0))
                    if m + 4 < 6:
                        load_mw(m + 4)
                    if m == 1:
                        P.dma_group("pool", [(w1[:, c, :], w1d[:, c, :]) for c in range(8)], w=["w1"], sem="w1")
                        P.dma("pool", wo1[:], wod[:, 0:8, :], w=["wo1"], sem="wo1")
                P.op("dve", lambda e: e.tensor_tensor(out=MOD[:], in0=bank(0, 48), in1=vs("modb"), op=ALU.add),
                     r=pk(0) + ["V"], w=["MOD"])
                P.op("dve", lambda e: e.scalar_tensor_tensor(out=A1[:], in0=MOD[:, 8:16], scalar=1.0, in1=vs("n1g"),
                                                             op0=ALU.add, op1=ALU.mult), r=["MOD", "V"], w=["A1"])
                P.op("dve", lambda e: e.scalar_tensor_tensor(out=A2[:], in0=MOD[:, 32:40], scalar=1.0, in1=vs("n2g"),
                                                             op0=ALU.add, op1=ALU.mult), r=["MOD", "V"], w=["A2"])
                for gi, (gb, o) in enumerate(((g1bc, 16),)):
                    for c in range(8):
                        P.op("dve", lambda e, c=c, o=o: e.tensor_scalar(out=dg[:], in0=idf[:], scalar1=MOD[:, o + c:o + c + 1],
                                                                        scalar2=None, op0=ALU.mult),
                             r=["MOD", "idf"], w=["dg"])
                        P.op("pe", lambda e, c=c, gi=gi: e.matmul(bank(1 + 2 * gi + c // 4, 128, (c % 4) * 128), onf[:], dg[:],
                                                                  start=True, stop=True),
                             r=["onf", "dg"], w=pk(1 + 2 * gi + c // 4))
                    P.op("dve", lambda e, gb=gb, gi=gi: e.tensor_copy(out=gb[:], in_=PS[:, (1 + 2 * gi) * 512:(3 + 2 * gi) * 512]),
                         r=pk(1 + 2 * gi, 2 + 2 * gi), w=["gbc%d" % gi])
                P.op("act", lambda e: e.activation(out=Abc[:], in_=vs("alog"), func=AF.Exp), r=["V"], w=["Abc"])
                P.op("dve", lambda e: e.tensor_scalar(out=Abc[:], in0=Abc[:], scalar1=-1.0, scalar2=None, op0=ALU.mult),
                     r=["Abc"], w=["Abc"])
                P.op("act", lambda e: e.activation(out=esink[:], in_=vs("sinks"), func=AF.Exp), r=["V"], w=["esink"])
                P.barrier()
            P.dma_group("sp", [(R[:, c, :], xo3[:, c, :]) for c in range(8)], w=[("R", c) for c in range(8)], sem="r")

            xc = [SB("xc%d" % i, [128, 8, 128], F32, e1) for i in range(2)]
            wk = dict(sq=SB("sq1", [128, 8, 128], BF16, e1), rs=SB("rs1", [128, 128], F32, e1),
                      eps=SB("eps1", [128, 1], F32, e1), inplace=True)
            hTs = [SB("hT1%d" % i, [128, 8, 128], BF16, e1) for i in range(2)]
            pre = SB("pre", [128, 16, 131], BF16, e1)
            cdiag = SB("cdiag", [128, 16, 4, 128], BF16, e1)
            cbT = SB("cbTs", [16, 128], BF16, e1)
            P.dma("pool", cbT[:], cbTd[:, :], w=["cbT"], sem="cbT")
            xbcT = SB("xbcT", [128, 16, 128], BF16, e1)
            sm = SB("sm1", [128, 16, 12], F32, e1)
            S32 = SB("S32", [128, 1024], F32, e1)
            Sb = SB("Sb", [128, 1024], BF16, e1)
            Xd = SB("Xd", [128, 16, 64], BF16, e1)
            Xdd = SB("Xdd", [128, 16, 64], BF16, e1)
            Btok = SB("Btok", [128, 4, 128], BF16, e1)
            rhsA = SB("rhsA", [128, 16, 128], F32, e1)
            Lt = SB("Lt", [128, 16, 128], BF16, e1)
            CBm = SB("CBm", [128, 4, 128], BF16, e1)
            _rf = rhsA[:].rearrange("p h l -> p (h l)")
            y1 = _rf[:, 0:1024].rearrange("p (h d) -> p h d", h=16)
            y2 = _rf[:, 1024:2048].rearrange("p (h d) -> p h d", h=16)
            szt = SB("szt", [128, 1024], BF16, e1)
            ssq = SB("ssq1", [128, 4], F32, e1)
            yn = SB("yn", [128, 1024], BF16, e1)
            ymT = SB("ymT", [128, 8, 128], BF16, e1)
            SMK = lambda i: ("sm", i)
            smv = lambda i: sm[:, :, i]

            P.op("pool", lambda e: e.memset(wk["eps"][:], EPS), w=["eps"])
            for c in range(8):
                P.op("dve", lambda e, c=c: e.scalar_tensor_tensor(out=wo1[:, c, :], in0=wo1[:, c, :], scalar=vs("wog", c),
                                                                  in1=g1bc[:], op0=ALU.mult, op1=ALU.mult),
                     r=["wo1", "V", "gbc0"], w=["wo1"])
            for tl in range(16):
                for k in range(4):
                    P.op("dve", lambda e, tl=tl, k=k: e.tensor_scalar(out=cdiag[:, tl, k, :], in0=idf[:],
                                                                     scalar1=vs("convw", tl * 4 + k), scalar2=None,
                                                                     op0=ALU.mult), r=["idf", "V"], w=["cdiag"])
            P.op("pool", lambda e: e.memset(S32[:], 0.0), w=["S32"])
            P.op("pool", lambda e: e.memset(Sb[:], 0.0), w=["Sb"])
            P.op("pool", lambda e: e.memset(pre[:], 0.0), w=["pre"])

            def LOADX(t_):
                src_ = xo3 if t_ >= 16 else xp3
                tc_ = (t_ % 16) * 128
                P.dma("sp", xc[t_ % 2][:], src_[:, :, tc_:tc_ + 128], w=[("xc", t_ % 2)], sem="xc%d" % (t_ % 2))

            def NORM(t_):
                wk["hn"] = xc[t_ % 2]
                norm_h(xc[t_ % 2][:], [("xc", t_ % 2)], hTs[t_ % 2][:], [("hT", t_ % 2)], A1, 0, wk, 128, 0)
            def INPROJ(t_):
                hT_, HK_ = hTs[t_ % 2], ("hT", t_ % 2)
                if t_ == 16:
                    P.op("pool", lambda e: e.tensor_scalar(out=pre[:, :, 0:3], in0=pre[:, :, 128:131],
                                                           scalar1=vs("flag", 0), scalar2=None, op0=ALU.mult),
                         r=["pre", "V"], w=["pre"])
                else:
                    P.op("pool", lambda e: e.tensor_copy(out=pre[:, :, 0:3], in_=pre[:, :, 128:131]),
                         r=["pre"], w=["pre"])
                for bg in range(4 if t_ >= 15 else 3):
                    b = 1 + bg % 2
                    for ti in range(4):
                        tl = bg * 4 + ti
                        for c in range(8):
                            P.op("pe", lambda e, b=b, ti=ti, tl=tl, c=c: e.matmul(
                                bank(b, 128, ti * 128), w1[:, c, tl * 128:(tl + 1) * 128], hT_[:, c, :],
                                start=(c == 0), stop=(c == 7)), r=["w1", HK_], w=pk(b))
                    if bg % 2 == 0:
                        P.op("act", lambda e, b=b, bg=bg: e.activation(
                            out=pre[:, bg * 4:(bg + 1) * 4, 3:131], in_=bank(b).rearrange("p (a n) -> p a n", a=4),
                            func=AF.Copy), r=pk(b), w=["pre"])
                    else:
                        P.op("dve", lambda e, b=b, bg=bg: e.tensor_copy(
                            out=pre[:, bg * 4:(bg + 1) * 4, 3:131], in_=bank(b).rearrange("p (a n) -> p a n", a=4)),
                            r=pk(b), w=["pre"])
            LOADX(0)
            NORM(0)
            INPROJ(0)
            for t in range(32):
                own = t >= 16
                tc = (t % 16) * 128
                par = t % 2
                src = xo3 if own else xp3
                if t + 1 < 32:
                    LOADX(t + 1)
                hT, HK = hTs[par], ("hT", par)
                for c in range(8):
                    P.op("pe", lambda e, c=c: e.matmul(bank(5, 16), hT[:, c, :], w1[:, c, 3072:3088],
                                                       start=(c == 0), stop=(c == 7)), r=[HK, "w1"], w=pk(5))
                P.op("dve", lambda e: e.tensor_tensor(out=smv(0), in0=bank(5, 16), in1=vs("dtb"), op=ALU.add),
                     r=pk(5) + ["V"], w=[SMK(0)])
                P.op("dve", lambda e: e.tensor_scalar(out=smv(1), in0=smv(0), scalar1=-1.0, scalar2=None, op0=ALU.mult),
                     r=[SMK(0)], w=[SMK(1)])
                P.op("dve", lambda e: e.tensor_tensor(out=smv(1), in0=smv(1), in1=smv(0), op=ALU.max),
                     r=[SMK(0), SMK(1)], w=[SMK(1)])
                P.op("act", lambda e: e.activation(out=smv(2), in_=smv(1), func=AF.Exp, scale=-1.0),
                     r=[SMK(1)], w=[SMK(2)])
                P.op("act", lambda e: e.activation(out=smv(3), in_=smv(2), func=AF.Ln, bias=onf[:, 0:1]),
                     r=[SMK(2), "onf"], w=[SMK(3)])
                P.op("dve", lambda e: e.scalar_tensor_tensor(out=smv(4), in0=smv(0), scalar=0.0, in1=smv(3),
                                                             op0=ALU.max, op1=ALU.add), r=[SMK(0), SMK(3)], w=[SMK(4)])
                P.op("dve", lambda e: e.tensor_tensor(out=smv(5), in0=smv(4), in1=Abc[:], op=ALU.mult),
                     r=[SMK(4), "Abc"], w=[SMK(5)])
                if t + 1 < 32:
                    NORM(t + 1)
                P.op("pe", lambda e: e.matmul(bank(5, 16, 16), U[:], smv(5), start=True, stop=True),
                     r=["U", SMK(5)], w=pk(5))
                P.op("pe", lambda e: e.matmul(bank(5, 16, 32), onf[:], smv(5), start=True, stop=True),
                     r=["onf", SMK(5)], w=pk(5))
                P.op("dve", lambda e: e.tensor_copy(out=smv(6), in_=bank(5, 16, 16)), r=pk(5), w=[SMK(6)])
                P.op("act", lambda e: e.activation(out=smv(7), in_=bank(5, 16, 16), func=AF.Exp), r=pk(5), w=[SMK(7)])
                P.op("dve", lambda e: e.tensor_tensor(out=smv(10), in0=bank(5, 16, 32), in1=smv(6), op=ALU.subtract),
                     r=pk(5) + [SMK(6)], w=[SMK(10)])
                P.op("act", lambda e: e.activation(out=smv(8), in_=smv(10), func=AF.Exp), r=[SMK(10)], w=[SMK(8)])
                P.op("act", lambda e: e.activation(out=smv(9), in_=bank(5, 16, 32), func=AF.Exp), r=pk(5), w=[SMK(9)])
                if own:
                    for hf in range(2):
                        for c in range(8):
                            P.op("pe", lambda e, hf=hf, c=c: e.matmul(bank(hf), hT[:, c, :],
                                                                      w1[:, c, 2048 + hf * 512: 2048 + (hf + 1) * 512],
                                                                      start=(c == 0), stop=(c == 7)), r=[HK, "w1"], w=pk(hf))
                    P.op("act", lambda e: e.activation(out=szt[:], in_=PS[:, 0:1024], func=AF.Silu), r=pk(0, 1), w=["szt"])
                ntl = 16 if t >= 15 else 12
                for bg in range(ntl // 4):
                    b = 3 + bg % 2
                    for ti in range(4):
                        tl = bg * 4 + ti
                        P.op("pe", lambda e, b=b, ti=ti, tl=tl: e.matmul(
                            bank(b, 128, ti * 128), cbT[:], idb[0:16, tl:tl + 1].broadcast_to([16, 128]),
                            start=True, stop=False), r=["cbT", "idb"], w=pk(b))
                        for k in range(4):
                            P.op("pe", lambda e, b=b, ti=ti, tl=tl, k=k: e.matmul(
                                bank(b, 128, ti * 128), cdiag[:, tl, k, :], pre[:, tl, k:k + 128],
                                start=False, stop=(k == 3)), r=["cdiag", "pre"], w=pk(b))
                    P.op("act", lambda e, b=b, bg=bg: e.activation(
                        out=xbcT[:, bg * 4:(bg + 1) * 4, :], in_=bank(b).rearrange("p (a n) -> p a n", a=4),
                        func=AF.Silu), r=pk(b), w=["xbcT"])
                for c in range(8):
                    P.op("pe", lambda e, c=c: e.transpose(out=bankb(6, 128 * (c + 1))[:, c * 128:(c + 1) * 128],
                                                          in_=xbcT[:, c, :], identity=idb[:]),
                         r=["xbcT", "idb"], w=pk(6))
                for g in range(4):
                    P.op("pe", lambda e, g=g: e.transpose(out=bankb(7, 128 * (g + 1))[:, g * 128:(g + 1) * 128],
                                                          in_=xbcT[:, 8 + g, :], identity=idb[:]),
                         r=["xbcT", "idb"], w=pk(7))
                P.op("dve", lambda e: e.tensor_tensor(out=Xd[:], in0=bankb(6).rearrange("p (h d) -> p h d", h=16),
                                                      in1=smv(4).unsqueeze(2).broadcast_to([128, 16, 64]), op=ALU.mult),
                     r=pk(6) + [SMK(4)], w=["Xd"])
                P.op("act", lambda e: e.activation(out=Btok[:], in_=bankb(7, 512).rearrange("p (g n) -> p g n", g=4),
                                                   func=AF.Copy), r=pk(7), w=["Btok"])
                P.op("pool", lambda e: e.tensor_tensor(out=Xdd[:], in0=Xd[:],
                                                       in1=smv(8).unsqueeze(2).broadcast_to([128, 16, 64]), op=ALU.mult),
                     r=["Xd", SMK(8)], w=["Xdd"])
                if own:
                    P.op("dve", lambda e: e.tensor_tensor(out=rhsA[:], in0=U[:].unsqueeze(1).broadcast_to([128, 16, 128]),
                                                          in1=smv(5).unsqueeze(2).broadcast_to([128, 16, 128]),
                                                          op=ALU.mult), r=["U", SMK(5)], w=["rhsA"])
                    for q4 in range(4):
                        P.op("pe", lambda e, q4=q4: e.matmul(bank(q4), SL[:], rhsA[:, q4 * 4:(q4 + 1) * 4, :],
                                                             start=True, stop=True), r=["SL", "rhsA"], w=pk(q4))
                    P.op("dve", lambda e: e.tensor_tensor(out=y2, in0=bankb(6).rearrange("p (h d) -> p h d", h=16),
                                                          in1=vs("dskip").unsqueeze(2).broadcast_to([128, 16, 64]),
                                                          op=ALU.mult), r=pk(6) + ["V"], w=["rhsA"])
                    P.op("act", lambda e: e.activation(out=Lt[:], in_=PS[:, 0:2048].rearrange("p (h l) -> p h l", h=16),
                                                       func=AF.Exp), r=pk(0, 1, 2, 3), w=["Lt"])
                    for g in range(4):
                        P.op("pe", lambda e, g=g: e.matmul(bank(7, 128, g * 128), xbcT[:, 8 + g, :], xbcT[:, 12 + g, :],
                                                           start=True, stop=True), r=["xbcT"], w=pk(7))
                    P.op("dve", lambda e: e.tensor_tensor(out=CBm[:], in0=bank(7).rearrange("p (g l) -> p g l", g=4),
                                                          in1=U[:].unsqueeze(1).broadcast_to([128, 4, 128]), op=ALU.mult),
                         r=pk(7) + ["U"], w=["CBm"])
                    P.op("dve", lambda e: e.tensor_tensor(
                        out=Lt[:].rearrange("p (g r) l -> p g r l", g=4), in0=Lt[:].rearrange("p (g r) l -> p g r l", g=4),
                        in1=CBm[:].unsqueeze(2).broadcast_to([128, 4, 4, 128]), op=ALU.mult), r=["Lt", "CBm"], w=["Lt"])
                    for h in range(16):
                        P.op("pe", lambda e, h=h: e.matmul(PS[:, 2048 + h * 64: 2048 + (h + 1) * 64], Lt[:, h, :], Xd[:, h, :],
                                                           start=True, stop=True), r=["Lt", "Xd"], w=pk(4 + h // 8))
                    for h in range(16):
                        P.op("pe", lambda e, h=h: e.matmul(PS[:, 3072 + h * 64: 3072 + (h + 1) * 64], xbcT[:, 12 + h // 4, :],
                                                           Sb[:, h * 64:(h + 1) * 64], start=True, stop=True),
                             r=["xbcT", "Sb"], w=pk(6 + h // 8))
                    if t + 1 < 32:
                        INPROJ(t + 1)
                    P.op("dve", lambda e: e.tensor_tensor(out=y1, in0=PS[:, 3072:4096].rearrange("p (h d) -> p h d", h=16),
                                                          in1=smv(7).unsqueeze(2).broadcast_to([128, 16, 64]), op=ALU.mult),
                         r=pk(6, 7) + [SMK(7)], w=["rhsA"])
                    P.op("dve", lambda e: e.tensor_tensor(out=y1, in0=PS[:, 2048:3072].rearrange("p (h d) -> p h d", h=16),
                                                          in1=y1, op=ALU.add), r=pk(4, 5) + ["rhsA"], w=["rhsA"])
                    P.op("dve", lambda e: e.tensor_tensor(out=y2, in0=y2, in1=y1, op=ALU.add),
                         r=["rhsA"], w=["rhsA"])
                    y1f = _rf[:, 0:1024]
                    P.op("dve", lambda e, y1f=y1f: e.tensor_tensor(out=y1f, in0=szt[:], in1=_rf[:, 1024:2048],
                                                                   op=ALU.mult), r=["szt", "rhsA"], w=["rhsA"])
                    P.op("pool", lambda e: e.memset(ssq[:], 0.0), w=["ssq"])
                    for g in range(4):
                        P.op("act", lambda e, g=g, y1f=y1f: e.activation(
                            out=Lt[:, 0:2, :].rearrange("p a l -> p (a l)"), in_=y1f[:, g * 256:(g + 1) * 256],
                            func=AF.Square, accum_out=ssq[:, g:g + 1]), r=["rhsA", "ssq"], w=["Lt", "ssq"])
                    P.op("act", lambda e: e.activation(out=ssq[:], in_=ssq[:], func=AF.Ln, bias=wk["eps"][:, 0:1],
                                                       scale=1.0 / 256), r=["ssq", "eps"], w=["ssq"])
                    P.op("act", lambda e: e.activation(out=ssq[:], in_=ssq[:], func=AF.Exp, scale=-0.5), r=["ssq"], w=["ssq"])
                    P.op("dve", lambda e: e.tensor_tensor(out=yn[:].rearrange("p (g f) -> p g f", g=4),
                                                          in0=y1f.rearrange("p (g f) -> p g f", g=4),
                                                          in1=ssq[:].unsqueeze(2).broadcast_to([128, 4, 256]), op=ALU.mult),
                         r=["rhsA", "ssq"], w=["yn"])
                    for c in range(8):
                        P.op("pe", lambda e, c=c: e.transpose(out=bankb(2, 128 * (c + 1))[:, c * 128:(c + 1) * 128],
                                                              in_=yn[:, c * 128:(c + 1) * 128], identity=idb[:]),
                             r=["yn", "idb"], w=pk(2))
                    P.op("act", lambda e: e.activation(out=ymT[:], in_=bankb(2).rearrange("p (c l) -> p c l", c=8),
                                                       func=AF.Copy), r=pk(2), w=["ymT"])
                    out_proj(P, bank, wo1, "wo1", ymT, "ymT", R, t - 16, (3, 4))
                if t < 31:
                    for h in range(16):
                        P.op("pe", lambda e, h=h: e.matmul(PS[:, 2560 + h * 64: 2560 + (h + 1) * 64], Btok[:, h // 4, :],
                                                           Xdd[:, h, :], start=True, stop=True),
                             r=["Btok", "Xdd"], w=pk(5 + h // 8))
                    if not own and t + 1 < 32:
                        INPROJ(t + 1)
                    P.op("pool", lambda e: e.tensor_tensor(out=S32[:].rearrange("p (h d) -> p h d", h=16),
                                                           in0=S32[:].rearrange("p (h d) -> p h d", h=16),
                                                           in1=smv(9).unsqueeze(2).broadcast_to([128, 16, 64]),
                                                           op=ALU.mult), r=["S32", SMK(9)], w=["S32"])
                    P.op("dve", lambda e: e.tensor_tensor(out=S32[:], in0=PS[:, 2560:3584], in1=S32[:], op=ALU.add),
                         r=pk(5, 6) + ["S32"], w=["S32"])
                    if t == 15:
                        P.op("dve", lambda e: e.tensor_scalar(out=S32[:], in0=S32[:], scalar1=vs("flag", 0), scalar2=None,
                                                              op0=ALU.mult), r=["S32", "V"], w=["S32"])
                    P.op("act", lambda e: e.activation(out=Sb[:], in_=S32[:], func=AF.Copy), r=["S32"], w=["Sb"])
            P.barrier()

        if stage == "ssd":
            return finish(nc, P, R, out3, es)

        with ExitStack() as eW:
            wg = [SB("wg%d" % i, [128, 8, 256], BF16, eW) for i in range(3)]
            wu = [SB("wu%d" % i, [128, 8, 256], BF16, eW) for i in range(3)]
            wd = [SB("wd%d" % i, [128, 2, 1024], BF16, eW) for i in range(3)]
            wr = SB("wrs", [128, 8, 64], F32, eW)

            def load_expert_dma(e_):
                s = e_ % 3
                P.dma_group("pool", [(wg[s][:].rearrange("p c n -> p (c n)"), egd[e_, :, :]),
                                     (wu[s][:].rearrange("p c n -> p (c n)"), eud[e_, :, :]),
                                     (wd[s][:].rearrange("p c n -> p (c n)"), edd[e_, :, :])],
                            w=[("wg", s), ("wu", s), ("wd", s)], sem="ew%d" % s)
            with ExitStack() as e2:
                w2 = SB("w2s", [128, 8, 1792], BF16, e2)
                wo2 = SB("wo2", [128, 8, 1024], BF16, e2)
                bT = SB("bT", [128, 16, 256], BF16, e2)
                xc = [SB("xd%d" % i, [128, 8, 128], F32, e2) for i in range(2)]
                wk = dict(sq=SB("sq2", [128, 8, 128], BF16, e2), rs=SB("rs2", [128, 128], F32, e2),
                          eps=SB("eps2", [128, 1], F32, e2), inplace=True)
                hT = SB("hT2", [128, 8, 128], BF16, e2)
                kT = [SB("kT%d" % i, [128, 4, 128], BF16, e2) for i in range(3)]
                qT = [SB("qT%d" % i, [128, 8, 128], BF16, e2) for i in range(2)]
                Va = [SB("Va%d" % i, [128, 4, 65], BF16, e2) for i in range(3)]
                PT = SB("PT", [128, 16, 2, 128], BF16, e2)
                den = SB("den", [128, 16], F32, e2)
                on = SB("on", [128, 16, 64], F32, e2)
                junk = SB("junk2", [128, 1024], F32, e2)
                ss1 = SB("ss1", [128, 1], F32, e2)
                ya = SB("ya", [128, 1024], BF16, e2)
                yaT = SB("yaT", [128, 8, 128], BF16, e2)

                P.op("pool", lambda e: e.memset(wk["eps"][:], EPS), w=["eps"])
                P.dma_group("pool", [(w2[:, c, :], w2d[:, c, :]) for c in range(8)], w=["w2"], sem="w2")
                P.dma("pool", wo2[:], wod[:, 8:16, :], w=["wo2"], sem="wo2")
                P.dma("pool", bT[:].rearrange("p h n -> p (h n)"), biasd[:, :], w=["bT"], sem="bT")
                load_expert_dma(0)
                load_expert_dma(1)
                P.dma("sp", wr[:], wrd[:, :, :], w=["wr"], sem="wr")
                for c in range(8):
                    P.op("dve", lambda e, c=c: e.scalar_tensor_tensor(out=wo2[:, c, :], in0=wo2[:, c, :], scalar=vs("wog", 8 + c),
                                                                      in1=g1bc[:], op0=ALU.mult, op1=ALU.mult),
                         r=["wo2", "V", "gbc0"], w=["wo2"])
                for i in range(3):
                    P.op("pool", lambda e, i=i: e.memset(Va[i][:], 1.0), w=[("Va", i)])

                def F(t):
                    own = t >= 0
                    par = t % 2
                    p3 = t % 3
                    if own:
                        P.dma("sp", xc[par][:], xo3[:, :, t * 128:(t + 1) * 128], w=[("xc", par)], sem="xc%d" % par)
                    else:
                        P.dma("sp", xc[par][:], xp3[:, :, NT - 128:NT], w=[("xc", par)], sem="xc%d" % par)
                    wk["hn"] = xc[par]
                    norm_h(xc[par][:], [("xc", par)], hT[:], ["hT"], A1, 0, wk, 128, 0)
                    for kh in range(4):
                        for c in range(8):
                            P.op("pe", lambda e, kh=kh, c=c: e.matmul(bank(1, 128, kh * 128), w2[:, c, 1024 + kh * 128:1024 + (kh + 1) * 128],
                                                                      hT[:, c, :], start=(c == 0), stop=(c == 7)),
                                 r=["w2", "hT"], w=pk(1))
                    P.op("act", lambda e, p3=p3: e.activation(out=kT[p3][:], in_=bank(1).rearrange("p (a n) -> p a n", a=4),
                                                                func=AF.Copy), r=pk(1), w=[("kT", p3)])
                    for c in range(8):
                        P.op("pe", lambda e, c=c: e.matmul(bank(0, 256, 128), hT[:, c, :], w2[:, c, 1536:1792],
                                                           start=(c == 0), stop=(c == 7)), r=["w2", "hT"], w=pk(0))
                    P.op("dve", lambda e, p3=p3: e.tensor_copy(out=Va[p3][:, :, 0:64],
                                                                in_=bank(0, 256, 128).rearrange("p (k d) -> p k d", k=4)),
                         r=pk(0), w=[("Va", p3)])
                    if not own:
                        return
                    for bg in range(2):
                        b = 2
                        for ti in range(4):
                            tl = bg * 4 + ti
                            for c in range(8):
                                P.op("pe", lambda e, b=b, ti=ti, tl=tl, c=c: e.matmul(
                                    bank(b, 128, ti * 128), w2[:, c, tl * 128:(tl + 1) * 128], hT[:, c, :],
                                    start=(c == 0), stop=(c == 7)), r=["w2", "hT"], w=pk(b))
                        P.op("act", lambda e, b=b, bg=bg, par=par: e.activation(out=qT[par][:, bg * 4:(bg + 1) * 4, :],
                                                                       in_=bank(b).rearrange("p (a n) -> p a n", a=4),
                                                                       func=AF.Copy, scale=0.125), r=pk(b), w=[("qT", par)])
                def B(t):
                    par = t % 2
                    p3, q3 = t % 3, (t - 1) % 3
                    for hp in range(8):
                        b = 5 + hp % 2
                        for hh in range(2):
                            h = 2 * hp + hh
                            kh = h // 4
                            lo, hi = hh * 64, (hh + 1) * 64
                            P.op("pe", lambda e, b=b, hh=hh, h=h: e.matmul(bank(b, 256, hh * 256), idb[:], bT[:, h, :],
                                                                           start=True, stop=False), r=["idb", "bT"], w=pk(b))
                            P.op("pe", lambda e, b=b, hh=hh, kh=kh, lo=lo, hi=hi, hp=hp: e.matmul(
                                bank(b, 128, hh * 256), kT[q3][lo:hi, kh, :], qT[par][lo:hi, hp, :], start=False, stop=False),
                                r=[("kT", q3), ("qT", par)], w=pk(b))
                            P.op("pe", lambda e, b=b, hh=hh, kh=kh, lo=lo, hi=hi, hp=hp: e.matmul(
                                bank(b, 128, hh * 256 + 128), kT[p3][lo:hi, kh, :], qT[par][lo:hi, hp, :], start=False, stop=True),
                                r=[("kT", p3), ("qT", par)], w=pk(b))
                        P.op("act", lambda e, b=b, hp=hp: e.activation(
                            out=PT[:, 2 * hp:2 * hp + 2, :, :].rearrange("p h b q -> p (h b q)"), in_=bank(b), func=AF.Exp),
                            r=pk(b), w=["PT"])
                    if t == 0:
                        P.op("pool", lambda e: e.tensor_scalar(out=PT[:, :, 0, :], in0=PT[:, :, 0, :], scalar1=vs("flag", 0),
                                                               scalar2=None, op0=ALU.mult), r=["PT", "V"], w=["PT"])
                    hb = ((0, 6, 3), (6, 12, 4), (12, 16, 7))
                    for h0, h1, b in hb:
                        for h in range(h0, h1):
                            kh = h // 4
                            o_ = (h - h0) * 65
                            P.op("pe", lambda e, b=b, o_=o_, h=h, kh=kh: e.matmul(bank(b, 65, o_), PT[:, h, 0, :], Va[q3][:, kh, :],
                                                                                  start=True, stop=False),
                                 r=["PT", ("Va", q3)], w=pk(b))
                            P.op("pe", lambda e, b=b, o_=o_, h=h, kh=kh: e.matmul(bank(b, 65, o_), PT[:, h, 1, :], Va[p3][:, kh, :],
                                                                                  start=False, stop=True),
                                 r=["PT", ("Va", p3)], w=pk(b))
                    for h0, h1, b in hb:
                        n = h1 - h0
                        pv = bank(b, n * 65).rearrange("p (h d) -> p h d", h=n)
                        P.op("dve", lambda e, pv=pv, h0=h0, h1=h1: e.tensor_tensor(out=den[:, h0:h1], in0=pv[:, :, 64],
                                                                                   in1=esink[:, h0:h1], op=ALU.add),
                             r=pk(b) + ["esink"], w=["den"])
                    P.op("dve", lambda e: e.reciprocal(out=den[:], in_=den[:]), r=["den"], w=["den"])
                    for h0, h1, b in hb:
                        n = h1 - h0
                        pv = bank(b, n * 65).rearrange("p (h d) -> p h d", h=n)
                        P.op("dve", lambda e, pv=pv, h0=h0, h1=h1, n=n: e.tensor_tensor(
                            out=on[:, h0:h1, :], in0=pv[:, :, 0:64], in1=den[:, h0:h1].unsqueeze(2).broadcast_to([128, n, 64]),
                            op=ALU.mult), r=pk(b) + ["den"], w=["on"])
                    P.op("pool", lambda e: e.memset(ss1[:], 0.0), w=["ss1"])
                    P.op("act", lambda e: e.activation(out=junk[:], in_=on[:].rearrange("p h d -> p (h d)"), func=AF.Square,
                                                       accum_out=ss1[:, 0:1]), r=["on", "ss1"], w=["junk", "ss1"])
                    P.op("act", lambda e: e.activation(out=ss1[:], in_=ss1[:], func=AF.Ln, bias=wk["eps"][:, 0:1],
                                                       scale=1.0 / 1024), r=["ss1", "eps"], w=["ss1"])
                    P.op("act", lambda e: e.activation(out=ss1[:], in_=ss1[:], func=AF.Exp, scale=-0.5), r=["ss1"], w=["ss1"])
                    P.op("dve", lambda e: e.tensor_scalar(out=ya[:], in0=on[:].rearrange("p h d -> p (h d)"), scalar1=ss1[:, 0:1],
                                                          scalar2=None, op0=ALU.mult), r=["on", "ss1"], w=["ya"])
                    for c in range(8):
                        P.op("pe", lambda e, c=c: e.transpose(out=bankb(5, 128 * (c + 1))[:, c * 128:(c + 1) * 128],
                                                              in_=ya[:, c * 128:(c + 1) * 128], identity=idb[:]),
                             r=["ya", "idb"], w=pk(5))
                    P.op("act", lambda e: e.activation(out=yaT[:], in_=bankb(5).rearrange("p (c l) -> p c l", c=8),
                                                       func=AF.Copy), r=pk(5), w=["yaT"])
                    out_proj(P, bank, wo2, "wo2", yaT, "yaT", R, t, (6, 7))
                F(-1)
                F(0)
                for t in range(16):
                    if t + 1 < 16:
                        F(t + 1)
                    B(t)
                P.barrier()

            if stage == "mixer":
                return finish(nc, P, R, out3, es)

            with ExitStack() as e3:
                h2T = SB("h2T", [128, 8, NT], BF16, e3)
                gT = SB("gT", [128, NT], BF16, e3)
                selE = SB("selE", [128, NEXP, 128], BF16, e3)
                wk = dict(sq=SB("sq3", [128, 8, 512], BF16, e3), rs=SB("rs3", [128, 512], F32, e3),
                          hn=SB("hn3", [128, 8, 512], F32, e3), eps=SB("eps3", [128, 1], F32, e3))
                rt = SB("rt", [128, 12, 64], F32, e3)
                gsb = SB("gsb", [128, 512], F32, e3)
                sg = [SB("sg%d" % i, [128, 512], BF16, e3) for i in range(2)]
                tt = [SB("tt%d" % i, [128, 512], BF16, e3) for i in range(2)]
                uu = [[SB("uu%d%d" % (i, f), [128, 512], BF16, e3) for f in range(2)] for i in range(2)]

                g2bc = SB("g2bc", [128, 1024], F32, e3)
                dg2 = SB("dg2", [128, 128], F32, e3)
                for c in range(8):
                    P.op("dve", lambda e, c=c: e.tensor_scalar(out=dg2[:], in0=idf[:], scalar1=MOD[:, 40 + c:41 + c],
                                                               scalar2=None, op0=ALU.mult), r=["MOD", "idf"], w=["dg2"])
                    P.op("pe", lambda e, c=c: e.matmul(bank(3 + c // 4, 128, (c % 4) * 128), onf[:], dg2[:], start=True, stop=True),
                         r=["onf", "dg2"], w=pk(3 + c // 4))
                P.op("dve", lambda e: e.tensor_copy(out=g2bc[:], in_=PS[:, 3 * 512:5 * 512]), r=pk(3, 4), w=["gbc1"])
                P.op("pool", lambda e: e.memset(wk["eps"][:], EPS), w=["eps"])
                P.op("pool", lambda e: e.memset(selE[:], 1.0), w=["selE"])
                P.op("pool", lambda e: e.affine_select(out=selE[:], in_=selE[:], pattern=[[1, NEXP], [0, 128]],
                                                       compare_op=ALU.is_equal, fill=0.0, base=0, channel_multiplier=-1),
                     r=["selE"], w=["selE"])
                P.op("pool", lambda e: e.memset(gT[:], 0.0), w=[("gT", g_) for g_ in range(4)])
                P.op("pool", lambda e: e.memset(gT[64:65, :], 1.0), w=[("gT", g_) for g_ in range(4)])

                def scale_expert(e_):
                    s = e_ % 3
                    P.op("pool", lambda e, s=s: e.tensor_tensor(out=wd[s][:], in0=wd[s][:],
                                                                in1=g2bc[:].unsqueeze(1).broadcast_to([128, 2, 1024]), op=ALU.mult),
                         r=[("wd", s), "gbc1"], w=[("wd", s)])

                def load_expert(e_):
                    load_expert_dma(e_)
                    scale_expert(e_)
                scale_expert(0)
                scale_expert(1)

                rv = lambda i, n=64: rt[:, i, 0:n]
                RK = lambda i: ("rt", i)
                def PRO(tg):
                    ts_ = slice(tg * 512, (tg + 1) * 512)
                    norm_h(R[:, :, ts_], [("R", c, tg) for c in range(8)], h2T[:, :, ts_], [("h2T", tg)], A2, 24, wk, 512, 5)
                    P.op("dve", lambda e: e.tensor_tensor(out=wk["hn"][:], in0=wk["hn"][:],
                                                          in1=MOD[:, 24:32].unsqueeze(2).broadcast_to([128, 8, 512]), op=ALU.add),
                         r=["hn", "MOD"], w=["hn"])
                    for tl in range(4):
                        for c in range(8):
                            P.op("pe", lambda e, tl=tl, c=c: e.matmul(bank(6, 64), wk["hn"][:, c, tl * 128:(tl + 1) * 128], wr[:, c, :],
                                                                      start=(c == 0), stop=(c == 7)), r=["hn", "wr"], w=pk(6))
                        P.op("act", lambda e: e.activation(out=rv(0), in_=bank(6, 64), func=AF.Sigmoid), r=pk(6), w=[RK(0)])
                        P.op("dve", lambda e: e.tensor_tensor(out=rv(1), in0=rv(0), in1=vs("rbias"), op=ALU.add),
                             r=[RK(0), "V"], w=[RK(1)])
                        g3 = lambda ap: ap.rearrange("p (g e) -> p g e", e=8)
                        b3 = lambda ap: ap.unsqueeze(2).broadcast_to([128, 8, 8])
                        P.op("dve", lambda e: e.tensor_reduce(out=rv(6, 8), in_=g3(rv(1)), axis=AX.X, op=ALU.max), r=[RK(1)], w=[RK(6)])
                        P.op("dve", lambda e: e.tensor_tensor(out=g3(rv(2)), in0=g3(rv(1)), in1=b3(rv(6, 8)), op=ALU.is_equal),
                             r=[RK(1), RK(6)], w=[RK(2)])
                        P.op("dve", lambda e: e.scalar_tensor_tensor(out=rv(2), in0=rv(2), scalar=-1e9, in1=rv(1), op0=ALU.mult,
                                                                     op1=ALU.add), r=[RK(2), RK(1)], w=[RK(2)])
                        P.op("dve", lambda e: e.tensor_reduce(out=rv(7, 8), in_=g3(rv(2)), axis=AX.X, op=ALU.max), r=[RK(2)], w=[RK(7)])
                        P.op("dve", lambda e: e.tensor_tensor(out=rv(7, 8), in0=rv(7, 8), in1=rv(6, 8), op=ALU.add),
                             r=[RK(7), RK(6)], w=[RK(7)])
                        P.op("dve", lambda e: e.max(out=rv(8, 8), in_=rv(7, 8)), r=[RK(7)], w=[RK(8)])
                        P.op("dve", lambda e: e.tensor_scalar(out=rv(9, 8), in0=rv(7, 8), scalar1=rt[:, 8, 3:4], scalar2=None,
                                                              op0=ALU.is_ge), r=[RK(7), RK(8)], w=[RK(9)])
                        P.op("dve", lambda e: e.scalar_tensor_tensor(out=g3(rv(3)), in0=g3(rv(1)), scalar=2.0, in1=b3(rv(9, 8)),
                                                                     op0=ALU.add, op1=ALU.mult), r=[RK(1), RK(9)], w=[RK(3)])
                        P.op("dve", lambda e: e.max(out=rv(10, 8), in_=rv(3)), r=[RK(3)], w=[RK(10)])
                        P.op("dve", lambda e: e.tensor_scalar(out=rv(4), in0=rv(3), scalar1=rt[:, 10, 7:8], scalar2=None,
                                                              op0=ALU.is_ge), r=[RK(3), RK(10)], w=[RK(4)])
                        P.op("dve", lambda e: e.tensor_tensor(out=rv(4), in0=rv(4), in1=rv(0), op=ALU.mult),
                             r=[RK(4), RK(0)], w=[RK(4)])
                        P.op("dve", lambda e: e.tensor_reduce(out=rv(11, 1), in_=rv(4), axis=AX.X, op=ALU.add), r=[RK(4)], w=[RK(11)])
                        P.op("dve", lambda e: e.reciprocal(out=rv(11, 1), in_=rv(11, 1)), r=[RK(11)], w=[RK(11)])
                        P.op("dve", lambda e: e.tensor_scalar(out=rv(5), in0=rv(4), scalar1=rt[:, 11, 0:1], scalar2=2.5,
                                                              op0=ALU.mult, op1=ALU.mult), r=[RK(4), RK(11)], w=[RK(5)])
                        P.op("pe", lambda e: e.transpose(out=bank(7, 128)[0:64, :], in_=rv(5), identity=idf[:]),
                             r=[RK(5), "idf"], w=pk(7))
                        col = tg * 512 + tl * 128
                        P.op("act", lambda e, col=col: e.activation(out=gT[0:64, col:col + 128], in_=bank(7, 128)[0:64, :],
                                                                    func=AF.Copy), r=pk(7), w=[("gT", tg)])

                PRO(0)
                if stage != "norouted":
                    experts = list(range(NEXP))
                else:
                    experts = [64]
                steps = [(e_, tg) for e_ in experts for tg in range(4)]

                def down(i):
                    e_, tg = steps[i]
                    s = e_ % 3
                    for dtl in range(8):
                        b = 5 + dtl % 3
                        for f in range(2):
                            P.op("pe", lambda e, b=b, s=s, f=f, dtl=dtl, i=i: e.matmul(
                                bank(b), wd[s][:, f, dtl * 128:(dtl + 1) * 128], uu[i % 2][f][:], start=(f == 0), stop=(f == 1)),
                                r=[("wd", s), ("uu", i % 2, f)], w=pk(b))
                        P.op("dve", lambda e, b=b, dtl=dtl, tg=tg: e.tensor_tensor(
                            out=R[:, dtl, tg * 512:(tg + 1) * 512], in0=bank(b), in1=R[:, dtl, tg * 512:(tg + 1) * 512], op=ALU.add),
                            r=pk(b) + [("R", dtl, tg)], w=[("R", dtl, tg)])

                for i, (e_, tg) in enumerate(steps):
                    s = e_ % 3
                    if stage == "norouted" and i == 0:
                        P.barrier()
                        load_expert(64)
                    P.op("pe", lambda e, e_=e_, tg=tg: e.matmul(bank(0), selE[:, e_, :], gT[:, tg * 512:(tg + 1) * 512],
                                                                start=True, stop=True), r=["selE", ("gT", tg)], w=pk(0))
                    P.op("act", lambda e: e.activation(out=gsb[:], in_=bank(0), func=AF.Copy), r=pk(0), w=["gsb"])
                    for f in range(2):
                        bG, bU = 1 + 2 * f, 2 + 2 * f
                        for c in range(8):
                            P.op("pe", lambda e, bG=bG, s=s, c=c, f=f, tg=tg: e.matmul(
                                bank(bG), wg[s][:, c, f * 128:(f + 1) * 128], h2T[:, c, tg * 512:(tg + 1) * 512],
                                start=(c == 0), stop=(c == 7)), r=[("wg", s), ("h2T", tg)], w=pk(bG))
                        for c in range(8):
                            P.op("pe", lambda e, bU=bU, s=s, c=c, f=f, tg=tg: e.matmul(
                                bank(bU), wu[s][:, c, f * 128:(f + 1) * 128], h2T[:, c, tg * 512:(tg + 1) * 512],
                                start=(c == 0), stop=(c == 7)), r=[("wu", s), ("h2T", tg)], w=pk(bU))
                        P.op("act", lambda e, bG=bG, f=f: e.activation(out=sg[f][:], in_=bank(bG), func=AF.Silu),
                             r=pk(bG), w=[("sg", f)])
                        P.op("dve", lambda e, bU=bU, f=f: e.tensor_tensor(out=tt[f][:], in0=bank(bU), in1=gsb[:], op=ALU.mult),
                             r=pk(bU) + ["gsb"], w=[("tt", f)])
                        P.op("dve", lambda e, f=f, i=i: e.tensor_tensor(out=uu[i % 2][f][:], in0=sg[f][:], in1=tt[f][:], op=ALU.mult),
                             r=[("sg", f), ("tt", f)], w=[("uu", i % 2, f)])
                    if i > 0:
                        down(i - 1)
                    if e_ == experts[0] and tg + 1 < 4:
                        PRO(tg + 1)
                    if tg == 0 and stage != "norouted" and e_ + 2 < NEXP:
                        load_expert(e_ + 2)
                down(len(steps) - 1)

                for tg in range(4):
                    ts_ = slice(tg * 512, (tg + 1) * 512)
                    sq, rs, hn = wk["sq"], wk["rs"], wk["hn"]
                    P.op("act", lambda e, ts_=ts_: e.activation(out=sq[:], in_=R[:, :, ts_], func=AF.Square),
                         r=[("R", c, tg) for c in range(8)], w=["sq"])
                    for c in range(8):
                        P.op("pe", lambda e, c=c: e.matmul(bank(0), onb[:], sq[:, c, :], start=(c == 0), stop=(c == 7)),
                             r=["sq", "onb"], w=pk(0))
                    P.op("act", lambda e: e.activation(out=rs[:], in_=bank(0), func=AF.Ln, bias=wk["eps"][:, 0:1], scale=1.0 / 1024),
                         r=pk(0) + ["eps"], w=["rs"])
                    P.op("act", lambda e: e.activation(out=rs[:], in_=rs[:], func=AF.Exp, scale=-0.5), r=["rs"], w=["rs"])
                    P.op("dve", lambda e, ts_=ts_: e.tensor_tensor(out=hn[:], in0=R[:, :, ts_],
                                                                   in1=rs[:].unsqueeze(1).broadcast_to([128, 8, 512]), op=ALU.mult),
                         r=[("R", c, tg) for c in range(8)] + ["rs"], w=["hn"])
                    P.op("pool", lambda e: e.tensor_tensor(out=hn[:], in0=hn[:], in1=vs("fg").unsqueeze(2).broadcast_to([128, 8, 512]),
                                                           op=ALU.mult), r=["hn", "V"], w=["hn"])
                    P.dma_group("sp", [(out3[:, c, ts_], hn[:, c, :]) for c in range(8)], r=["hn"], sem="out")
                P.final_wait("sp", "out")
                P.emit()
    return nc


def out_proj(P, bank, wo, wokey, yT, ykey, R, t, banks):
    for half in range(2):
        b = banks[half]
        for di in range(4):
            dtl = half * 4 + di
            for fc in range(8):
                P.op("pe", lambda e, b=b, di=di, dtl=dtl, fc=fc: e.matmul(
                    bank(b, 128, di * 128), wo[:, fc, dtl * 128:(dtl + 1) * 128], yT[:, fc, :],
                    start=(fc == 0), stop=(fc == 7)), r=[wokey, ykey], w=[("ps", b)])
        P.op("dve", lambda e, b=b, half=half: e.tensor_tensor(
            out=R[:, half * 4:(half + 1) * 4, t * 128:(t + 1) * 128], in0=bank(b).rearrange("p (a n) -> p a n", a=4),
            in1=R[:, half * 4:(half + 1) * 4, t * 128:(t + 1) * 128], op=ALU.add),
            r=[("ps", b)] + [("R", half * 4 + i) for i in range(4)], w=[("R", half * 4 + i) for i in range(4)])


def finish(nc, P, R, out3, es):
    for c in range(8):
        P.dma("sp", out3[:, c, :], R[:, c, :], r=[("R", c)], sem="out")
    P.final_wait("sp", "out")
    P.emit()
    return nc


def _fm(v, n):
    return np.ascontiguousarray(np.asarray(v, np.float32).reshape(n, 128).T)


def _wl(w):
    K, N = w.shape
    return np.ascontiguousarray(np.asarray(w, np.float32).reshape(K // 128, 128, N).transpose(1, 0, 2))


def _bucket_table():
    import math
    q = np.arange(128)[None, :]
    j = np.arange(128)[:, None]
    tabs, valid = [], []
    for blk in range(2):
        dist = q + 128 - (blk * 128 + j)
        ok = (dist >= 0) & (dist < 128)
        d = np.maximum(dist, 0)
        dd = np.maximum(d, 1).astype(np.float32)
        large = 16 + (np.log(dd / np.float32(16)) / np.float32(math.log(128 / 16)) * np.float32(16)).astype(np.int32)
        large = np.minimum(large, 31)
        tabs.append(np.where(d < 16, d, large))
        valid.append(ok)
    return np.stack(tabs, 1), np.stack(valid, 1)


def make_in_maps(inp):
    f = lambda k: np.asarray(inp[k], np.float32)
    x, c = f("x"), f("c")
    W = f("w_in")[0]
    z_, xbc_, dt_, q_, k_, v_ = (W[:, 0:1024], W[:, 1024:3072], W[:, 3072:3088], W[:, 3088:4112], W[:, 4112:4368],
                                 W[:, 4368:4624])
    w1 = _wl(np.concatenate([xbc_, z_, dt_], 1))
    kd = np.concatenate([np.concatenate([k_[:, h * 64:(h + 1) * 64]] * 2, 1) for h in range(4)], 1)
    w2 = _wl(np.concatenate([q_, kd, v_], 1))
    wo = _wl(f("w_out")[0])
    wr = _wl(f("router_w")[0])
    modw = _wl(f("mod_w")[0])
    tab, ok = _bucket_table()
    rb = f("rel_bias")
    bias = rb[tab]
    bias = np.where(ok[..., None], bias, np.float32(-30000.0)).transpose(0, 3, 1, 2)
    biasT = np.ascontiguousarray(bias.reshape(128, 16 * 256), np.float32)
    eg = np.concatenate([f("exp_w_gate")[0], f("sh_w_gate")], 0)
    eu = np.concatenate([f("exp_w_up")[0], f("sh_w_up")], 0)
    ed = np.concatenate([f("exp_w_down")[0], f("sh_w_down")], 0)
    eg = np.ascontiguousarray(eg.reshape(NEXP, 8, 128, 256).transpose(0, 2, 1, 3).reshape(NEXP, 128, 2048))
    eu = np.ascontiguousarray(eu.reshape(NEXP, 8, 128, 256).transpose(0, 2, 1, 3).reshape(NEXP, 128, 2048))
    ed = np.ascontiguousarray(ed.reshape(NEXP, 2, 128, 1024).transpose(0, 2, 1, 3).reshape(NEXP, 128, 2048))
    bc = lambda v: np.broadcast_to(np.asarray(v, np.float32).reshape(1, -1), (128, np.asarray(v).size))
    cbT = np.ascontiguousarray(f("conv_b")[0].reshape(16, 128))
    maps = []
    for core in range(8):
        b, hf = core // 2, core % 2
        vec = np.zeros((128, NV), np.float32)

        def put(n, a):
            o, w = _VOFF[n]
            vec[:, o:o + w] = a
        put("n1g", _fm(f("norm1_g")[0], 8))
        put("n2g", _fm(f("norm2_g")[0], 8))
        put("fg", _fm(f("final_g"), 8))
        put("modb", _fm(f("mod_b")[0], 48))
        put("convw", f("conv_w")[0].T.reshape(16, 128, 4).transpose(1, 0, 2).reshape(128, 64))
        put("convb", _fm(f("conv_b")[0], 16))
        put("dtb", bc(f("dt_bias")[0]))
        put("alog", bc(f("a_log")[0]))
        put("dskip", bc(f("d_skip")[0]))
        put("sinks", bc(f("sinks")[0]))
        put("rbias", bc(f("router_bias")[0]))
        put("wog", _fm(np.concatenate([f("ssm_norm_g")[0], f("att_norm_g")[0]]), 16))
        put("flag", np.full((128, 1), float(hf), np.float32))
        put("cT", _fm(c[b], 8))
        xo = np.ascontiguousarray(x[b, hf * NT:(hf + 1) * NT, :].T)
        xp = np.ascontiguousarray(x[b, 0:NT, :].T) if hf == 1 else np.zeros((1024, NT), np.float32)
        maps.append(dict(xoT=xo, xpT=xp, vec=vec, modw=modw, cbT=cbT, w1=w1, w2=w2, wo=wo, wr=wr, biasT=biasT,
                         eg=eg, eu=eu, ed=ed))
    return maps


_NC_CACHE = {}


def run(inp, stage="full"):
    if stage not in _NC_CACHE:
        _NC_CACHE[stage] = build_nc(stage)
    nc = _NC_CACHE[stage]
    maps = make_in_maps(inp)
    res = run_bass_kernel_spmd(nc, maps, core_ids=list(range(8)))
    out = np.empty((4, 4096, 1024), np.float32)
    for core in range(8):
        b, hf = core // 2, core % 2
        out[b, hf * NT:(hf + 1) * NT, :] = res.results[core]["outT"].T
    return out


def kernel(**inputs):
    return run(inputs, "full")
```

```python
import numpy as np
from contextlib import ExitStack
import concourse.bass as bass
import concourse.mybir as mybir
from concourse.bass_utils import run_bass_kernel_spmd

F32, BF16 = mybir.dt.float32, mybir.dt.bfloat16
AF = mybir.ActivationFunctionType
ALU = mybir.AluOpType
AX = mybir.AxisListType
EPS = 1e-6
NT = 2048
NCH = NT // 128
NEXP = 65
NDUMMY = 16

_VOFF = {}
_o = 0
for _n, _w in (("n1g", 8), ("n2g", 8), ("fg", 8), ("modb", 48), ("convw", 64), ("convb", 16), ("dtb", 16), ("alog", 16),
               ("dskip", 16), ("sinks", 16), ("rbias", 64), ("wog", 16), ("flag", 1), ("cT", 8)):
    _VOFF[_n] = (_o, _w)
    _o += _w
NV = _o


class _Rec:
    def __init__(self):
        self.call = None

    def __getattr__(self, name):
        def f(*a, **k):
            self.call = (name, a, k)
            return self
        return f


class Prog:
    def __init__(self, nc, es):
        self.nc, self.es = nc, es
        self.names = ("pe", "dve", "act", "pool", "sp")
        self.semh, self.cnt = {}, {}
        for k in self.names:
            self.semh[k] = es.enter_context(nc.semaphore("c_" + k))
            self.cnt[k] = 0
        self.seen = {k: {} for k in self.names}
        self.lastw, self.readers = {}, {}
        self.ops = {k: [] for k in self.names}

    def _deps(self, eng, r, w):
        deps = {}

        def add(d):
            if d is None:
                return
            s, v = d
            if s == eng and eng in ("pe", "sp"):
                return
            if deps.get(s, 0) < v:
                deps[s] = v
        for k in r:
            add(self.lastw.get(k))
        for k in w:
            add(self.lastw.get(k))
            for s, v in self.readers.get(k, {}).items():
                add((s, v))
        out = []
        for s, v in deps.items():
            if self.seen[eng].get(s, 0) < v:
                self.seen[eng][s] = v
                out.append((s, v))
        return out

    def _mark(self, me, r, w):
        for k in r:
            self.readers.setdefault(k, {})[me[0]] = me[1]
        for k in w:
            self.lastw[k] = me
            self.readers[k] = {}

    def op(self, eng, fn, r=(), w=()):
        rec = _Rec()
        fn(rec)
        name_, a_, k_ = rec.call
        fn = (lambda e, name_=name_, a_=a_, k_=k_: getattr(e, name_)(*a_, **k_))
        waits = self._deps(eng, r, w)
        self.cnt[eng] += 1
        self._mark((eng, self.cnt[eng]), r, w)
        self.ops[eng].append((waits, fn, (eng, 1)))

    def dma(self, q, out, in_, r=(), w=(), sem="d"):
        if sem not in self.semh:
            self.semh[sem] = self.es.enter_context(self.nc.semaphore("d_" + sem))
            self.cnt[sem] = 0
        waits = self._deps(q, r, w)
        self.cnt[sem] += 16
        self._mark((sem, self.cnt[sem]), r, w)
        self.ops[q].append((waits, (lambda e, o=out, i=in_: e.dma_start(out=o, in_=i)), (sem, 16)))

    def dma_group(self, q, pairs, r=(), w=(), sem="d"):
        if sem not in self.semh:
            self.semh[sem] = self.es.enter_context(self.nc.semaphore("d_" + sem))
            self.cnt[sem] = 0
        waits = self._deps(q, r, w)
        for i, (o, i_) in enumerate(pairs):
            self.cnt[sem] += 16
            self.ops[q].append((waits if i == 0 else [], (lambda e, o=o, i_=i_: e.dma_start(out=o, in_=i_)), (sem, 16)))
        self._mark((sem, self.cnt[sem]), r, w)

    def barrier(self):
        tgt = dict(self.cnt)
        for eng in self.names:
            waits = []
            for s, v in tgt.items():
                if v > 0 and self.seen[eng].get(s, 0) < v and not (s == eng):
                    self.seen[eng][s] = v
                    waits.append((s, v))
            if waits:
                self.ops[eng].append((waits, None, None))
        self.lastw, self.readers = {}, {}

    def final_wait(self, eng, sem):
        self.ops[eng].append(([(sem, self.cnt[sem])], None, None))

    def emit(self):
        block = self.es.enter_context(self.nc.Block())
        decs = dict(pe=block.tensor, dve=block.vector, act=block.scalar, pool=block.gpsimd, sp=block.sync)
        for name in self.names:
            ops = self.ops[name]

            def body(e, ops=ops):
                for waits, fn, inc in ops:
                    for ws, wv in waits:
                        e.wait_ge(self.semh[ws], wv)
                    if fn is not None:
                        fn(e).then_inc(self.semh[inc[0]], inc[1])
            decs[name](body)


def build_nc(stage="full"):
    nc = bass.Bass("TRN2", target_bir_lowering=False)
    D = lambda n, sh: nc.dram_tensor(n, sh, F32, kind="ExternalInput").ap()
    xoT = D("xoT", [1024, NT])
    xpT = D("xpT", [1024, NT])
    vec = D("vec", [128, NV])
    modw = D("modw", [128, 8, 6144])
    cbTd = D("cbT", [16, 128])
    w1d = D("w1", [128, 8, 3088])
    w2d = D("w2", [128, 8, 1792])
    wod = D("wo", [128, 16, 1024])
    wrd = D("wr", [128, 8, 64])
    biasd = D("biasT", [128, 16 * 256])
    egd = D("eg", [NEXP, 128, 8 * 256])
    eud = D("eu", [NEXP, 128, 8 * 256])
    edd = D("ed", [NEXP, 128, 2 * 1024])
    outT = nc.dram_tensor("outT", [1024, NT], F32, kind="ExternalOutput").ap()
    xo3 = xoT.rearrange("(c p) t -> p c t", p=128)
    xp3 = xpT.rearrange("(c p) t -> p c t", p=128)
    out3 = outT.rearrange("(c p) t -> p c t", p=128)

    with ExitStack() as es:
        P = Prog(nc, es)
        SB = lambda n, sh, dt, st=es: st.enter_context(nc.sbuf_tensor(n, sh, dt))
        PS = es.enter_context(nc.psum_tensor("PS", [128, 4096], F32))
        PSB = PS[:].bitcast(BF16)

        def bank(b, n=512, off=0):
            return PS[:, b * 512 + off: b * 512 + off + n]

        def bankb(b, n=1024):
            return PSB[:, b * 1024: b * 1024 + n]
        pk = lambda *bs: [("ps", b) for b in bs]

        R = SB("R", [128, 8, NT], F32)
        V = SB("V", [128, NV], F32)
        U = SB("U", [128, 128], F32)
        SL = SB("SL", [128, 128], F32)
        idf = SB("idf", [128, 128], F32)
        idb = SB("idb", [128, 128], BF16)
        onf = SB("onf", [128, 128], F32)
        onb = SB("onb", [128, 128], BF16)
        MOD = SB("MOD", [128, 48], F32)
        A1 = SB("A1", [128, 8], F32)
        A2 = SB("A2", [128, 8], F32)
        g1bc = SB("g1bc", [128, 1024], F32)
        Abc = SB("Abc", [128, 16], F32)
        esink = SB("esink", [128, 16], F32)
        vs = lambda n, i=None: (V[:, _VOFF[n][0]:_VOFF[n][0] + _VOFF[n][1]] if i is None
                                else V[:, _VOFF[n][0] + i:_VOFF[n][0] + i + 1])

        P.dma("sp", V[:], vec[:, :], w=["V"], sem="v")
        P.op("pool", lambda e: e.memset(U[:], 1.0), w=["U"])
        P.op("pool", lambda e: e.affine_select(out=U[:], in_=U[:], pattern=[[1, 128]], compare_op=ALU.is_ge,
                                               fill=0.0, base=0, channel_multiplier=-1), r=["U"], w=["U"])
        P.op("pool", lambda e: e.memset(SL[:], 1.0), w=["SL"])
        P.op("pool", lambda e: e.affine_select(out=SL[:], in_=SL[:], pattern=[[-1, 128]], compare_op=ALU.is_ge,
                                               fill=0.0, base=-1, channel_multiplier=1), r=["SL"], w=["SL"])
        P.op("pool", lambda e: e.memset(idf[:], 1.0), w=["idf"])
        P.op("pool", lambda e: e.affine_select(out=idf[:], in_=idf[:], pattern=[[1, 128]], compare_op=ALU.is_equal,
                                               fill=0.0, base=0, channel_multiplier=-1), r=["idf"], w=["idf"])
        P.op("pool", lambda e: e.tensor_copy(out=idb[:], in_=idf[:]), r=["idf"], w=["idb"])
        P.op("pool", lambda e: e.memset(onf[:], 1.0), w=["onf"])
        P.op("pool", lambda e: e.memset(onb[:], 1.0), w=["onb"])

        def norm_h(xsrc, xkey, hT, hkey, Ag, sh_off, wk, n, ssb):
            sq, rs, hn = wk["sq"], wk["rs"], wk["hn"]
            hk = list(xkey) if wk.get("inplace") else ["hn"]
            P.op("act", lambda e: e.activation(out=sq[:, :, :n], in_=xsrc, func=AF.Square), r=xkey, w=["sq"])
            if wk.get("inplace"):
                P.op("pool", lambda e: e.tensor_tensor(out=hn[:, :, :n], in0=hn[:, :, :n],
                                                       in1=Ag[:].unsqueeze(2).broadcast_to([128, 8, n]), op=ALU.mult),
                     r=hk + ["A1", "A2"], w=hk)
            for c in range(8):
                P.op("pe", lambda e, c=c: e.matmul(bank(ssb, n), onb[:], sq[:, c, :n], start=(c == 0), stop=(c == 7)),
                     r=["sq", "onb"], w=pk(ssb))
            P.op("act", lambda e: e.activation(out=rs[:, :n], in_=bank(ssb, n), func=AF.Ln, bias=wk["eps"][:, 0:1],
                                               scale=1.0 / 1024), r=pk(ssb) + ["eps"], w=["rs"])
            P.op("act", lambda e: e.activation(out=rs[:, :n], in_=rs[:, :n], func=AF.Exp, scale=-0.5), r=["rs"], w=["rs"])
            if wk.get("inplace"):
                P.op("dve", lambda e: e.tensor_tensor(out=hn[:, :, :n], in0=hn[:, :, :n],
                                                      in1=rs[:, :n].unsqueeze(1).broadcast_to([128, 8, n]), op=ALU.mult),
                     r=hk + ["rs"], w=hk)
            else:
                P.op("dve", lambda e: e.tensor_tensor(out=hn[:, :, :n], in0=xsrc,
                                                      in1=rs[:, :n].unsqueeze(1).broadcast_to([128, 8, n]), op=ALU.mult),
                     r=xkey + ["rs"], w=hk)
                P.op("pool", lambda e: e.tensor_tensor(out=hn[:, :, :n], in0=hn[:, :, :n],
                                                       in1=Ag[:].unsqueeze(2).broadcast_to([128, 8, n]), op=ALU.mult),
                     r=hk + ["A1", "A2"], w=hk)
            P.op("dve", lambda e: e.tensor_tensor(out=hT, in0=hn[:, :, :n],
                                                  in1=MOD[:, sh_off:sh_off + 8].unsqueeze(2).broadcast_to([128, 8, n]),
                                                  op=ALU.add), r=hk + ["MOD"], w=hkey)

        with ExitStack() as e1:
            w1 = SB("w1s", [128, 8, 3088], BF16, e1)
            wo1 = SB("wo1", [128, 8, 1024], BF16, e1)
            with ExitStack() as esA:
                sc = SB("sc", [128, 8], BF16, esA)
                mw = [SB("mw%d" % i, [128, 8, 1024], BF16, esA) for i in range(4)]
                dg = SB("dg", [128, 128], F32, esA)
                P.op("act", lambda e: e.activation(out=sc[:], in_=vs("cT"), func=AF.Silu), r=["V"], w=["sc"])

                def load_mw(m):
                    P.dma_group("pool", [(mw[m % 4][:, c, :], modw[:, c, m * 1024:(m + 1) * 1024]) for c in range(8)],
                                w=[("mw", m % 4)], sem="mw%d" % (m % 4))
                for m in range(4):
                    load_mw(m)
                for m in range(6):
                    s = m % 4
                    for j in range(8):
                        col = m * 8 + j
                        for c in range(8):
                            P.op("pe", lambda e, s=s, j=j, c=c, col=col: e.matmul(
                                bank(0, 1, col), mw[s][:, c, j * 128:(j + 1) * 128], sc[:, c:c + 1],
                                start=(c == 0), stop=(c == 7)), r=[("mw", s), "sc"], w=pk(0))
                    if m + 4 < 6:
                        load_mw(m + 4)
                    if m == 1:
                        P.dma_group("pool", [(w1[:, c, :], w1d[:, c, :]) for c in range(8)], w=["w1"], sem="w1")
                        P.dma("pool", wo1[:], wod[:, 0:8, :], w=["wo1"], sem="wo1")
                P.op("dve", lambda e: e.tensor_tensor(out=MOD[:], in0=bank(0, 48), in1=vs("modb"), op=ALU.add),
                     r=pk(0) + ["V"], w=["MOD"])
                P.op("dve", lambda e: e.scalar_tensor_tensor(out=A1[:], in0=MOD[:, 8:16], scalar=1.0, in1=vs("n1g"),
                                                             op0=ALU.add, op1=ALU.mult), r=["MOD", "V"], w=["A1"])
                P.op("dve", lambda e: e.scalar_tensor_tensor(out=A2[:], in0=MOD[:, 32:40], scalar=1.0, in1=vs("n2g"),
                                                             op0=ALU.add, op1=ALU.mult), r=["MOD", "V"], w=["A2"])
                for gi, (gb, o) in enumerate(((g1bc, 16),)):
                    for c in range(8):
                        P.op("dve", lambda e, c=c, o=o: e.tensor_scalar(out=dg[:], in0=idf[:], scalar1=MOD[:, o + c:o + c + 1],
                                                                        scalar2=None, op0=ALU.mult),
                             r=["MOD", "idf"], w=["dg"])
                        P.op("pe", lambda e, c=c, gi=gi: e.matmul(bank(1 + 2 * gi + c // 4, 128, (c % 4) * 128), onf[:], dg[:],
                                                                  start=True, stop=True),
                             r=["onf", "dg"], w=pk(1 + 2 * gi + c // 4))
                    P.op("dve", lambda e, gb=gb, gi=gi: e.tensor_copy(out=gb[:], in_=PS[:, (1 + 2 * gi) * 512:(3 + 2 * gi) * 512]),
                         r=pk(1 + 2 * gi, 2 + 2 * gi), w=["gbc%d" % gi])
                P.op("act", lambda e: e.activation(out=Abc[:], in_=vs("alog"), func=AF.Exp), r=["V"], w=["Abc"])
                P.op("dve", lambda e: e.tensor_scalar(out=Abc[:], in0=Abc[:], scalar1=-1.0, scalar2=None, op0=ALU.mult),
                     r=["Abc"], w=["Abc"])
                P.op("act", lambda e: e.activation(out=esink[:], in_=vs("sinks"), func=AF.Exp), r=["V"], w=["esink"])
                P.barrier()
            P.dma_group("sp", [(R[:, c, :], xo3[:, c, :]) for c in range(8)], w=[("R", c) for c in range(8)], sem="r")

            xc = [SB("xc%d" % i, [128, 8, 128], F32, e1) for i in range(2)]
            wk = dict(sq=SB("sq1", [128, 8, 128], BF16, e1), rs=SB("rs1", [128, 128], F32, e1),
                      eps=SB("eps1", [128, 1], F32, e1), inplace=True)
            hTs = [SB("hT1%d" % i, [128, 8, 128], BF16, e1) for i in range(2)]
            pre = SB("pre", [128, 16, 131], BF16, e1)
            cdiag = SB("cdiag", [128, 16, 4, 128], BF16, e1)
            cbT = SB("cbTs", [16, 128], BF16, e1)
            P.dma("pool", cbT[:], cbTd[:, :], w=["cbT"], sem="cbT")
            xbcT = SB("xbcT", [128, 16, 128], BF16, e1)
            sm = SB("sm1", [128, 16, 12], F32, e1)
            S32 = SB("S32", [128, 1024], F32, e1)
            Sb = SB("Sb", [128, 1024], BF16, e1)
            Xd = SB("Xd", [128, 16, 64], BF16, e1)
            Xdd = SB("Xdd", [128, 16, 64], BF16, e1)
            Btok = SB("Btok", [128, 4, 128], BF16, e1)
            rhsA = SB("rhsA", [128, 16, 128], F32, e1)
            Lt = SB("Lt", [128, 16, 128], BF16, e1)
            CBm = SB("CBm", [128, 4, 128], BF16, e1)
            _rf = rhsA[:].rearrange("p h l -> p (h l)")
            y1 = _rf[:, 0:1024].rearrange("p (h d) -> p h d", h=16)
            y2 = _rf[:, 1024:2048].rearrange("p (h d) -> p h d", h=16)
            szt = SB("szt", [128, 1024], BF16, e1)
            ssq = SB("ssq1", [128, 4], F32, e1)
            yn = SB("yn", [128, 1024], BF16, e1)
            ymT = SB("ymT", [128, 8, 128], BF16, e1)
            SMK = lambda i: ("sm", i)
            smv = lambda i: sm[:, :, i]

            P.op("pool", lambda e: e.memset(wk["eps"][:], EPS), w=["eps"])
            for c in range(8):
                P.op("dve", lambda e, c=c: e.scalar_tensor_tensor(out=wo1[:, c, :], in0=wo1[:, c, :], scalar=vs("wog", c),
                                                                  in1=g1bc[:], op0=ALU.mult, op1=ALU.mult),
                     r=["wo1", "V", "gbc0"], w=["wo1"])
            for tl in range(16):
                for k in range(4):
                    P.op("dve", lambda e, tl=tl, k=k: e.tensor_scalar(out=cdiag[:, tl, k, :], in0=idf[:],
                                                                     scalar1=vs("convw", tl * 4 + k), scalar2=None,
                                                                     op0=ALU.mult), r=["idf", "V"], w=["cdiag"])
            P.op("pool", lambda e: e.memset(S32[:], 0.0), w=["S32"])
            P.op("pool", lambda e: e.memset(Sb[:], 0.0), w=["Sb"])
            P.op("pool", lambda e: e.memset(pre[:], 0.0), w=["pre"])

            def LOADX(t_):
                src_ = xo3 if t_ >= 16 else xp3
                tc_ = (t_ % 16) * 128
                P.dma("sp", xc[t_ % 2][:], src_[:, :, tc_:tc_ + 128], w=[("xc", t_ % 2)], sem="xc%d" % (t_ % 2))

            def NORM(t_):
                wk["hn"] = xc[t_ % 2]
                norm_h(xc[t_ % 2][:], [("xc", t_ % 2)], hTs[t_ % 2][:], [("hT", t_ % 2)], A1, 0, wk, 128, 0)
            LOADX(0)
            NORM(0)
            for t in range(32):
                own = t >= 16
                tc = (t % 16) * 128
                par = t % 2
                src = xo3 if own else xp3
                if t + 1 < 32:
                    LOADX(t + 1)
                hT, HK = hTs[par], ("hT", par)
                for c in range(8):
                    P.op("pe", lambda e, c=c: e.matmul(bank(5, 16), hT[:, c, :], w1[:, c, 3072:3088],
                                                       start=(c == 0), stop=(c == 7)), r=[HK, "w1"], w=pk(5))
                P.op("dve", lambda e: e.tensor_tensor(out=smv(0), in0=bank(5, 16), in1=vs("dtb"), op=ALU.add),
                     r=pk(5) + ["V"], w=[SMK(0)])
                P.op("dve", lambda e: e.tensor_scalar(out=smv(1), in0=smv(0), scalar1=-1.0, scalar2=None, op0=ALU.mult),
                     r=[SMK(0)], w=[SMK(1)])
                P.op("dve", lambda e: e.tensor_tensor(out=smv(1), in0=smv(1), in1=smv(0), op=ALU.max),
                     r=[SMK(0), SMK(1)], w=[SMK(1)])
                P.op("act", lambda e: e.activation(out=smv(2), in_=smv(1), func=AF.Exp, scale=-1.0),
                     r=[SMK(1)], w=[SMK(2)])
                P.op("act", lambda e: e.activation(out=smv(3), in_=smv(2), func=AF.Ln, bias=onf[:, 0:1]),
                     r=[SMK(2), "onf"], w=[SMK(3)])
                P.op("dve", lambda e: e.scalar_tensor_tensor(out=smv(4), in0=smv(0), scalar=0.0, in1=smv(3),
                                                             op0=ALU.max, op1=ALU.add), r=[SMK(0), SMK(3)], w=[SMK(4)])
                P.op("dve", lambda e: e.tensor_tensor(out=smv(5), in0=smv(4), in1=Abc[:], op=ALU.mult),
                     r=[SMK(4), "Abc"], w=[SMK(5)])
                cur = pre
                if t == 16:
                    P.op("pool", lambda e: e.tensor_scalar(out=pre[:, :, 0:3], in0=pre[:, :, 128:131],
                                                           scalar1=vs("flag", 0), scalar2=None, op0=ALU.mult),
                         r=["pre", "V"], w=["pre"])
                else:
                    P.op("pool", lambda e: e.tensor_copy(out=pre[:, :, 0:3], in_=pre[:, :, 128:131]),
                         r=["pre"], w=["pre"])
                for bg in range(4 if t >= 15 else 3):
                    b = 1 + bg % 2
                    for ti in range(4):
                        tl = bg * 4 + ti
                        for c in range(8):
                            P.op("pe", lambda e, b=b, ti=ti, tl=tl, c=c: e.matmul(
                                bank(b, 128, ti * 128), w1[:, c, tl * 128:(tl + 1) * 128], hT[:, c, :],
                                start=(c == 0), stop=(c == 7)), r=["w1", HK], w=pk(b))
                    eng = "act" if bg % 2 == 0 else "dve"
                    if eng == "act":
                        P.op("act", lambda e, b=b, bg=bg, cur=cur: e.activation(
                            out=cur[:, bg * 4:(bg + 1) * 4, 3:131], in_=bank(b).rearrange("p (a n) -> p a n", a=4),
                            func=AF.Copy), r=pk(b), w=["pre"])
                    else:
                        P.op("dve", lambda e, b=b, bg=bg, cur=cur: e.tensor_copy(
                            out=cur[:, bg * 4:(bg + 1) * 4, 3:131], in_=bank(b).rearrange("p (a n) -> p a n", a=4)),
                            r=pk(b), w=["pre"])
                if t + 1 < 32:
                    NORM(t + 1)
                P.op("pe", lambda e: e.matmul(bank(5, 16, 16), U[:], smv(5), start=True, stop=True),
                     r=["U", SMK(5)], w=pk(5))
                P.op("pe", lambda e: e.matmul(bank(5, 16, 32), onf[:], smv(5), start=True, stop=True),
                     r=["onf", SMK(5)], w=pk(5))
                P.op("dve", lambda e: e.tensor_copy(out=smv(6), in_=bank(5, 16, 16)), r=pk(5), w=[SMK(6)])
                P.op("act", lambda e: e.activation(out=smv(7), in_=bank(5, 16, 16), func=AF.Exp), r=pk(5), w=[SMK(7)])
                P.op("dve", lambda e: e.tensor_tensor(out=smv(10), in0=bank(5, 16, 32), in1=smv(6), op=ALU.subtract),
                     r=pk(5) + [SMK(6)], w=[SMK(10)])
                P.op("act", lambda e: e.activation(out=smv(8), in_=smv(10), func=AF.Exp), r=[SMK(10)], w=[SMK(8)])
                P.op("act", lambda e: e.activation(out=smv(9), in_=bank(5, 16, 32), func=AF.Exp), r=pk(5), w=[SMK(9)])
                if own:
                    for hf in range(2):
                        for c in range(8):
                            P.op("pe", lambda e, hf=hf, c=c: e.matmul(bank(hf), hT[:, c, :],
                                                                      w1[:, c, 2048 + hf * 512: 2048 + (hf + 1) * 512],
                                                                      start=(c == 0), stop=(c == 7)), r=[HK, "w1"], w=pk(hf))
                    P.op("act", lambda e: e.activation(out=szt[:], in_=PS[:, 0:1024], func=AF.Silu), r=pk(0, 1), w=["szt"])
                ntl = 16 if t >= 15 else 12
                for bg in range(ntl // 4):
                    b = 3 + bg % 2
                    for ti in range(4):
                        tl = bg * 4 + ti
                        P.op("pe", lambda e, b=b, ti=ti, tl=tl: e.matmul(
                            bank(b, 128, ti * 128), cbT[:], idb[0:16, tl:tl + 1].broadcast_to([16, 128]),
                            start=True, stop=False), r=["cbT", "idb"], w=pk(b))
                        for k in range(4):
                            P.op("pe", lambda e, b=b, ti=ti, tl=tl, k=k: e.matmul(
                                bank(b, 128, ti * 128), cdiag[:, tl, k, :], pre[:, tl, k:k + 128],
                                start=False, stop=(k == 3)), r=["cdiag", "pre"], w=pk(b))
                    P.op("act", lambda e, b=b, bg=bg: e.activation(
                        out=xbcT[:, bg * 4:(bg + 1) * 4, :], in_=bank(b).rearrange("p (a n) -> p a n", a=4),
                        func=AF.Silu), r=pk(b), w=["xbcT"])
                for c in range(8):
                    P.op("pe", lambda e, c=c: e.transpose(out=bankb(6, 128 * (c + 1))[:, c * 128:(c + 1) * 128],
                                                          in_=xbcT[:, c, :], identity=idb[:]),
                         r=["xbcT", "idb"], w=pk(6))
                for g in range(4):
                    P.op("pe", lambda e, g=g: e.transpose(out=bankb(7, 128 * (g + 1))[:, g * 128:(g + 1) * 128],
                                                          in_=xbcT[:, 8 + g, :], identity=idb[:]),
                         r=["xbcT", "idb"], w=pk(7))
                P.op("dve", lambda e: e.tensor_tensor(out=Xd[:], in0=bankb(6).rearrange("p (h d) -> p h d", h=16),
                                                      in1=smv(4).unsqueeze(2).broadcast_to([128, 16, 64]), op=ALU.mult),
                     r=pk(6) + [SMK(4)], w=["Xd"])
                P.op("act", lambda e: e.activation(out=Btok[:], in_=bankb(7, 512).rearrange("p (g n) -> p g n", g=4),
                                                   func=AF.Copy), r=pk(7), w=["Btok"])
                P.op("pool", lambda e: e.tensor_tensor(out=Xdd[:], in0=Xd[:],
                                                       in1=smv(8).unsqueeze(2).broadcast_to([128, 16, 64]), op=ALU.mult),
                     r=["Xd", SMK(8)], w=["Xdd"])
                if own:
                    P.op("dve", lambda e: e.tensor_tensor(out=rhsA[:], in0=U[:].unsqueeze(1).broadcast_to([128, 16, 128]),
                                                          in1=smv(5).unsqueeze(2).broadcast_to([128, 16, 128]),
                                                          op=ALU.mult), r=["U", SMK(5)], w=["rhsA"])
                    for q4 in range(4):
                        P.op("pe", lambda e, q4=q4: e.matmul(bank(q4), SL[:], rhsA[:, q4 * 4:(q4 + 1) * 4, :],
                                                             start=True, stop=True), r=["SL", "rhsA"], w=pk(q4))
                    P.op("dve", lambda e: e.tensor_tensor(out=y2, in0=bankb(6).rearrange("p (h d) -> p h d", h=16),
                                                          in1=vs("dskip").unsqueeze(2).broadcast_to([128, 16, 64]),
                                                          op=ALU.mult), r=pk(6) + ["V"], w=["rhsA"])
                    P.op("act", lambda e: e.activation(out=Lt[:], in_=PS[:, 0:2048].rearrange("p (h l) -> p h l", h=16),
                                                       func=AF.Exp), r=pk(0, 1, 2, 3), w=["Lt"])
                    for g in range(4):
                        P.op("pe", lambda e, g=g: e.matmul(bank(7, 128, g * 128), xbcT[:, 8 + g, :], xbcT[:, 12 + g, :],
                                                           start=True, stop=True), r=["xbcT"], w=pk(7))
                    P.op("dve", lambda e: e.tensor_tensor(out=CBm[:], in0=bank(7).rearrange("p (g l) -> p g l", g=4),
                                                          in1=U[:].unsqueeze(1).broadcast_to([128, 4, 128]), op=ALU.mult),
                         r=pk(7) + ["U"], w=["CBm"])
                    P.op("dve", lambda e: e.tensor_tensor(
                        out=Lt[:].rearrange("p (g r) l -> p g r l", g=4), in0=Lt[:].rearrange("p (g r) l -> p g r l", g=4),
                        in1=CBm[:].unsqueeze(2).broadcast_to([128, 4, 4, 128]), op=ALU.mult), r=["Lt", "CBm"], w=["Lt"])
                    for h in range(16):
                        P.op("pe", lambda e, h=h: e.matmul(PS[:, 2048 + h * 64: 2048 + (h + 1) * 64], Lt[:, h, :], Xd[:, h, :],
                                                           start=True, stop=True), r=["Lt", "Xd"], w=pk(4 + h // 8))
                    for h in range(16):
                        P.op("pe", lambda e, h=h: e.matmul(PS[:, 3072 + h * 64: 3072 + (h + 1) * 64], xbcT[:, 12 + h // 4, :],
                                                           Sb[:, h * 64:(h + 1) * 64], start=True, stop=True),
                             r=["xbcT", "Sb"], w=pk(6 + h // 8))
                    for dmy in range(NDUMMY):
                        P.op("pe", lambda e, dmy=dmy: e.matmul(bank(dmy % 2), idb[:], w1[:, dmy % 8, 0:512], start=True, stop=True),
                             r=["idb", "w1"], w=pk(dmy % 2))
                    P.op("dve", lambda e: e.tensor_tensor(out=y1, in0=PS[:, 3072:4096].rearrange("p (h d) -> p h d", h=16),
                                                          in1=smv(7).unsqueeze(2).broadcast_to([128, 16, 64]), op=ALU.mult),
                         r=pk(6, 7) + [SMK(7)], w=["rhsA"])
                    P.op("dve", lambda e: e.tensor_tensor(out=y1, in0=PS[:, 2048:3072].rearrange("p (h d) -> p h d", h=16),
                                                          in1=y1, op=ALU.add), r=pk(4, 5) + ["rhsA"], w=["rhsA"])
                    P.op("dve", lambda e: e.tensor_tensor(out=y2, in0=y2, in1=y1, op=ALU.add),
                         r=["rhsA"], w=["rhsA"])
                    y1f = _rf[:, 0:1024]
                    P.op("dve", lambda e, y1f=y1f: e.tensor_tensor(out=y1f, in0=szt[:], in1=_rf[:, 1024:2048],
                                                                   op=ALU.mult), r=["szt", "rhsA"], w=["rhsA"])
                    P.op("pool", lambda e: e.memset(ssq[:], 0.0), w=["ssq"])
                    for g in range(4):
                        P.op("act", lambda e, g=g, y1f=y1f: e.activation(
                            out=Lt[:, 0:2, :].rearrange("p a l -> p (a l)"), in_=y1f[:, g * 256:(g + 1) * 256],
                            func=AF.Square, accum_out=ssq[:, g:g + 1]), r=["rhsA", "ssq"], w=["Lt", "ssq"])
                    P.op("act", lambda e: e.activation(out=ssq[:], in_=ssq[:], func=AF.Ln, bias=wk["eps"][:, 0:1],
                                                       scale=1.0 / 256), r=["ssq", "eps"], w=["ssq"])
                    P.op("act", lambda e: e.activation(out=ssq[:], in_=ssq[:], func=AF.Exp, scale=-0.5), r=["ssq"], w=["ssq"])
                    P.op("dve", lambda e: e.tensor_tensor(out=yn[:].rearrange("p (g f) -> p g f", g=4),
                                                          in0=y1f.rearrange("p (g f) -> p g f", g=4),
                                                          in1=ssq[:].unsqueeze(2).broadcast_to([128, 4, 256]), op=ALU.mult),
                         r=["rhsA", "ssq"], w=["yn"])
                    for c in range(8):
                        P.op("pe", lambda e, c=c: e.transpose(out=bankb(2, 128 * (c + 1))[:, c * 128:(c + 1) * 128],
                                                              in_=yn[:, c * 128:(c + 1) * 128], identity=idb[:]),
                             r=["yn", "idb"], w=pk(2))
                    P.op("act", lambda e: e.activation(out=ymT[:], in_=bankb(2).rearrange("p (c l) -> p c l", c=8),
                                                       func=AF.Copy), r=pk(2), w=["ymT"])
                    out_proj(P, bank, wo1, "wo1", ymT, "ymT", R, t - 16, (3, 4))
                if t < 31:
                    for h in range(16):
                        P.op("pe", lambda e, h=h: e.matmul(PS[:, 2560 + h * 64: 2560 + (h + 1) * 64], Btok[:, h // 4, :],
                                                           Xdd[:, h, :], start=True, stop=True),
                             r=["Btok", "Xdd"], w=pk(5 + h // 8))
                    P.op("pool", lambda e: e.tensor_tensor(out=S32[:].rearrange("p (h d) -> p h d", h=16),
                                                           in0=S32[:].rearrange("p (h d) -> p h d", h=16),
                                                           in1=smv(9).unsqueeze(2).broadcast_to([128, 16, 64]),
                                                           op=ALU.mult), r=["S32", SMK(9)], w=["S32"])
                    P.op("dve", lambda e: e.tensor_tensor(out=S32[:], in0=PS[:, 2560:3584], in1=S32[:], op=ALU.add),
                         r=pk(5, 6) + ["S32"], w=["S32"])
                    if t == 15:
                        P.op("dve", lambda e: e.tensor_scalar(out=S32[:], in0=S32[:], scalar1=vs("flag", 0), scalar2=None,
                                                              op0=ALU.mult), r=["S32", "V"], w=["S32"])
                    P.op("act", lambda e: e.activation(out=Sb[:], in_=S32[:], func=AF.Copy), r=["S32"], w=["Sb"])
            P.barrier()

        if stage == "ssd":
            return finish(nc, P, R, out3, es)

        with ExitStack() as eW:
            wg = [SB("wg%d" % i, [128, 8, 256], BF16, eW) for i in range(3)]
            wu = [SB("wu%d" % i, [128, 8, 256], BF16, eW) for i in range(3)]
            wd = [SB("wd%d" % i, [128, 2, 1024], BF16, eW) for i in range(3)]
            wr = SB("wrs", [128, 8, 64], F32, eW)

            def load_expert_dma(e_):
                s = e_ % 3
                P.dma_group("pool", [(wg[s][:].rearrange("p c n -> p (c n)"), egd[e_, :, :]),
                                     (wu[s][:].rearrange("p c n -> p (c n)"), eud[e_, :, :]),
                                     (wd[s][:].rearrange("p c n -> p (c n)"), edd[e_, :, :])],
                            w=[("wg", s), ("wu", s), ("wd", s)], sem="ew%d" % s)
            with ExitStack() as e2:
                w2 = SB("w2s", [128, 8, 1792], BF16, e2)
                wo2 = SB("wo2", [128, 8, 1024], BF16, e2)
                bT = SB("bT", [128, 16, 256], BF16, e2)
                xc = [SB("xd%d" % i, [128, 8, 128], F32, e2) for i in range(2)]
                wk = dict(sq=SB("sq2", [128, 8, 128], BF16, e2), rs=SB("rs2", [128, 128], F32, e2),
                          eps=SB("eps2", [128, 1], F32, e2), inplace=True)
                hT = SB("hT2", [128, 8, 128], BF16, e2)
                kT = [SB("kT%d" % i, [128, 4, 128], BF16, e2) for i in range(3)]
                qT = [SB("qT%d" % i, [128, 8, 128], BF16, e2) for i in range(2)]
                Va = [SB("Va%d" % i, [128, 4, 65], BF16, e2) for i in range(3)]
                PT = SB("PT", [128, 16, 2, 128], BF16, e2)
                den = SB("den", [128, 16], F32, e2)
                on = SB("on", [128, 16, 64], F32, e2)
                junk = SB("junk2", [128, 1024], F32, e2)
                ss1 = SB("ss1", [128, 1], F32, e2)
                ya = SB("ya", [128, 1024], BF16, e2)
                yaT = SB("yaT", [128, 8, 128], BF16, e2)

                P.op("pool", lambda e: e.memset(wk["eps"][:], EPS), w=["eps"])
                P.dma_group("pool", [(w2[:, c, :], w2d[:, c, :]) for c in range(8)], w=["w2"], sem="w2")
                P.dma("pool", wo2[:], wod[:, 8:16, :], w=["wo2"], sem="wo2")
                P.dma("pool", bT[:].rearrange("p h n -> p (h n)"), biasd[:, :], w=["bT"], sem="bT")
                load_expert_dma(0)
                load_expert_dma(1)
                P.dma("sp", wr[:], wrd[:, :, :], w=["wr"], sem="wr")
                for c in range(8):
                    P.op("dve", lambda e, c=c: e.scalar_tensor_tensor(out=wo2[:, c, :], in0=wo2[:, c, :], scalar=vs("wog", 8 + c),
                                                                      in1=g1bc[:], op0=ALU.mult, op1=ALU.mult),
                         r=["wo2", "V", "gbc0"], w=["wo2"])
                for i in range(3):
                    P.op("pool", lambda e, i=i: e.memset(Va[i][:], 1.0), w=[("Va", i)])

                def F(t):
                    own = t >= 0
                    par = t % 2
                    p3 = t % 3
                    if own:
                        P.dma("sp", xc[par][:], xo3[:, :, t * 128:(t + 1) * 128], w=[("xc", par)], sem="xc%d" % par)
                    else:
                        P.dma("sp", xc[par][:], xp3[:, :, NT - 128:NT], w=[("xc", par)], sem="xc%d" % par)
                    wk["hn"] = xc[par]
                    norm_h(xc[par][:], [("xc", par)], hT[:], ["hT"], A1, 0, wk, 128, 0)
                    for kh in range(4):
                        for c in range(8):
                            P.op("pe", lambda e, kh=kh, c=c: e.matmul(bank(1, 128, kh * 128), w2[:, c, 1024 + kh * 128:1024 + (kh + 1) * 128],
                                                                      hT[:, c, :], start=(c == 0), stop=(c == 7)),
                                 r=["w2", "hT"], w=pk(1))
                    P.op("act", lambda e, p3=p3: e.activation(out=kT[p3][:], in_=bank(1).rearrange("p (a n) -> p a n", a=4),
                                                                func=AF.Copy), r=pk(1), w=[("kT", p3)])
                    for c in range(8):
                        P.op("pe", lambda e, c=c: e.matmul(bank(0, 256, 128), hT[:, c, :], w2[:, c, 1536:1792],
                                                           start=(c == 0), stop=(c == 7)), r=["w2", "hT"], w=pk(0))
                    P.op("dve", lambda e, p3=p3: e.tensor_copy(out=Va[p3][:, :, 0:64],
                                                                in_=bank(0, 256, 128).rearrange("p (k d) -> p k d", k=4)),
                         r=pk(0), w=[("Va", p3)])
                    if not own:
                        return
                    for bg in range(2):
                        b = 2
                        for ti in range(4):
                            tl = bg * 4 + ti
                            for c in range(8):
                                P.op("pe", lambda e, b=b, ti=ti, tl=tl, c=c: e.matmul(
                                    bank(b, 128, ti * 128), w2[:, c, tl * 128:(tl + 1) * 128], hT[:, c, :],
                                    start=(c == 0), stop=(c == 7)), r=["w2", "hT"], w=pk(b))
                        P.op("act", lambda e, b=b, bg=bg, par=par: e.activation(out=qT[par][:, bg * 4:(bg + 1) * 4, :],
                                                                       in_=bank(b).rearrange("p (a n) -> p a n", a=4),
                                                                       func=AF.Copy, scale=0.125), r=pk(b), w=[("qT", par)])
                def B(t):
                    par = t % 2
                    p3, q3 = t % 3, (t - 1) % 3
                    for hp in range(8):
                        b = 5 + hp % 2
                        for hh in range(2):
                            h = 2 * hp + hh
                            kh = h // 4
                            lo, hi = hh * 64, (hh + 1) * 64
                            P.op("pe", lambda e, b=b, hh=hh, h=h: e.matmul(bank(b, 256, hh * 256), idb[:], bT[:, h, :],
                                                                           start=True, stop=False), r=["idb", "bT"], w=pk(b))
                            P.op("pe", lambda e, b=b, hh=hh, kh=kh, lo=lo, hi=hi, hp=hp: e.matmul(
                                bank(b, 128, hh * 256), kT[q3][lo:hi, kh, :], qT[par][lo:hi, hp, :], start=False, stop=False),
                                r=[("kT", q3), ("qT", par)], w=pk(b))
                            P.op("pe", lambda e, b=b, hh=hh, kh=kh, lo=lo, hi=hi, hp=hp: e.matmul(
                                bank(b, 128, hh * 256 + 128), kT[p3][lo:hi, kh, :], qT[par][lo:hi, hp, :], start=False, stop=True),
                                r=[("kT", p3), ("qT", par)], w=pk(b))
                        P.op("act", lambda e, b=b, hp=hp: e.activation(
                            out=PT[:, 2 * hp:2 * hp + 2, :, :].rearrange("p h b q -> p (h b q)"), in_=bank(b), func=AF.Exp),
                            r=pk(b), w=["PT"])
                    if t == 0:
                        P.op("pool", lambda e: e.tensor_scalar(out=PT[:, :, 0, :], in0=PT[:, :, 0, :], scalar1=vs("flag", 0),
                                                               scalar2=None, op0=ALU.mult), r=["PT", "V"], w=["PT"])
                    hb = ((0, 6, 3), (6, 12, 4), (12, 16, 7))
                    for h0, h1, b in hb:
                        for h in range(h0, h1):
                            kh = h // 4
                            o_ = (h - h0) * 65
                            P.op("pe", lambda e, b=b, o_=o_, h=h, kh=kh: e.matmul(bank(b, 65, o_), PT[:, h, 0, :], Va[q3][:, kh, :],
                                                                                  start=True, stop=False),
                                 r=["PT", ("Va", q3)], w=pk(b))
                            P.op("pe", lambda e, b=b, o_=o_, h=h, kh=kh: e.matmul(bank(b, 65, o_), PT[:, h, 1, :], Va[p3][:, kh, :],
                                                                                  start=False, stop=True),
                                 r=["PT", ("Va", p3)], w=pk(b))
                    for h0, h1, b in hb:
                        n = h1 - h0
                        pv = bank(b, n * 65).rearrange("p (h d) -> p h d", h=n)
                        P.op("dve", lambda e, pv=pv, h0=h0, h1=h1: e.tensor_tensor(out=den[:, h0:h1], in0=pv[:, :, 64],
                                                                                   in1=esink[:, h0:h1], op=ALU.add),
                             r=pk(b) + ["esink"], w=["den"])
                    P.op("dve", lambda e: e.reciprocal(out=den[:], in_=den[:]), r=["den"], w=["den"])
                    for h0, h1, b in hb:
                        n = h1 - h0
                        pv = bank(b, n * 65).rearrange("p (h d) -> p h d", h=n)
                        P.op("dve", lambda e, pv=pv, h0=h0, h1=h1, n=n: e.tensor_tensor(
                            out=on[:, h0:h1, :], in0=pv[:, :, 0:64], in1=den[:, h0:h1].unsqueeze(2).broadcast_to([128, n, 64]),
                            op=ALU.mult), r=pk(b) + ["den"], w=["on"])
                    P.op("pool", lambda e: e.memset(ss1[:], 0.0), w=["ss1"])
                    P.op("act", lambda e: e.activation(out=junk[:], in_=on[:].rearrange("p h d -> p (h d)"), func=AF.Square,
                                                       accum_out=ss1[:, 0:1]), r=["on", "ss1"], w=["junk", "ss1"])
                    P.op("act", lambda e: e.activation(out=ss1[:], in_=ss1[:], func=AF.Ln, bias=wk["eps"][:, 0:1],
                                                       scale=1.0 / 1024), r=["ss1", "eps"], w=["ss1"])
                    P.op("act", lambda e: e.activation(out=ss1[:], in_=ss1[:], func=AF.Exp, scale=-0.5), r=["ss1"], w=["ss1"])
                    P.op("dve", lambda e: e.tensor_scalar(out=ya[:], in0=on[:].rearrange("p h d -> p (h d)"), scalar1=ss1[:, 0:1],
                                                          scalar2=None, op0=ALU.mult), r=["on", "ss1"], w=["ya"])
                    for c in range(8):
                        P.op("pe", lambda e, c=c: e.transpose(out=bankb(5, 128 * (c + 1))[:, c * 128:(c + 1) * 128],
                                                              in_=ya[:, c * 128:(c + 1) * 128], identity=idb[:]),
                             r=["ya", "idb"], w=pk(5))
                    P.op("act", lambda e: e.activation(out=yaT[:], in_=bankb(5).rearrange("p (c l) -> p c l", c=8),
                                                       func=AF.Copy), r=pk(5), w=["yaT"])
                    out_proj(P, bank, wo2, "wo2", yaT, "yaT", R, t, (6, 7))
                F(-1)
                F(0)
                for t in range(16):
                    if t + 1 < 16:
                        F(t + 1)
                    B(t)
                P.barrier()

            if stage == "mixer":
                return finish(nc, P, R, out3, es)

            with ExitStack() as e3:
                h2T = SB("h2T", [128, 8, NT], BF16, e3)
                gT = SB("gT", [128, NT], BF16, e3)
                selE = SB("selE", [128, NEXP, 128], BF16, e3)
                wk = dict(sq=SB("sq3", [128, 8, 512], BF16, e3), rs=SB("rs3", [128, 512], F32, e3),
                          hn=SB("hn3", [128, 8, 512], F32, e3), eps=SB("eps3", [128, 1], F32, e3))
                rt = SB("rt", [128, 12, 64], F32, e3)
                gsb = SB("gsb", [128, 512], F32, e3)
                sg = [SB("sg%d" % i, [128, 512], BF16, e3) for i in range(2)]
                tt = [SB("tt%d" % i, [128, 512], BF16, e3) for i in range(2)]
                uu = [[SB("uu%d%d" % (i, f), [128, 512], BF16, e3) for f in range(2)] for i in range(2)]

                g2bc = SB("g2bc", [128, 1024], F32, e3)
                dg2 = SB("dg2", [128, 128], F32, e3)
                for c in range(8):
                    P.op("dve", lambda e, c=c: e.tensor_scalar(out=dg2[:], in0=idf[:], scalar1=MOD[:, 40 + c:41 + c],
                                                               scalar2=None, op0=ALU.mult), r=["MOD", "idf"], w=["dg2"])
                    P.op("pe", lambda e, c=c: e.matmul(bank(3 + c // 4, 128, (c % 4) * 128), onf[:], dg2[:], start=True, stop=True),
                         r=["onf", "dg2"], w=pk(3 + c // 4))
                P.op("dve", lambda e: e.tensor_copy(out=g2bc[:], in_=PS[:, 3 * 512:5 * 512]), r=pk(3, 4), w=["gbc1"])
                P.op("pool", lambda e: e.memset(wk["eps"][:], EPS), w=["eps"])
                P.op("pool", lambda e: e.memset(selE[:], 1.0), w=["selE"])
                P.op("pool", lambda e: e.affine_select(out=selE[:], in_=selE[:], pattern=[[1, NEXP], [0, 128]],
                                                       compare_op=ALU.is_equal, fill=0.0, base=0, channel_multiplier=-1),
                     r=["selE"], w=["selE"])
                P.op("pool", lambda e: e.memset(gT[:], 0.0), w=[("gT", g_) for g_ in range(4)])
                P.op("pool", lambda e: e.memset(gT[64:65, :], 1.0), w=[("gT", g_) for g_ in range(4)])

                def scale_expert(e_):
                    s = e_ % 3
                    P.op("pool", lambda e, s=s: e.tensor_tensor(out=wd[s][:], in0=wd[s][:],
                                                                in1=g2bc[:].unsqueeze(1).broadcast_to([128, 2, 1024]), op=ALU.mult),
                         r=[("wd", s), "gbc1"], w=[("wd", s)])

                def load_expert(e_):
                    load_expert_dma(e_)
                    scale_expert(e_)
                scale_expert(0)
                scale_expert(1)

                rv = lambda i, n=64: rt[:, i, 0:n]
                RK = lambda i: ("rt", i)
                def PRO(tg):
                    ts_ = slice(tg * 512, (tg + 1) * 512)
                    norm_h(R[:, :, ts_], [("R", c, tg) for c in range(8)], h2T[:, :, ts_], [("h2T", tg)], A2, 24, wk, 512, 5)
                    P.op("dve", lambda e: e.tensor_tensor(out=wk["hn"][:], in0=wk["hn"][:],
                                                          in1=MOD[:, 24:32].unsqueeze(2).broadcast_to([128, 8, 512]), op=ALU.add),
                         r=["hn", "MOD"], w=["hn"])
                    for tl in range(4):
                        for c in range(8):
                            P.op("pe", lambda e, tl=tl, c=c: e.matmul(bank(6, 64), wk["hn"][:, c, tl * 128:(tl + 1) * 128], wr[:, c, :],
                                                                      start=(c == 0), stop=(c == 7)), r=["hn", "wr"], w=pk(6))
                        P.op("act", lambda e: e.activation(out=rv(0), in_=bank(6, 64), func=AF.Sigmoid), r=pk(6), w=[RK(0)])
                        P.op("dve", lambda e: e.tensor_tensor(out=rv(1), in0=rv(0), in1=vs("rbias"), op=ALU.add),
                             r=[RK(0), "V"], w=[RK(1)])
                        g3 = lambda ap: ap.rearrange("p (g e) -> p g e", e=8)
                        b3 = lambda ap: ap.unsqueeze(2).broadcast_to([128, 8, 8])
                        P.op("dve", lambda e: e.tensor_reduce(out=rv(6, 8), in_=g3(rv(1)), axis=AX.X, op=ALU.max), r=[RK(1)], w=[RK(6)])
                        P.op("dve", lambda e: e.tensor_tensor(out=g3(rv(2)), in0=g3(rv(1)), in1=b3(rv(6, 8)), op=ALU.is_equal),
                             r=[RK(1), RK(6)], w=[RK(2)])
                        P.op("dve", lambda e: e.scalar_tensor_tensor(out=rv(2), in0=rv(2), scalar=-1e9, in1=rv(1), op0=ALU.mult,
                                                                     op1=ALU.add), r=[RK(2), RK(1)], w=[RK(2)])
                        P.op("dve", lambda e: e.tensor_reduce(out=rv(7, 8), in_=g3(rv(2)), axis=AX.X, op=ALU.max), r=[RK(2)], w=[RK(7)])
                        P.op("dve", lambda e: e.tensor_tensor(out=rv(7, 8), in0=rv(7, 8), in1=rv(6, 8), op=ALU.add),
                             r=[RK(7), RK(6)], w=[RK(7)])
                        P.op("dve", lambda e: e.max(out=rv(8, 8), in_=rv(7, 8)), r=[RK(7)], w=[RK(8)])
                        P.op("dve", lambda e: e.tensor_scalar(out=rv(9, 8), in0=rv(7, 8), scalar1=rt[:, 8, 3:4], scalar2=None,
                                                              op0=ALU.is_ge), r=[RK(7), RK(8)], w=[RK(9)])
                        P.op("dve", lambda e: e.scalar_tensor_tensor(out=g3(rv(3)), in0=g3(rv(1)), scalar=2.0, in1=b3(rv(9, 8)),
                                                                     op0=ALU.add, op1=ALU.mult), r=[RK(1), RK(9)], w=[RK(3)])
                        P.op("dve", lambda e: e.max(out=rv(10, 8), in_=rv(3)), r=[RK(3)], w=[RK(10)])
                        P.op("dve", lambda e: e.tensor_scalar(out=rv(4), in0=rv(3), scalar1=rt[:, 10, 7:8], scalar2=None,
                                                              op0=ALU.is_ge), r=[RK(3), RK(10)], w=[RK(4)])
                        P.op("dve", lambda e: e.tensor_tensor(out=rv(4), in0=rv(4), in1=rv(0), op=ALU.mult),
                             r=[RK(4), RK(0)], w=[RK(4)])
                        P.op("dve", lambda e: e.tensor_reduce(out=rv(11, 1), in_=rv(4), axis=AX.X, op=ALU.add), r=[RK(4)], w=[RK(11)])
                        P.op("dve", lambda e: e.reciprocal(out=rv(11, 1), in_=rv(11, 1)), r=[RK(11)], w=[RK(11)])
                        P.op("dve", lambda e: e.tensor_scalar(out=rv(5), in0=rv(4), scalar1=rt[:, 11, 0:1], scalar2=2.5,
                                                              op0=ALU.mult, op1=ALU.mult), r=[RK(4), RK(11)], w=[RK(5)])
                        P.op("pe", lambda e: e.transpose(out=bank(7, 128)[0:64, :], in_=rv(5), identity=idf[:]),
                             r=[RK(5), "idf"], w=pk(7))
                        col = tg * 512 + tl * 128
                        P.op("act", lambda e, col=col: e.activation(out=gT[0:64, col:col + 128], in_=bank(7, 128)[0:64, :],
                                                                    func=AF.Copy), r=pk(7), w=[("gT", tg)])

                PRO(0)
                if stage != "norouted":
                    experts = list(range(NEXP))
                else:
                    experts = [64]
                steps = [(e_, tg) for e_ in experts for tg in range(4)]

                def down(i):
                    e_, tg = steps[i]
                    s = e_ % 3
                    for dtl in range(8):
                        b = 5 + dtl % 3
                        for f in range(2):
                            P.op("pe", lambda e, b=b, s=s, f=f, dtl=dtl, i=i: e.matmul(
                                bank(b), wd[s][:, f, dtl * 128:(dtl + 1) * 128], uu[i % 2][f][:], start=(f == 0), stop=(f == 1)),
                                r=[("wd", s), ("uu", i % 2, f)], w=pk(b))
                        P.op("dve", lambda e, b=b, dtl=dtl, tg=tg: e.tensor_tensor(
                            out=R[:, dtl, tg * 512:(tg + 1) * 512], in0=bank(b), in1=R[:, dtl, tg * 512:(tg + 1) * 512], op=ALU.add),
                            r=pk(b) + [("R", dtl, tg)], w=[("R", dtl, tg)])

                for i, (e_, tg) in enumerate(steps):
                    s = e_ % 3
                    if stage == "norouted" and i == 0:
                        P.barrier()
                        load_expert(64)
                    P.op("pe", lambda e, e_=e_, tg=tg: e.matmul(bank(0), selE[:, e_, :], gT[:, tg * 512:(tg + 1) * 512],
                                                                start=True, stop=True), r=["selE", ("gT", tg)], w=pk(0))
                    P.op("act", lambda e: e.activation(out=gsb[:], in_=bank(0), func=AF.Copy), r=pk(0), w=["gsb"])
                    for f in range(2):
                        bG, bU = 1 + 2 * f, 2 + 2 * f
                        for c in range(8):
                            P.op("pe", lambda e, bG=bG, s=s, c=c, f=f, tg=tg: e.matmul(
                                bank(bG), wg[s][:, c, f * 128:(f + 1) * 128], h2T[:, c, tg * 512:(tg + 1) * 512],
                                start=(c == 0), stop=(c == 7)), r=[("wg", s), ("h2T", tg)], w=pk(bG))
                        for c in range(8):
                            P.op("pe", lambda e, bU=bU, s=s, c=c, f=f, tg=tg: e.matmul(
                                bank(bU), wu[s][:, c, f * 128:(f + 1) * 128], h2T[:, c, tg * 512:(tg + 1) * 512],
                                start=(c == 0), stop=(c == 7)), r=[("wu", s), ("h2T", tg)], w=pk(bU))
                        P.op("act", lambda e, bG=bG, f=f: e.activation(out=sg[f][:], in_=bank(bG), func=AF.Silu),
                             r=pk(bG), w=[("sg", f)])
                        P.op("dve", lambda e, bU=bU, f=f: e.tensor_tensor(out=tt[f][:], in0=bank(bU), in1=gsb[:], op=ALU.mult),
                             r=pk(bU) + ["gsb"], w=[("tt", f)])
                        P.op("dve", lambda e, f=f, i=i: e.tensor_tensor(out=uu[i % 2][f][:], in0=sg[f][:], in1=tt[f][:], op=ALU.mult),
                             r=[("sg", f), ("tt", f)], w=[("uu", i % 2, f)])
                    if i > 0:
                        down(i - 1)
                    if e_ == experts[0] and tg + 1 < 4:
                        PRO(tg + 1)
                    if tg == 0 and stage != "norouted" and e_ + 2 < NEXP:
                        load_expert(e_ + 2)
                down(len(steps) - 1)

                for tg in range(4):
                    ts_ = slice(tg * 512, (tg + 1) * 512)
                    sq, rs, hn = wk["sq"], wk["rs"], wk["hn"]
                    P.op("act", lambda e, ts_=ts_: e.activation(out=sq[:], in_=R[:, :, ts_], func=AF.Square),
                         r=[("R", c, tg) for c in range(8)], w=["sq"])
                    for c in range(8):
                        P.op("pe", lambda e, c=c: e.matmul(bank(0), onb[:], sq[:, c, :], start=(c == 0), stop=(c == 7)),
                             r=["sq", "onb"], w=pk(0))
                    P.op("act", lambda e: e.activation(out=rs[:], in_=bank(0), func=AF.Ln, bias=wk["eps"][:, 0:1], scale=1.0 / 1024),
                         r=pk(0) + ["eps"], w=["rs"])
                    P.op("act", lambda e: e.activation(out=rs[:], in_=rs[:], func=AF.Exp, scale=-0.5), r=["rs"], w=["rs"])
                    P.op("dve", lambda e, ts_=ts_: e.tensor_tensor(out=hn[:], in0=R[:, :, ts_],
                                                                   in1=rs[:].unsqueeze(1).broadcast_to([128, 8, 512]), op=ALU.mult),
                         r=[("R", c, tg) for c in range(8)] + ["rs"], w=["hn"])
                    P.op("pool", lambda e: e.tensor_tensor(out=hn[:], in0=hn[:], in1=vs("fg").unsqueeze(2).broadcast_to([128, 8, 512]),
                                                           op=ALU.mult), r=["hn", "V"], w=["hn"])
                    P.dma_group("sp", [(out3[:, c, ts_], hn[:, c, :]) for c in range(8)], r=["hn"], sem="out")
                P.final_wait("sp", "out")
                P.emit()
    return nc


def out_proj(P, bank, wo, wokey, yT, ykey, R, t, banks):
    for half in range(2):
        b = banks[half]
        for di in range(4):
            dtl = half * 4 + di
            for fc in range(8):
                P.op("pe", lambda e, b=b, di=di, dtl=dtl, fc=fc: e.matmul(
                    bank(b, 128, di * 128), wo[:, fc, dtl * 128:(dtl + 1) * 128], yT[:, fc, :],
                    start=(fc == 0), stop=(fc == 7)), r=[wokey, ykey], w=[("ps", b)])
        P.op("dve", lambda e, b=b, half=half: e.tensor_tensor(
            out=R[:, half * 4:(half + 1) * 4, t * 128:(t + 1) * 128], in0=bank(b).rearrange("p (a n) -> p a n", a=4),
            in1=R[:, half * 4:(half + 1) * 4, t * 128:(t + 1) * 128], op=ALU.add),
            r=[("ps", b)] + [("R", half * 4 + i) for i in range(4)], w=[("R", half * 4 + i) for i in range(4)])


def finish(nc, P, R, out3, es):
    for c in range(8):
        P.dma("sp", out3[:, c, :], R[:, c, :], r=[("R", c)], sem="out")
    P.final_wait("sp", "out")
    P.emit()
    return nc


def _fm(v, n):
    return np.ascontiguousarray(np.asarray(v, np.float32).reshape(n, 128).T)


def _wl(w):
    K, N = w.shape
    return np.ascontiguousarray(np.asarray(w, np.float32).reshape(K // 128, 128, N).transpose(1, 0, 2))


def _bucket_table():
    import math
    q = np.arange(128)[None, :]
    j = np.arange(128)[:, None]
    tabs, valid = [], []
    for blk in range(2):
        dist = q + 128 - (blk * 128 + j)
        ok = (dist >= 0) & (dist < 128)
        d = np.maximum(dist, 0)
        dd = np.maximum(d, 1).astype(np.float32)
        large = 16 + (np.log(dd / np.float32(16)) / np.float32(math.log(128 / 16)) * np.float32(16)).astype(np.int32)
        large = np.minimum(large, 31)
        tabs.append(np.where(d < 16, d, large))
        valid.append(ok)
    return np.stack(tabs, 1), np.stack(valid, 1)


def make_in_maps(inp):
    f = lambda k: np.asarray(inp[k], np.float32)
    x, c = f("x"), f("c")
    W = f("w_in")[0]
    z_, xbc_, dt_, q_, k_, v_ = (W[:, 0:1024], W[:, 1024:3072], W[:, 3072:3088], W[:, 3088:4112], W[:, 4112:4368],
                                 W[:, 4368:4624])
    w1 = _wl(np.concatenate([xbc_, z_, dt_], 1))
    kd = np.concatenate([np.concatenate([k_[:, h * 64:(h + 1) * 64]] * 2, 1) for h in range(4)], 1)
    w2 = _wl(np.concatenate([q_, kd, v_], 1))
    wo = _wl(f("w_out")[0])
    wr = _wl(f("router_w")[0])
    modw = _wl(f("mod_w")[0])
    tab, ok = _bucket_table()
    rb = f("rel_bias")
    bias = rb[tab]
    bias = np.where(ok[..., None], bias, np.float32(-30000.0)).transpose(0, 3, 1, 2)
    biasT = np.ascontiguousarray(bias.reshape(128, 16 * 256), np.float32)
    eg = np.concatenate([f("exp_w_gate")[0], f("sh_w_gate")], 0)
    eu = np.concatenate([f("exp_w_up")[0], f("sh_w_up")], 0)
    ed = np.concatenate([f("exp_w_down")[0], f("sh_w_down")], 0)
    eg = np.ascontiguousarray(eg.reshape(NEXP, 8, 128, 256).transpose(0, 2, 1, 3).reshape(NEXP, 128, 2048))
    eu = np.ascontiguousarray(eu.reshape(NEXP, 8, 128, 256).transpose(0, 2, 1, 3).reshape(NEXP, 128, 2048))
    ed = np.ascontiguousarray(ed.reshape(NEXP, 2, 128, 1024).transpose(0, 2, 1, 3).reshape(NEXP, 128, 2048))
    bc = lambda v: np.broadcast_to(np.asarray(v, np.float32).reshape(1, -1), (128, np.asarray(v).size))
    cbT = np.ascontiguousarray(f("conv_b")[0].reshape(16, 128))
    maps = []
    for core in range(8):
        b, hf = core // 2, core % 2
        vec = np.zeros((128, NV), np.float32)

        def put(n, a):
            o, w = _VOFF[n]
            vec[:, o:o + w] = a
        put("n1g", _fm(f("norm1_g")[0], 8))
        put("n2g", _fm(f("norm2_g")[0], 8))
        put("fg", _fm(f("final_g"), 8))
        put("modb", _fm(f("mod_b")[0], 48))
        put("convw", f("conv_w")[0].T.reshape(16, 128, 4).transpose(1, 0, 2).reshape(128, 64))
        put("convb", _fm(f("conv_b")[0], 16))
        put("dtb", bc(f("dt_bias")[0]))
        put("alog", bc(f("a_log")[0]))
        put("dskip", bc(f("d_skip")[0]))
        put("sinks", bc(f("sinks")[0]))
        put("rbias", bc(f("router_bias")[0]))
        put("wog", _fm(np.concatenate([f("ssm_norm_g")[0], f("att_norm_g")[0]]), 16))
        put("flag", np.full((128, 1), float(hf), np.float32))
        put("cT", _fm(c[b], 8))
        xo = np.ascontiguousarray(x[b, hf * NT:(hf + 1) * NT, :].T)
        xp = np.ascontiguousarray(x[b, 0:NT, :].T) if hf == 1 else np.zeros((1024, NT), np.float32)
        maps.append(dict(xoT=xo, xpT=xp, vec=vec, modw=modw, cbT=cbT, w1=w1, w2=w2, wo=wo, wr=wr, biasT=biasT,
                         eg=eg, eu=eu, ed=ed))
    return maps


_NC_CACHE = {}


def run(inp, stage="full"):
    if stage not in _NC_CACHE:
        _NC_CACHE[stage] = build_nc(stage)
    nc = _NC_CACHE[stage]
    maps = make_in_maps(inp)
    res = run_bass_kernel_spmd(nc, maps, core_ids=list(range(8)))
    out = np.empty((4, 4096, 1024), np.float32)
    for core in range(8):
        b, hf = core // 2, core % 2
        out[b, hf * NT:(hf + 1) * NT, :] = res.results[core]["outT"].T
    return out


def kernel(**inputs):
    return run(inputs, "full")
```

```python
import numpy as np
from contextlib import ExitStack
import concourse.bass as bass
import concourse.mybir as mybir
from concourse.bass_utils import run_bass_kernel_spmd

F32, BF16 = mybir.dt.float32, mybir.dt.bfloat16
AF = mybir.ActivationFunctionType
ALU = mybir.AluOpType
AX = mybir.AxisListType
EPS = 1e-6
NT = 2048
NCH = NT // 128
NEXP = 65
NDUMMY = 16

_VOFF = {}
_o = 0
for _n, _w in (("n1g", 8), ("n2g", 8), ("fg", 8), ("modb", 48), ("convw", 64), ("convb", 16), ("dtb", 16), ("alog", 16),
               ("dskip", 16), ("sinks", 16), ("rbias", 64), ("wog", 16), ("flag", 1), ("cT", 8)):
    _VOFF[_n] = (_o, _w)
    _o += _w
NV = _o


class _Rec:
    def __init__(self):
        self.call = None

    def __getattr__(self, name):
        def f(*a, **k):
            self.call = (name, a, k)
            return self
        return f


class Prog:
    def __init__(self, nc, es):
        self.nc, self.es = nc, es
        self.names = ("pe", "dve", "act", "pool", "sp")
        self.semh, self.cnt = {}, {}
        for k in self.names:
            self.semh[k] = es.enter_context(nc.semaphore("c_" + k))
            self.cnt[k] = 0
        self.seen = {k: {} for k in self.names}
        self.lastw, self.readers = {}, {}
        self.ops = {k: [] for k in self.names}

    def _deps(self, eng, r, w):
        deps = {}

        def add(d):
            if d is None:
                return
            s, v = d
            if s == eng and eng in ("pe", "sp"):
                return
            if deps.get(s, 0) < v:
                deps[s] = v
        for k in r:
            add(self.lastw.get(k))
        for k in w:
            add(self.lastw.get(k))
            for s, v in self.readers.get(k, {}).items():
                add((s, v))
        out = []
        for s, v in deps.items():
            if self.seen[eng].get(s, 0) < v:
                self.seen[eng][s] = v
                out.append((s, v))
        return out

    def _mark(self, me, r, w):
        for k in r:
            self.readers.setdefault(k, {})[me[0]] = me[1]
        for k in w:
            self.lastw[k] = me
            self.readers[k] = {}

    def op(self, eng, fn, r=(), w=()):
        rec = _Rec()
        fn(rec)
        name_, a_, k_ = rec.call
        fn = (lambda e, name_=name_, a_=a_, k_=k_: getattr(e, name_)(*a_, **k_))
        waits = self._deps(eng, r, w)
        self.cnt[eng] += 1
        self._mark((eng, self.cnt[eng]), r, w)
        self.ops[eng].append((waits, fn, (eng, 1)))

    def dma(self, q, out, in_, r=(), w=(), sem="d"):
        if sem not in self.semh:
            self.semh[sem] = self.es.enter_context(self.nc.semaphore("d_" + sem))
            self.cnt[sem] = 0
        waits = self._deps(q, r, w)
        self.cnt[sem] += 16
        self._mark((sem, self.cnt[sem]), r, w)
        self.ops[q].append((waits, (lambda e, o=out, i=in_: e.dma_start(out=o, in_=i)), (sem, 16)))

    def dma_group(self, q, pairs, r=(), w=(), sem="d"):
        if sem not in self.semh:
            self.semh[sem] = self.es.enter_context(self.nc.semaphore("d_" + sem))
            self.cnt[sem] = 0
        waits = self._deps(q, r, w)
        for i, (o, i_) in enumerate(pairs):
            self.cnt[sem] += 16
            self.ops[q].append((waits if i == 0 else [], (lambda e, o=o, i_=i_: e.dma_start(out=o, in_=i_)), (sem, 16)))
        self._mark((sem, self.cnt[sem]), r, w)

    def barrier(self):
        tgt = dict(self.cnt)
        for eng in self.names:
            waits = []
            for s, v in tgt.items():
                if v > 0 and self.seen[eng].get(s, 0) < v and not (s == eng and eng in ("pe", "sp")):
                    self.seen[eng][s] = v
                    waits.append((s, v))
            if waits:
                self.ops[eng].append((waits, None, None))
        self.lastw, self.readers = {}, {}

    def final_wait(self, eng, sem):
        self.ops[eng].append(([(sem, self.cnt[sem])], None, None))

    def emit(self):
        block = self.es.enter_context(self.nc.Block())
        decs = dict(pe=block.tensor, dve=block.vector, act=block.scalar, pool=block.gpsimd, sp=block.sync)
        for name in self.names:
            ops = self.ops[name]

            def body(e, ops=ops):
                for waits, fn, inc in ops:
                    for ws, wv in waits:
                        e.wait_ge(self.semh[ws], wv)
                    if fn is not None:
                        fn(e).then_inc(self.semh[inc[0]], inc[1])
            decs[name](body)


def build_nc(stage="full"):
    nc = bass.Bass("TRN2", target_bir_lowering=False)
    D = lambda n, sh: nc.dram_tensor(n, sh, F32, kind="ExternalInput").ap()
    xoT = D("xoT", [1024, NT])
    xpT = D("xpT", [1024, NT])
    vec = D("vec", [128, NV])
    modw = D("modw", [128, 8, 6144])
    cbTd = D("cbT", [16, 128])
    w1d = D("w1", [128, 8, 3088])
    w2d = D("w2", [128, 8, 1792])
    wod = D("wo", [128, 16, 1024])
    wrd = D("wr", [128, 8, 64])
    biasd = D("biasT", [128, 16 * 256])
    egd = D("eg", [NEXP, 128, 8 * 256])
    eud = D("eu", [NEXP, 128, 8 * 256])
    edd = D("ed", [NEXP, 128, 2 * 1024])
    outT = nc.dram_tensor("outT", [1024, NT], F32, kind="ExternalOutput").ap()
    xo3 = xoT.rearrange("(c p) t -> p c t", p=128)
    xp3 = xpT.rearrange("(c p) t -> p c t", p=128)
    out3 = outT.rearrange("(c p) t -> p c t", p=128)

    with ExitStack() as es:
        P = Prog(nc, es)
        SB = lambda n, sh, dt, st=es: st.enter_context(nc.sbuf_tensor(n, sh, dt))
        PS = es.enter_context(nc.psum_tensor("PS", [128, 4096], F32))
        PSB = PS[:].bitcast(BF16)

        def bank(b, n=512, off=0):
            return PS[:, b * 512 + off: b * 512 + off + n]

        def bankb(b, n=1024):
            return PSB[:, b * 1024: b * 1024 + n]
        pk = lambda *bs: [("ps", b) for b in bs]

        R = SB("R", [128, 8, NT], F32)
        V = SB("V", [128, NV], F32)
        U = SB("U", [128, 128], F32)
        SL = SB("SL", [128, 128], F32)
        idf = SB("idf", [128, 128], F32)
        idb = SB("idb", [128, 128], BF16)
        onf = SB("onf", [128, 128], F32)
        onb = SB("onb", [128, 128], BF16)
        MOD = SB("MOD", [128, 48], F32)
        A1 = SB("A1", [128, 8], F32)
        A2 = SB("A2", [128, 8], F32)
        g1bc = SB("g1bc", [128, 1024], F32)
        Abc = SB("Abc", [128, 16], F32)
        esink = SB("esink", [128, 16], F32)
        vs = lambda n, i=None: (V[:, _VOFF[n][0]:_VOFF[n][0] + _VOFF[n][1]] if i is None
                                else V[:, _VOFF[n][0] + i:_VOFF[n][0] + i + 1])

        P.dma("sp", V[:], vec[:, :], w=["V"], sem="v")
        P.op("pool", lambda e: e.memset(U[:], 1.0), w=["U"])
        P.op("pool", lambda e: e.affine_select(out=U[:], in_=U[:], pattern=[[1, 128]], compare_op=ALU.is_ge,
                                               fill=0.0, base=0, channel_multiplier=-1), r=["U"], w=["U"])
        P.op("pool", lambda e: e.memset(SL[:], 1.0), w=["SL"])
        P.op("pool", lambda e: e.affine_select(out=SL[:], in_=SL[:], pattern=[[-1, 128]], compare_op=ALU.is_ge,
                                               fill=0.0, base=-1, channel_multiplier=1), r=["SL"], w=["SL"])
        P.op("pool", lambda e: e.memset(idf[:], 1.0), w=["idf"])
        P.op("pool", lambda e: e.affine_select(out=idf[:], in_=idf[:], pattern=[[1, 128]], compare_op=ALU.is_equal,
                                               fill=0.0, base=0, channel_multiplier=-1), r=["idf"], w=["idf"])
        P.op("pool", lambda e: e.tensor_copy(out=idb[:], in_=idf[:]), r=["idf"], w=["idb"])
        P.op("pool", lambda e: e.memset(onf[:], 1.0), w=["onf"])
        P.op("pool", lambda e: e.memset(onb[:], 1.0), w=["onb"])

        def norm_h(xsrc, xkey, hT, hkey, Ag, sh_off, wk, n, ssb):
            sq, rs, hn = wk["sq"], wk["rs"], wk["hn"]
            hk = list(xkey) if wk.get("inplace") else ["hn"]
            P.op("act", lambda e: e.activation(out=sq[:, :, :n], in_=xsrc, func=AF.Square), r=xkey, w=["sq"])
            if wk.get("inplace"):
                P.op("pool", lambda e: e.tensor_tensor(out=hn[:, :, :n], in0=hn[:, :, :n],
                                                       in1=Ag[:].unsqueeze(2).broadcast_to([128, 8, n]), op=ALU.mult),
                     r=hk + ["A1", "A2"], w=hk)
            for c in range(8):
                P.op("pe", lambda e, c=c: e.matmul(bank(ssb, n), onb[:], sq[:, c, :n], start=(c == 0), stop=(c == 7)),
                     r=["sq", "onb"], w=pk(ssb))
            P.op("act", lambda e: e.activation(out=rs[:, :n], in_=bank(ssb, n), func=AF.Ln, bias=wk["eps"][:, 0:1],
                                               scale=1.0 / 1024), r=pk(ssb) + ["eps"], w=["rs"])
            P.op("act", lambda e: e.activation(out=rs[:, :n], in_=rs[:, :n], func=AF.Exp, scale=-0.5), r=["rs"], w=["rs"])
            if wk.get("inplace"):
                P.op("dve", lambda e: e.tensor_tensor(out=hn[:, :, :n], in0=hn[:, :, :n],
                                                      in1=rs[:, :n].unsqueeze(1).broadcast_to([128, 8, n]), op=ALU.mult),
                     r=hk + ["rs"], w=hk)
            else:
                P.op("dve", lambda e: e.tensor_tensor(out=hn[:, :, :n], in0=xsrc,
                                                      in1=rs[:, :n].unsqueeze(1).broadcast_to([128, 8, n]), op=ALU.mult),
                     r=xkey + ["rs"], w=hk)
                P.op("pool", lambda e: e.tensor_tensor(out=hn[:, :, :n], in0=hn[:, :, :n],
                                                       in1=Ag[:].unsqueeze(2).broadcast_to([128, 8, n]), op=ALU.mult),
                     r=hk + ["A1", "A2"], w=hk)
            P.op("dve", lambda e: e.tensor_tensor(out=hT, in0=hn[:, :, :n],
                                                  in1=MOD[:, sh_off:sh_off + 8].unsqueeze(2).broadcast_to([128, 8, n]),
                                                  op=ALU.add), r=hk + ["MOD"], w=hkey)

        with ExitStack() as e1:
            w1 = SB("w1s", [128, 8, 3088], BF16, e1)
            wo1 = SB("wo1", [128, 8, 1024], BF16, e1)
            with ExitStack() as esA:
                sc = SB("sc", [128, 8], BF16, esA)
                mw = [SB("mw%d" % i, [128, 8, 1024], BF16, esA) for i in range(4)]
                dg = SB("dg", [128, 128], F32, esA)
                P.op("act", lambda e: e.activation(out=sc[:], in_=vs("cT"), func=AF.Silu), r=["V"], w=["sc"])

                def load_mw(m):
                    P.dma_group("pool", [(mw[m % 4][:, c, :], modw[:, c, m * 1024:(m + 1) * 1024]) for c in range(8)],
                                w=[("mw", m % 4)], sem="mw%d" % (m % 4))
                for m in range(4):
                    load_mw(m)
                for m in range(6):
                    s = m % 4
                    for j in range(8):
                        col = m * 8 + j
                        for c in range(8):
                            P.op("pe", lambda e, s=s, j=j, c=c, col=col: e.matmul(
                                bank(0, 1, col), mw[s][:, c, j * 128:(j + 1) * 128], sc[:, c:c + 1],
                                start=(c == 0), stop=(c == 7)), r=[("mw", s), "sc"], w=pk(0))
                    if m + 4 < 6:
                        load_mw(m + 4)
                    if m == 1:
                        P.dma_group("pool", [(w1[:, c, :], w1d[:, c, :]) for c in range(8)], w=["w1"], sem="w1")
                        P.dma("pool", wo1[:], wod[:, 0:8, :], w=["wo1"], sem="wo1")
                P.op("dve", lambda e: e.tensor_tensor(out=MOD[:], in0=bank(0, 48), in1=vs("modb"), op=ALU.add),
                     r=pk(0) + ["V"], w=["MOD"])
                P.op("dve", lambda e: e.scalar_tensor_tensor(out=A1[:], in0=MOD[:, 8:16], scalar=1.0, in1=vs("n1g"),
                                                             op0=ALU.add, op1=ALU.mult), r=["MOD", "V"], w=["A1"])
                P.op("dve", lambda e: e.scalar_tensor_tensor(out=A2[:], in0=MOD[:, 32:40], scalar=1.0, in1=vs("n2g"),
                                                             op0=ALU.add, op1=ALU.mult), r=["MOD", "V"], w=["A2"])
                for gi, (gb, o) in enumerate(((g1bc, 16),)):
                    for c in range(8):
                        P.op("dve", lambda e, c=c, o=o: e.tensor_scalar(out=dg[:], in0=idf[:], scalar1=MOD[:, o + c:o + c + 1],
                                                                        scalar2=None, op0=ALU.mult),
                             r=["MOD", "idf"], w=["dg"])
                        P.op("pe", lambda e, c=c, gi=gi: e.matmul(bank(1 + 2 * gi + c // 4, 128, (c % 4) * 128), onf[:], dg[:],
                                                                  start=True, stop=True),
                             r=["onf", "dg"], w=pk(1 + 2 * gi + c // 4))
                    P.op("dve", lambda e, gb=gb, gi=gi: e.tensor_copy(out=gb[:], in_=PS[:, (1 + 2 * gi) * 512:(3 + 2 * gi) * 512]),
                         r=pk(1 + 2 * gi, 2 + 2 * gi), w=["gbc%d" % gi])
                P.op("act", lambda e: e.activation(out=Abc[:], in_=vs("alog"), func=AF.Exp), r=["V"], w=["Abc"])
                P.op("dve", lambda e: e.tensor_scalar(out=Abc[:], in0=Abc[:], scalar1=-1.0, scalar2=None, op0=ALU.mult),
                     r=["Abc"], w=["Abc"])
                P.op("act", lambda e: e.activation(out=esink[:], in_=vs("sinks"), func=AF.Exp), r=["V"], w=["esink"])
                P.barrier()
            P.dma_group("sp", [(R[:, c, :], xo3[:, c, :]) for c in range(8)], w=[("R", c) for c in range(8)], sem="r")

            xc = [SB("xc%d" % i, [128, 8, 128], F32, e1) for i in range(2)]
            wk = dict(sq=SB("sq1", [128, 8, 128], BF16, e1), rs=SB("rs1", [128, 128], F32, e1),
                      eps=SB("eps1", [128, 1], F32, e1), inplace=True)
            hTs = [SB("hT1%d" % i, [128, 8, 128], BF16, e1) for i in range(2)]
            pre = SB("pre", [128, 16, 131], BF16, e1)
            cdiag = SB("cdiag", [128, 16, 4, 128], BF16, e1)
            cbT = SB("cbTs", [16, 128], BF16, e1)
            P.dma("pool", cbT[:], cbTd[:, :], w=["cbT"], sem="cbT")
            xbcT = SB("xbcT", [128, 16, 128], BF16, e1)
            sm = SB("sm1", [128, 16, 12], F32, e1)
            S32 = SB("S32", [128, 1024], F32, e1)
            Sb = SB("Sb", [128, 1024], BF16, e1)
            Xd = SB("Xd", [128, 16, 64], BF16, e1)
            Xdd = SB("Xdd", [128, 16, 64], BF16, e1)
            Btok = SB("Btok", [128, 4, 128], BF16, e1)
            rhsA = SB("rhsA", [128, 16, 128], F32, e1)
            Lt = SB("Lt", [128, 16, 128], BF16, e1)
            CBm = SB("CBm", [128, 4, 128], BF16, e1)
            _rf = rhsA[:].rearrange("p h l -> p (h l)")
            y1 = _rf[:, 0:1024].rearrange("p (h d) -> p h d", h=16)
            y2 = _rf[:, 1024:2048].rearrange("p (h d) -> p h d", h=16)
            szt = SB("szt", [128, 1024], BF16, e1)
            ssq = SB("ssq1", [128, 4], F32, e1)
            yn = SB("yn", [128, 1024], BF16, e1)
            ymT = SB("ymT", [128, 8, 128], BF16, e1)
            SMK = lambda i: ("sm", i)
            smv = lambda i: sm[:, :, i]

            P.op("pool", lambda e: e.memset(wk["eps"][:], EPS), w=["eps"])
            for c in range(8):
                P.op("dve", lambda e, c=c: e.scalar_tensor_tensor(out=wo1[:, c, :], in0=wo1[:, c, :], scalar=vs("wog", c),
                                                                  in1=g1bc[:], op0=ALU.mult, op1=ALU.mult),
                     r=["wo1", "V", "gbc0"], w=["wo1"])
            for tl in range(16):
                for k in range(4):
                    P.op("dve", lambda e, tl=tl, k=k: e.tensor_scalar(out=cdiag[:, tl, k, :], in0=idf[:],
                                                                     scalar1=vs("convw", tl * 4 + k), scalar2=None,
                                                                     op0=ALU.mult), r=["idf", "V"], w=["cdiag"])
            P.op("pool", lambda e: e.memset(S32[:], 0.0), w=["S32"])
            P.op("pool", lambda e: e.memset(Sb[:], 0.0), w=["Sb"])
            P.op("pool", lambda e: e.memset(pre[:], 0.0), w=["pre"])

            def LOADX(t_):
                src_ = xo3 if t_ >= 16 else xp3
                tc_ = (t_ % 16) * 128
                P.dma("sp", xc[t_ % 2][:], src_[:, :, tc_:tc_ + 128], w=[("xc", t_ % 2)], sem="xc%d" % (t_ % 2))

            def NORM(t_):
                wk["hn"] = xc[t_ % 2]
                norm_h(xc[t_ % 2][:], [("xc", t_ % 2)], hTs[t_ % 2][:], [("hT", t_ % 2)], A1, 0, wk, 128, 0)
            LOADX(0)
            NORM(0)
            for t in range(32):
                own = t >= 16
                tc = (t % 16) * 128
                par = t % 2
                src = xo3 if own else xp3
                if t + 1 < 32:
                    LOADX(t + 1)
                hT, HK = hTs[par], ("hT", par)
                for c in range(8):
                    P.op("pe", lambda e, c=c: e.matmul(bank(5, 16), hT[:, c, :], w1[:, c, 3072:3088],
                                                       start=(c == 0), stop=(c == 7)), r=[HK, "w1"], w=pk(5))
                P.op("dve", lambda e: e.tensor_tensor(out=smv(0), in0=bank(5, 16), in1=vs("dtb"), op=ALU.add),
                     r=pk(5) + ["V"], w=[SMK(0)])
                P.op("dve", lambda e: e.tensor_scalar(out=smv(1), in0=smv(0), scalar1=-1.0, scalar2=None, op0=ALU.mult),
                     r=[SMK(0)], w=[SMK(1)])
                P.op("dve", lambda e: e.tensor_tensor(out=smv(1), in0=smv(1), in1=smv(0), op=ALU.max),
                     r=[SMK(0), SMK(1)], w=[SMK(1)])
                P.op("act", lambda e: e.activation(out=smv(2), in_=smv(1), func=AF.Exp, scale=-1.0),
                     r=[SMK(1)], w=[SMK(2)])
                P.op("act", lambda e: e.activation(out=smv(3), in_=smv(2), func=AF.Ln, bias=onf[:, 0:1]),
                     r=[SMK(2), "onf"], w=[SMK(3)])
                P.op("dve", lambda e: e.scalar_tensor_tensor(out=smv(4), in0=smv(0), scalar=0.0, in1=smv(3),
                                                             op0=ALU.max, op1=ALU.add), r=[SMK(0), SMK(3)], w=[SMK(4)])
                P.op("dve", lambda e: e.tensor_tensor(out=smv(5), in0=smv(4), in1=Abc[:], op=ALU.mult),
                     r=[SMK(4), "Abc"], w=[SMK(5)])
                cur = pre
                if t == 16:
                    P.op("pool", lambda e: e.tensor_scalar(out=pre[:, :, 0:3], in0=pre[:, :, 128:131],
                                                           scalar1=vs("flag", 0), scalar2=None, op0=ALU.mult),
                         r=["pre", "V"], w=["pre"])
                else:
                    P.op("pool", lambda e: e.tensor_copy(out=pre[:, :, 0:3], in_=pre[:, :, 128:131]),
                         r=["pre"], w=["pre"])
                for bg in range(4 if t >= 15 else 3):
                    b = 1 + bg % 2
                    for ti in range(4):
                        tl = bg * 4 + ti
                        for c in range(8):
                            P.op("pe", lambda e, b=b, ti=ti, tl=tl, c=c: e.matmul(
                                bank(b, 128, ti * 128), w1[:, c, tl * 128:(tl + 1) * 128], hT[:, c, :],
                                start=(c == 0), stop=(c == 7)), r=["w1", HK], w=pk(b))
                    eng = "act" if bg % 2 == 0 else "dve"
                    if eng == "act":
                        P.op("act", lambda e, b=b, bg=bg, cur=cur: e.activation(
                            out=cur[:, bg * 4:(bg + 1) * 4, 3:131], in_=bank(b).rearrange("p (a n) -> p a n", a=4),
                            func=AF.Copy), r=pk(b), w=["pre"])
                    else:
                        P.op("dve", lambda e, b=b, bg=bg, cur=cur: e.tensor_copy(
                            out=cur[:, bg * 4:(bg + 1) * 4, 3:131], in_=bank(b).rearrange("p (a n) -> p a n", a=4)),
                            r=pk(b), w=["pre"])
                if t + 1 < 32:
                    NORM(t + 1)
                P.op("pe", lambda e: e.matmul(bank(5, 16, 16), U[:], smv(5), start=True, stop=True),
                     r=["U", SMK(5)], w=pk(5))
                P.op("pe", lambda e: e.matmul(bank(5, 16, 32), onf[:], smv(5), start=True, stop=True),
                     r=["onf", SMK(5)], w=pk(5))
                P.op("dve", lambda e: e.tensor_copy(out=smv(6), in_=bank(5, 16, 16)), r=pk(5), w=[SMK(6)])
                P.op("act", lambda e: e.activation(out=smv(7), in_=bank(5, 16, 16), func=AF.Exp), r=pk(5), w=[SMK(7)])
                P.op("dve", lambda e: e.tensor_tensor(out=smv(10), in0=bank(5, 16, 32), in1=smv(6), op=ALU.subtract),
                     r=pk(5) + [SMK(6)], w=[SMK(10)])
                P.op("act", lambda e: e.activation(out=smv(8), in_=smv(10), func=AF.Exp), r=[SMK(10)], w=[SMK(8)])
                P.op("act", lambda e: e.activation(out=smv(9), in_=bank(5, 16, 32), func=AF.Exp), r=pk(5), w=[SMK(9)])
                if own:
                    for hf in range(2):
                        for c in range(8):
                            P.op("pe", lambda e, hf=hf, c=c: e.matmul(bank(hf), hT[:, c, :],
                                                                      w1[:, c, 2048 + hf * 512: 2048 + (hf + 1) * 512],
                                                                      start=(c == 0), stop=(c == 7)), r=[HK, "w1"], w=pk(hf))
                    P.op("act", lambda e: e.activation(out=szt[:], in_=PS[:, 0:1024], func=AF.Silu), r=pk(0, 1), w=["szt"])
                ntl = 16 if t >= 15 else 12
                for bg in range(ntl // 4):
                    b = 3 + bg % 2
                    for ti in range(4):
                        tl = bg * 4 + ti
                        P.op("pe", lambda e, b=b, ti=ti, tl=tl: e.matmul(
                            bank(b, 128, ti * 128), cbT[:], idb[0:16, tl:tl + 1].broadcast_to([16, 128]),
                            start=True, stop=False), r=["cbT", "idb"], w=pk(b))
                        for k in range(4):
                            P.op("pe", lambda e, b=b, ti=ti, tl=tl, k=k: e.matmul(
                                bank(b, 128, ti * 128), cdiag[:, tl, k, :], pre[:, tl, k:k + 128],
                                start=False, stop=(k == 3)), r=["cdiag", "pre"], w=pk(b))
                    P.op("act", lambda e, b=b, bg=bg: e.activation(
                        out=xbcT[:, bg * 4:(bg + 1) * 4, :], in_=bank(b).rearrange("p (a n) -> p a n", a=4),
                        func=AF.Silu), r=pk(b), w=["xbcT"])
                for c in range(8):
                    P.op("pe", lambda e, c=c: e.transpose(out=bankb(6, 128 * (c + 1))[:, c * 128:(c + 1) * 128],
                                                          in_=xbcT[:, c, :], identity=idb[:]),
                         r=["xbcT", "idb"], w=pk(6))
                for g in range(4):
                    P.op("pe", lambda e, g=g: e.transpose(out=bankb(7, 128 * (g + 1))[:, g * 128:(g + 1) * 128],
                                                          in_=xbcT[:, 8 + g, :], identity=idb[:]),
                         r=["xbcT", "idb"], w=pk(7))
                P.op("dve", lambda e: e.tensor_tensor(out=Xd[:], in0=bankb(6).rearrange("p (h d) -> p h d", h=16),
                                                      in1=smv(4).unsqueeze(2).broadcast_to([128, 16, 64]), op=ALU.mult),
                     r=pk(6) + [SMK(4)], w=["Xd"])
                P.op("act", lambda e: e.activation(out=Btok[:], in_=bankb(7, 512).rearrange("p (g n) -> p g n", g=4),
                                                   func=AF.Copy), r=pk(7), w=["Btok"])
                P.op("pool", lambda e: e.tensor_tensor(out=Xdd[:], in0=Xd[:],
                                                       in1=smv(8).unsqueeze(2).broadcast_to([128, 16, 64]), op=ALU.mult),
                     r=["Xd", SMK(8)], w=["Xdd"])
                if own:
                    P.op("dve", lambda e: e.tensor_tensor(out=rhsA[:], in0=U[:].unsqueeze(1).broadcast_to([128, 16, 128]),
                                                          in1=smv(5).unsqueeze(2).broadcast_to([128, 16, 128]),
                                                          op=ALU.mult), r=["U", SMK(5)], w=["rhsA"])
                    for q4 in range(4):
                        P.op("pe", lambda e, q4=q4: e.matmul(bank(q4), SL[:], rhsA[:, q4 * 4:(q4 + 1) * 4, :],
                                                             start=True, stop=True), r=["SL", "rhsA"], w=pk(q4))
                    P.op("dve", lambda e: e.tensor_tensor(out=y2, in0=bankb(6).rearrange("p (h d) -> p h d", h=16),
                                                          in1=vs("dskip").unsqueeze(2).broadcast_to([128, 16, 64]),
                                                          op=ALU.mult), r=pk(6) + ["V"], w=["rhsA"])
                    P.op("act", lambda e: e.activation(out=Lt[:], in_=PS[:, 0:2048].rearrange("p (h l) -> p h l", h=16),
                                                       func=AF.Exp), r=pk(0, 1, 2, 3), w=["Lt"])
                    for g in range(4):
                        P.op("pe", lambda e, g=g: e.matmul(bank(7, 128, g * 128), xbcT[:, 8 + g, :], xbcT[:, 12 + g, :],
                                                           start=True, stop=True), r=["xbcT"], w=pk(7))
                    P.op("dve", lambda e: e.tensor_tensor(out=CBm[:], in0=bank(7).rearrange("p (g l) -> p g l", g=4),
                                                          in1=U[:].unsqueeze(1).broadcast_to([128, 4, 128]), op=ALU.mult),
                         r=pk(7) + ["U"], w=["CBm"])
                    P.op("dve", lambda e: e.tensor_tensor(
                        out=Lt[:].rearrange("p (g r) l -> p g r l", g=4), in0=Lt[:].rearrange("p (g r) l -> p g r l", g=4),
                        in1=CBm[:].unsqueeze(2).broadcast_to([128, 4, 4, 128]), op=ALU.mult), r=["Lt", "CBm"], w=["Lt"])
                    for h in range(16):
                        P.op("pe", lambda e, h=h: e.matmul(PS[:, 2048 + h * 64: 2048 + (h + 1) * 64], Lt[:, h, :], Xd[:, h, :],
                                                           start=True, stop=True), r=["Lt", "Xd"], w=pk(4 + h // 8))
                    for h in range(16):
                        P.op("pe", lambda e, h=h: e.matmul(PS[:, 3072 + h * 64: 3072 + (h + 1) * 64], xbcT[:, 12 + h // 4, :],
                                                           Sb[:, h * 64:(h + 1) * 64], start=True, stop=True),
                             r=["xbcT", "Sb"], w=pk(6 + h // 8))
                    for dmy in range(NDUMMY):
                        P.op("pe", lambda e, dmy=dmy: e.matmul(bank(dmy % 2), idb[:], w1[:, dmy % 8, 0:512], start=True, stop=True),
                             r=["idb", "w1"], w=pk(dmy % 2))
                    P.op("dve", lambda e: e.tensor_tensor(out=y1, in0=PS[:, 3072:4096].rearrange("p (h d) -> p h d", h=16),
                                                          in1=smv(7).unsqueeze(2).broadcast_to([128, 16, 64]), op=ALU.mult),
                         r=pk(6, 7) + [SMK(7)], w=["rhsA"])
                    P.op("dve", lambda e: e.tensor_tensor(out=y1, in0=PS[:, 2048:3072].rearrange("p (h d) -> p h d", h=16),
                                                          in1=y1, op=ALU.add), r=pk(4, 5) + ["rhsA"], w=["rhsA"])
                    P.op("dve", lambda e: e.tensor_tensor(out=y2, in0=y2, in1=y1, op=ALU.add),
                         r=["rhsA"], w=["rhsA"])
                    y1f = _rf[:, 0:1024]
                    P.op("dve", lambda e, y1f=y1f: e.tensor_tensor(out=y1f, in0=szt[:], in1=_rf[:, 1024:2048],
                                                                   op=ALU.mult), r=["szt", "rhsA"], w=["rhsA"])
                    P.op("pool", lambda e: e.memset(ssq[:], 0.0), w=["ssq"])
                    for g in range(4):
                        P.op("act", lambda e, g=g, y1f=y1f: e.activation(
                            out=Lt[:, 0:2, :].rearrange("p a l -> p (a l)"), in_=y1f[:, g * 256:(g + 1) * 256],
                            func=AF.Square, accum_out=ssq[:, g:g + 1]), r=["rhsA", "ssq"], w=["Lt", "ssq"])
                    P.op("act", lambda e: e.activation(out=ssq[:], in_=ssq[:], func=AF.Ln, bias=wk["eps"][:, 0:1],
                                                       scale=1.0 / 256), r=["ssq", "eps"], w=["ssq"])
                    P.op("act", lambda e: e.activation(out=ssq[:], in_=ssq[:], func=AF.Exp, scale=-0.5), r=["ssq"], w=["ssq"])
                    P.op("dve", lambda e: e.tensor_tensor(out=yn[:].rearrange("p (g f) -> p g f", g=4),
                                                          in0=y1f.rearrange("p (g f) -> p g f", g=4),
                                                          in1=ssq[:].unsqueeze(2).broadcast_to([128, 4, 256]), op=ALU.mult),
                         r=["rhsA", "ssq"], w=["yn"])
                    for c in range(8):
                        P.op("pe", lambda e, c=c: e.transpose(out=bankb(2, 128 * (c + 1))[:, c * 128:(c + 1) * 128],
                                                              in_=yn[:, c * 128:(c + 1) * 128], identity=idb[:]),
                             r=["yn", "idb"], w=pk(2))
                    P.op("act", lambda e: e.activation(out=ymT[:], in_=bankb(2).rearrange("p (c l) -> p c l", c=8),
                                                       func=AF.Copy), r=pk(2), w=["ymT"])
                    out_proj(P, bank, wo1, "wo1", ymT, "ymT", R, t - 16, (3, 4))
                if t < 31:
                    for h in range(16):
                        P.op("pe", lambda e, h=h: e.matmul(PS[:, 2560 + h * 64: 2560 + (h + 1) * 64], Btok[:, h // 4, :],
                                                           Xdd[:, h, :], start=True, stop=True),
                             r=["Btok", "Xdd"], w=pk(5 + h // 8))
                    P.op("pool", lambda e: e.tensor_tensor(out=S32[:].rearrange("p (h d) -> p h d", h=16),
                                                           in0=S32[:].rearrange("p (h d) -> p h d", h=16),
                                                           in1=smv(9).unsqueeze(2).broadcast_to([128, 16, 64]),
                                                           op=ALU.mult), r=["S32", SMK(9)], w=["S32"])
                    P.op("dve", lambda e: e.tensor_tensor(out=S32[:], in0=PS[:, 2560:3584], in1=S32[:], op=ALU.add),
                         r=pk(5, 6) + ["S32"], w=["S32"])
                    if t == 15:
                        P.op("dve", lambda e: e.tensor_scalar(out=S32[:], in0=S32[:], scalar1=vs("flag", 0), scalar2=None,
                                                              op0=ALU.mult), r=["S32", "V"], w=["S32"])
                    P.op("act", lambda e: e.activation(out=Sb[:], in_=S32[:], func=AF.Copy), r=["S32"], w=["Sb"])
            P.barrier()

        if stage == "ssd":
            return finish(nc, P, R, out3, es)

        with ExitStack() as eW:
            wg = [SB("wg%d" % i, [128, 8, 256], BF16, eW) for i in range(3)]
            wu = [SB("wu%d" % i, [128, 8, 256], BF16, eW) for i in range(3)]
            wd = [SB("wd%d" % i, [128, 2, 1024], BF16, eW) for i in range(3)]
            wr = SB("wrs", [128, 8, 64], F32, eW)

            def load_expert_dma(e_):
                s = e_ % 3
                P.dma_group("pool", [(wg[s][:].rearrange("p c n -> p (c n)"), egd[e_, :, :]),
                                     (wu[s][:].rearrange("p c n -> p (c n)"), eud[e_, :, :]),
                                     (wd[s][:].rearrange("p c n -> p (c n)"), edd[e_, :, :])],
                            w=[("wg", s), ("wu", s), ("wd", s)], sem="ew%d" % s)
            with ExitStack() as e2:
                w2 = SB("w2s", [128, 8, 1792], BF16, e2)
                wo2 = SB("wo2", [128, 8, 1024], BF16, e2)
                bT = SB("bT", [128, 16, 256], BF16, e2)
                xc = [SB("xd%d" % i, [128, 8, 128], F32, e2) for i in range(2)]
                wk = dict(sq=SB("sq2", [128, 8, 128], BF16, e2), rs=SB("rs2", [128, 128], F32, e2),
                          eps=SB("eps2", [128, 1], F32, e2), inplace=True)
                hT = SB("hT2", [128, 8, 128], BF16, e2)
                kT = [SB("kT%d" % i, [128, 4, 128], BF16, e2) for i in range(3)]
                qT = [SB("qT%d" % i, [128, 8, 128], BF16, e2) for i in range(2)]
                Va = [SB("Va%d" % i, [128, 4, 65], BF16, e2) for i in range(3)]
                PT = SB("PT", [128, 16, 2, 128], BF16, e2)
                den = SB("den", [128, 16], F32, e2)
                on = SB("on", [128, 16, 64], F32, e2)
                junk = SB("junk2", [128, 1024], F32, e2)
                ss1 = SB("ss1", [128, 1], F32, e2)
                ya = SB("ya", [128, 1024], BF16, e2)
                yaT = SB("yaT", [128, 8, 128], BF16, e2)

                P.op("pool", lambda e: e.memset(wk["eps"][:], EPS), w=["eps"])
                P.dma_group("pool", [(w2[:, c, :], w2d[:, c, :]) for c in range(8)], w=["w2"], sem="w2")
                P.dma("pool", wo2[:], wod[:, 8:16, :], w=["wo2"], sem="wo2")
                P.dma("pool", bT[:].rearrange("p h n -> p (h n)"), biasd[:, :], w=["bT"], sem="bT")
                load_expert_dma(0)
                load_expert_dma(1)
                P.dma("sp", wr[:], wrd[:, :, :], w=["wr"], sem="wr")
                for c in range(8):
                    P.op("dve", lambda e, c=c: e.scalar_tensor_tensor(out=wo2[:, c, :], in0=wo2[:, c, :], scalar=vs("wog", 8 + c),
                                                                      in1=g1bc[:], op0=ALU.mult, op1=ALU.mult),
                         r=["wo2", "V", "gbc0"], w=["wo2"])
                for i in range(3):
                    P.op("pool", lambda e, i=i: e.memset(Va[i][:], 1.0), w=[("Va", i)])

                def F(t):
                    own = t >= 0
                    par = t % 2
                    p3 = t % 3
                    if own:
                        P.dma("sp", xc[par][:], xo3[:, :, t * 128:(t + 1) * 128], w=[("xc", par)], sem="xc%d" % par)
                    else:
                        P.dma("sp", xc[par][:], xp3[:, :, NT - 128:NT], w=[("xc", par)], sem="xc%d" % par)
                    wk["hn"] = xc[par]
                    norm_h(xc[par][:], [("xc", par)], hT[:], ["hT"], A1, 0, wk, 128, 0)
                    for kh in range(4):
                        for c in range(8):
                            P.op("pe", lambda e, kh=kh, c=c: e.matmul(bank(1, 128, kh * 128), w2[:, c, 1024 + kh * 128:1024 + (kh + 1) * 128],
                                                                      hT[:, c, :], start=(c == 0), stop=(c == 7)),
                                 r=["w2", "hT"], w=pk(1))
                    P.op("act", lambda e, p3=p3: e.activation(out=kT[p3][:], in_=bank(1).rearrange("p (a n) -> p a n", a=4),
                                                                func=AF.Copy), r=pk(1), w=[("kT", p3)])
                    for c in range(8):
                        P.op("pe", lambda e, c=c: e.matmul(bank(0, 256, 128), hT[:, c, :], w2[:, c, 1536:1792],
                                                           start=(c == 0), stop=(c == 7)), r=["w2", "hT"], w=pk(0))
                    P.op("dve", lambda e, p3=p3: e.tensor_copy(out=Va[p3][:, :, 0:64],
                                                                in_=bank(0, 256, 128).rearrange("p (k d) -> p k d", k=4)),
                         r=pk(0), w=[("Va", p3)])
                    if not own:
                        return
                    for bg in range(2):
                        b = 2
                        for ti in range(4):
                            tl = bg * 4 + ti
                            for c in range(8):
                                P.op("pe", lambda e, b=b, ti=ti, tl=tl, c=c: e.matmul(
                                    bank(b, 128, ti * 128), w2[:, c, tl * 128:(tl + 1) * 128], hT[:, c, :],
                                    start=(c == 0), stop=(c == 7)), r=["w2", "hT"], w=pk(b))
                        P.op("act", lambda e, b=b, bg=bg, par=par: e.activation(out=qT[par][:, bg * 4:(bg + 1) * 4, :],
                                                                       in_=bank(b).rearrange("p (a n) -> p a n", a=4),
                                                                       func=AF.Copy, scale=0.125), r=pk(b), w=[("qT", par)])
                def B(t):
                    par = t % 2
                    p3, q3 = t % 3, (t - 1) % 3
                    for hp in range(8):
                        b = 5 + hp % 2
                        for hh in range(2):
                            h = 2 * hp + hh
                            kh = h // 4
                            lo, hi = hh * 64, (hh + 1) * 64
                            P.op("pe", lambda e, b=b, hh=hh, h=h: e.matmul(bank(b, 256, hh * 256), idb[:], bT[:, h, :],
                                                                           start=True, stop=False), r=["idb", "bT"], w=pk(b))
                            P.op("pe", lambda e, b=b, hh=hh, kh=kh, lo=lo, hi=hi, hp=hp: e.matmul(
                                bank(b, 128, hh * 256), kT[q3][lo:hi, kh, :], qT[par][lo:hi, hp, :], start=False, stop=False),
                                r=[("kT", q3), ("qT", par)], w=pk(b))
                            P.op("pe", lambda e, b=b, hh=hh, kh=kh, lo=lo, hi=hi, hp=hp: e.matmul(
                                bank(b, 128, hh * 256 + 128), kT[p3][lo:hi, kh, :], qT[par][lo:hi, hp, :], start=False, stop=True),
                                r=[("kT", p3), ("qT", par)], w=pk(b))
                        P.op("act", lambda e, b=b, hp=hp: e.activation(
                            out=PT[:, 2 * hp:2 * hp + 2, :, :].rearrange("p h b q -> p (h b q)"), in_=bank(b), func=AF.Exp),
                            r=pk(b), w=["PT"])
                    if t == 0:
                        P.op("pool", lambda e: e.tensor_scalar(out=PT[:, :, 0, :], in0=PT[:, :, 0, :], scalar1=vs("flag", 0),
                                                               scalar2=None, op0=ALU.mult), r=["PT", "V"], w=["PT"])
                    hb = ((0, 6, 3), (6, 12, 4), (12, 16, 7))
                    for h0, h1, b in hb:
                        for h in range(h0, h1):
                            kh = h // 4
                            o_ = (h - h0) * 65
                            P.op("pe", lambda e, b=b, o_=o_, h=h, kh=kh: e.matmul(bank(b, 65, o_), PT[:, h, 0, :], Va[q3][:, kh, :],
                                                                                  start=True, stop=False),
                                 r=["PT", ("Va", q3)], w=pk(b))
                            P.op("pe", lambda e, b=b, o_=o_, h=h, kh=kh: e.matmul(bank(b, 65, o_), PT[:, h, 1, :], Va[p3][:, kh, :],
                                                                                  start=False, stop=True),
                                 r=["PT", ("Va", p3)], w=pk(b))
                    for h0, h1, b in hb:
                        n = h1 - h0
                        pv = bank(b, n * 65).rearrange("p (h d) -> p h d", h=n)
                        P.op("dve", lambda e, pv=pv, h0=h0, h1=h1: e.tensor_tensor(out=den[:, h0:h1], in0=pv[:, :, 64],
                                                                                   in1=esink[:, h0:h1], op=ALU.add),
                             r=pk(b) + ["esink"], w=["den"])
                    P.op("dve", lambda e: e.reciprocal(out=den[:], in_=den[:]), r=["den"], w=["den"])
                    for h0, h1, b in hb:
                        n = h1 - h0
                        pv = bank(b, n * 65).rearrange("p (h d) -> p h d", h=n)
                        P.op("dve", lambda e, pv=pv, h0=h0, h1=h1, n=n: e.tensor_tensor(
                            out=on[:, h0:h1, :], in0=pv[:, :, 0:64], in1=den[:, h0:h1].unsqueeze(2).broadcast_to([128, n, 64]),
                            op=ALU.mult), r=pk(b) + ["den"], w=["on"])
                    P.op("pool", lambda e: e.memset(ss1[:], 0.0), w=["ss1"])
                    P.op("act", lambda e: e.activation(out=junk[:], in_=on[:].rearrange("p h d -> p (h d)"), func=AF.Square,
                                                       accum_out=ss1[:, 0:1]), r=["on", "ss1"], w=["junk", "ss1"])
                    P.op("act", lambda e: e.activation(out=ss1[:], in_=ss1[:], func=AF.Ln, bias=wk["eps"][:, 0:1],
                                                       scale=1.0 / 1024), r=["ss1", "eps"], w=["ss1"])
                    P.op("act", lambda e: e.activation(out=ss1[:], in_=ss1[:], func=AF.Exp, scale=-0.5), r=["ss1"], w=["ss1"])
                    P.op("dve", lambda e: e.tensor_scalar(out=ya[:], in0=on[:].rearrange("p h d -> p (h d)"), scalar1=ss1[:, 0:1],
                                                          scalar2=None, op0=ALU.mult), r=["on", "ss1"], w=["ya"])
                    for c in range(8):
                        P.op("pe", lambda e, c=c: e.transpose(out=bankb(5, 128 * (c + 1))[:, c * 128:(c + 1) * 128],
                                                              in_=ya[:, c * 128:(c + 1) * 128], identity=idb[:]),
                             r=["ya", "idb"], w=pk(5))
                    P.op("act", lambda e: e.activation(out=yaT[:], in_=bankb(5).rearrange("p (c l) -> p c l", c=8),
                                                       func=AF.Copy), r=pk(5), w=["yaT"])
                    out_proj(P, bank, wo2, "wo2", yaT, "yaT", R, t, (6, 7))
                F(-1)
                F(0)
                for t in range(16):
                    if t + 1 < 16:
                        F(t + 1)
                    B(t)
                P.barrier()

            if stage == "mixer":
                return finish(nc, P, R, out3, es)

            with ExitStack() as e3:
                h2T = SB("h2T", [128, 8, NT], BF16, e3)
                gT = SB("gT", [128, NT], BF16, e3)
                selE = SB("selE", [128, NEXP, 128], BF16, e3)
                wk = dict(sq=SB("sq3", [128, 8, 512], BF16, e3), rs=SB("rs3", [128, 512], F32, e3),
                          hn=SB("hn3", [128, 8, 512], F32, e3), eps=SB("eps3", [128, 1], F32, e3))
                rt = SB("rt", [128, 12, 64], F32, e3)
                gsb = SB("gsb", [128, 512], F32, e3)
                sg = [SB("sg%d" % i, [128, 512], BF16, e3) for i in range(2)]
                tt = [SB("tt%d" % i, [128, 512], BF16, e3) for i in range(2)]
                uu = [[SB("uu%d%d" % (i, f), [128, 512], BF16, e3) for f in range(2)] for i in range(2)]

                g2bc = SB("g2bc", [128, 1024], F32, e3)
                dg2 = SB("dg2", [128, 128], F32, e3)
                for c in range(8):
                    P.op("dve", lambda e, c=c: e.tensor_scalar(out=dg2[:], in0=idf[:], scalar1=MOD[:, 40 + c:41 + c],
                                                               scalar2=None, op0=ALU.mult), r=["MOD", "idf"], w=["dg2"])
                    P.op("pe", lambda e, c=c: e.matmul(bank(3 + c // 4, 128, (c % 4) * 128), onf[:], dg2[:], start=True, stop=True),
                         r=["onf", "dg2"], w=pk(3 + c // 4))
                P.op("dve", lambda e: e.tensor_copy(out=g2bc[:], in_=PS[:, 3 * 512:5 * 512]), r=pk(3, 4), w=["gbc1"])
                P.op("pool", lambda e: e.memset(wk["eps"][:], EPS), w=["eps"])
                P.op("pool", lambda e: e.memset(selE[:], 1.0), w=["selE"])
                P.op("pool", lambda e: e.affine_select(out=selE[:], in_=selE[:], pattern=[[1, NEXP], [0, 128]],
                                                       compare_op=ALU.is_equal, fill=0.0, base=0, channel_multiplier=-1),
                     r=["selE"], w=["selE"])
                P.op("pool", lambda e: e.memset(gT[:], 0.0), w=[("gT", g_) for g_ in range(4)])
                P.op("pool", lambda e: e.memset(gT[64:65, :], 1.0), w=[("gT", g_) for g_ in range(4)])

                def scale_expert(e_):
                    s = e_ % 3
                    P.op("pool", lambda e, s=s: e.tensor_tensor(out=wd[s][:], in0=wd[s][:],
                                                                in1=g2bc[:].unsqueeze(1).broadcast_to([128, 2, 1024]), op=ALU.mult),
                         r=[("wd", s), "gbc1"], w=[("wd", s)])

                def load_expert(e_):
                    load_expert_dma(e_)
                    scale_expert(e_)
                scale_expert(0)
                scale_expert(1)

                rv = lambda i, n=64: rt[:, i, 0:n]
                RK = lambda i: ("rt", i)
                def PRO(tg):
                    ts_ = slice(tg * 512, (tg + 1) * 512)
                    norm_h(R[:, :, ts_], [("R", c, tg) for c in range(8)], h2T[:, :, ts_], [("h2T", tg)], A2, 24, wk, 512, 5)
                    P.op("dve", lambda e: e.tensor_tensor(out=wk["hn"][:], in0=wk["hn"][:],
                                                          in1=MOD[:, 24:32].unsqueeze(2).broadcast_to([128, 8, 512]), op=ALU.add),
                         r=["hn", "MOD"], w=["hn"])
                    for tl in range(4):
                        for c in range(8):
                            P.op("pe", lambda e, tl=tl, c=c: e.matmul(bank(6, 64), wk["hn"][:, c, tl * 128:(tl + 1) * 128], wr[:, c, :],
                                                                      start=(c == 0), stop=(c == 7)), r=["hn", "wr"], w=pk(6))
                        P.op("act", lambda e: e.activation(out=rv(0), in_=bank(6, 64), func=AF.Sigmoid), r=pk(6), w=[RK(0)])
                        P.op("dve", lambda e: e.tensor_tensor(out=rv(1), in0=rv(0), in1=vs("rbias"), op=ALU.add),
                             r=[RK(0), "V"], w=[RK(1)])
                        g3 = lambda ap: ap.rearrange("p (g e) -> p g e", e=8)
                        b3 = lambda ap: ap.unsqueeze(2).broadcast_to([128, 8, 8])
                        P.op("dve", lambda e: e.tensor_reduce(out=rv(6, 8), in_=g3(rv(1)), axis=AX.X, op=ALU.max), r=[RK(1)], w=[RK(6)])
                        P.op("dve", lambda e: e.tensor_tensor(out=g3(rv(2)), in0=g3(rv(1)), in1=b3(rv(6, 8)), op=ALU.is_equal),
                             r=[RK(1), RK(6)], w=[RK(2)])
                        P.op("dve", lambda e: e.scalar_tensor_tensor(out=rv(2), in0=rv(2), scalar=-1e9, in1=rv(1), op0=ALU.mult,
                                                                     op1=ALU.add), r=[RK(2), RK(1)], w=[RK(2)])
                        P.op("dve", lambda e: e.tensor_reduce(out=rv(7, 8), in_=g3(rv(2)), axis=AX.X, op=ALU.max), r=[RK(2)], w=[RK(7)])
                        P.op("dve", lambda e: e.tensor_tensor(out=rv(7, 8), in0=rv(7, 8), in1=rv(6, 8), op=ALU.add),
                             r=[RK(7), RK(6)], w=[RK(7)])
                        P.op("dve", lambda e: e.max(out=rv(8, 8), in_=rv(7, 8)), r=[RK(7)], w=[RK(8)])
                        P.op("dve", lambda e: e.tensor_scalar(out=rv(9, 8), in0=rv(7, 8), scalar1=rt[:, 8, 3:4], scalar2=None,
                                                              op0=ALU.is_ge), r=[RK(7), RK(8)], w=[RK(9)])
                        P.op("dve", lambda e: e.scalar_tensor_tensor(out=g3(rv(3)), in0=g3(rv(1)), scalar=2.0, in1=b3(rv(9, 8)),
                                                                     op0=ALU.add, op1=ALU.mult), r=[RK(1), RK(9)], w=[RK(3)])
                        P.op("dve", lambda e: e.max(out=rv(10, 8), in_=rv(3)), r=[RK(3)], w=[RK(10)])
                        P.op("dve", lambda e: e.tensor_scalar(out=rv(4), in0=rv(3), scalar1=rt[:, 10, 7:8], scalar2=None,
                                                              op0=ALU.is_ge), r=[RK(3), RK(10)], w=[RK(4)])
                        P.op("dve", lambda e: e.tensor_tensor(out=rv(4), in0=rv(4), in1=rv(0), op=ALU.mult),
                             r=[RK(4), RK(0)], w=[RK(4)])
                        P.op("dve", lambda e: e.tensor_reduce(out=rv(11, 1), in_=rv(4), axis=AX.X, op=ALU.add), r=[RK(4)], w=[RK(11)])
                        P.op("dve", lambda e: e.reciprocal(out=rv(11, 1), in_=rv(11, 1)), r=[RK(11)], w=[RK(11)])
                        P.op("dve", lambda e: e.tensor_scalar(out=rv(5), in0=rv(4), scalar1=rt[:, 11, 0:1], scalar2=2.5,
                                                              op0=ALU.mult, op1=ALU.mult), r=[RK(4), RK(11)], w=[RK(5)])
                        P.op("pe", lambda e: e.transpose(out=bank(7, 128)[0:64, :], in_=rv(5), identity=idf[:]),
                             r=[RK(5), "idf"], w=pk(7))
                        col = tg * 512 + tl * 128
                        P.op("act", lambda e, col=col: e.activation(out=gT[0:64, col:col + 128], in_=bank(7, 128)[0:64, :],
                                                                    func=AF.Copy), r=pk(7), w=[("gT", tg)])

                PRO(0)
                if stage != "norouted":
                    experts = list(range(NEXP))
                else:
                    experts = [64]
                steps = [(e_, tg) for e_ in experts for tg in range(4)]

                def down(i):
                    e_, tg = steps[i]
                    s = e_ % 3
                    for dtl in range(8):
                        b = 5 + dtl % 3
                        for f in range(2):
                            P.op("pe", lambda e, b=b, s=s, f=f, dtl=dtl, i=i: e.matmul(
                                bank(b), wd[s][:, f, dtl * 128:(dtl + 1) * 128], uu[i % 2][f][:], start=(f == 0), stop=(f == 1)),
                                r=[("wd", s), ("uu", i % 2, f)], w=pk(b))
                        P.op("dve", lambda e, b=b, dtl=dtl, tg=tg: e.tensor_tensor(
                            out=R[:, dtl, tg * 512:(tg + 1) * 512], in0=bank(b), in1=R[:, dtl, tg * 512:(tg + 1) * 512], op=ALU.add),
                            r=pk(b) + [("R", dtl, tg)], w=[("R", dtl, tg)])

                for i, (e_, tg) in enumerate(steps):
                    s = e_ % 3
                    if stage == "norouted" and i == 0:
                        P.barrier()
                        load_expert(64)
                    P.op("pe", lambda e, e_=e_, tg=tg: e.matmul(bank(0), selE[:, e_, :], gT[:, tg * 512:(tg + 1) * 512],
                                                                start=True, stop=True), r=["selE", ("gT", tg)], w=pk(0))
                    P.op("act", lambda e: e.activation(out=gsb[:], in_=bank(0), func=AF.Copy), r=pk(0), w=["gsb"])
                    for f in range(2):
                        bG, bU = 1 + 2 * f, 2 + 2 * f
                        for c in range(8):
                            P.op("pe", lambda e, bG=bG, s=s, c=c, f=f, tg=tg: e.matmul(
                                bank(bG), wg[s][:, c, f * 128:(f + 1) * 128], h2T[:, c, tg * 512:(tg + 1) * 512],
                                start=(c == 0), stop=(c == 7)), r=[("wg", s), ("h2T", tg)], w=pk(bG))
                        for c in range(8):
                            P.op("pe", lambda e, bU=bU, s=s, c=c, f=f, tg=tg: e.matmul(
                                bank(bU), wu[s][:, c, f * 128:(f + 1) * 128], h2T[:, c, tg * 512:(tg + 1) * 512],
                                start=(c == 0), stop=(c == 7)), r=[("wu", s), ("h2T", tg)], w=pk(bU))
                        P.op("act", lambda e, bG=bG, f=f: e.activation(out=sg[f][:], in_=bank(bG), func=AF.Silu),
                             r=pk(bG), w=[("sg", f)])
                        P.op("dve", lambda e, bU=bU, f=f: e.tensor_tensor(out=tt[f][:], in0=bank(bU), in1=gsb[:], op=ALU.mult),
                             r=pk(bU) + ["gsb"], w=[("tt", f)])
                        P.op("dve", lambda e, f=f, i=i: e.tensor_tensor(out=uu[i % 2][f][:], in0=sg[f][:], in1=tt[f][:], op=ALU.mult),
                             r=[("sg", f), ("tt", f)], w=[("uu", i % 2, f)])
                    if i > 0:
                        down(i - 1)
                    if e_ == experts[0] and tg + 1 < 4:
                        PRO(tg + 1)
                    if tg == 0 and stage != "norouted" and e_ + 2 < NEXP:
                        load_expert(e_ + 2)
                down(len(steps) - 1)

                for tg in range(4):
                    ts_ = slice(tg * 512, (tg + 1) * 512)
                    sq, rs, hn = wk["sq"], wk["rs"], wk["hn"]
                    P.op("act", lambda e, ts_=ts_: e.activation(out=sq[:], in_=R[:, :, ts_], func=AF.Square),
                         r=[("R", c, tg) for c in range(8)], w=["sq"])
                    for c in range(8):
                        P.op("pe", lambda e, c=c: e.matmul(bank(0), onb[:], sq[:, c, :], start=(c == 0), stop=(c == 7)),
                             r=["sq", "onb"], w=pk(0))
                    P.op("act", lambda e: e.activation(out=rs[:], in_=bank(0), func=AF.Ln, bias=wk["eps"][:, 0:1], scale=1.0 / 1024),
                         r=pk(0) + ["eps"], w=["rs"])
                    P.op("act", lambda e: e.activation(out=rs[:], in_=rs[:], func=AF.Exp, scale=-0.5), r=["rs"], w=["rs"])
                    P.op("dve", lambda e, ts_=ts_: e.tensor_tensor(out=hn[:], in0=R[:, :, ts_],
                                                                   in1=rs[:].unsqueeze(1).broadcast_to([128, 8, 512]), op=ALU.mult),
                         r=[("R", c, tg) for c in range(8)] + ["rs"], w=["hn"])
                    P.op("pool", lambda e: e.tensor_tensor(out=hn[:], in0=hn[:], in1=vs("fg").unsqueeze(2).broadcast_to([128, 8, 512]),
                                                           op=ALU.mult), r=["hn", "V"], w=["hn"])
                    P.dma_group("sp", [(out3[:, c, ts_], hn[:, c, :]) for c in range(8)], r=["hn"], sem="out")
                P.final_wait("sp", "out")
                P.emit()
    return nc


def out_proj(P, bank, wo, wokey, yT, ykey, R, t, banks):
    for half in range(2):
        b = banks[half]
        for di in range(4):
            dtl = half * 4 + di
            for fc in range(8):
                P.op("pe", lambda e, b=b, di=di, dtl=dtl, fc=fc: e.matmul(
                    bank(b, 128, di * 128), wo[:, fc, dtl * 128:(dtl + 1) * 128], yT[:, fc, :],
                    start=(fc == 0), stop=(fc == 7)), r=[wokey, ykey], w=[("ps", b)])
        P.op("dve", lambda e, b=b, half=half: e.tensor_tensor(
            out=R[:, half * 4:(half + 1) * 4, t * 128:(t + 1) * 128], in0=bank(b).rearrange("p (a n) -> p a n", a=4),
            in1=R[:, half * 4:(half + 1) * 4, t * 128:(t + 1) * 128], op=ALU.add),
            r=[("ps", b)] + [("R", half * 4 + i) for i in range(4)], w=[("R", half * 4 + i) for i in range(4)])


def finish(nc, P, R, out3, es):
    for c in range(8):
        P.dma("sp", out3[:, c, :], R[:, c, :], r=[("R", c)], sem="out")
    P.final_wait("sp", "out")
    P.emit()
    return nc


def _fm(v, n):
    return np.ascontiguousarray(np.asarray(v, np.float32).reshape(n, 128).T)


def _wl(w):
    K, N = w.shape
    return np.ascontiguousarray(np.asarray(w, np.float32).reshape(K // 128, 128, N).transpose(1, 0, 2))


def _bucket_table():
    import math
    q = np.arange(128)[None, :]
    j = np.arange(128)[:, None]
    tabs, valid = [], []
    for blk in range(2):
        dist = q + 128 - (blk * 128 + j)
        ok = (dist >= 0) & (dist < 128)
        d = np.maximum(dist, 0)
        dd = np.maximum(d, 1).astype(np.float32)
        large = 16 + (np.log(dd / np.float32(16)) / np.float32(math.log(128 / 16)) * np.float32(16)).astype(np.int32)
        large = np.minimum(large, 31)
        tabs.append(np.where(d < 16, d, large))
        valid.append(ok)
    return np.stack(tabs, 1), np.stack(valid, 1)


def make_in_maps(inp):
    f = lambda k: np.asarray(inp[k], np.float32)
    x, c = f("x"), f("c")
    W = f("w_in")[0]
    z_, xbc_, dt_, q_, k_, v_ = (W[:, 0:1024], W[:, 1024:3072], W[:, 3072:3088], W[:, 3088:4112], W[:, 4112:4368],
                                 W[:, 4368:4624])
    w1 = _wl(np.concatenate([xbc_, z_, dt_], 1))
    kd = np.concatenate([np.concatenate([k_[:, h * 64:(h + 1) * 64]] * 2, 1) for h in range(4)], 1)
    w2 = _wl(np.concatenate([q_, kd, v_], 1))
    wo = _wl(f("w_out")[0])
    wr = _wl(f("router_w")[0])
    modw = _wl(f("mod_w")[0])
    tab, ok = _bucket_table()
    rb = f("rel_bias")
    bias = rb[tab]
    bias = np.where(ok[..., None], bias, np.float32(-30000.0)).transpose(0, 3, 1, 2)
    biasT = np.ascontiguousarray(bias.reshape(128, 16 * 256), np.float32)
    eg = np.concatenate([f("exp_w_gate")[0], f("sh_w_gate")], 0)
    eu = np.concatenate([f("exp_w_up")[0], f("sh_w_up")], 0)
    ed = np.concatenate([f("exp_w_down")[0], f("sh_w_down")], 0)
    eg = np.ascontiguousarray(eg.reshape(NEXP, 8, 128, 256).transpose(0, 2, 1, 3).reshape(NEXP, 128, 2048))
    eu = np.ascontiguousarray(eu.reshape(NEXP, 8, 128, 256).transpose(0, 2, 1, 3).reshape(NEXP, 128, 2048))
    ed = np.ascontiguousarray(ed.reshape(NEXP, 2, 128, 1024).transpose(0, 2, 1, 3).reshape(NEXP, 128, 2048))
    bc = lambda v: np.broadcast_to(np.asarray(v, np.float32).reshape(1, -1), (128, np.asarray(v).size))
    cbT = np.ascontiguousarray(f("conv_b")[0].reshape(16, 128))
    maps = []
    for core in range(8):
        b, hf = core // 2, core % 2
        vec = np.zeros((128, NV), np.float32)

        def put(n, a):
            o, w = _VOFF[n]
            vec[:, o:o + w] = a
        put("n1g", _fm(f("norm1_g")[0], 8))
        put("n2g", _fm(f("norm2_g")[0], 8))
        put("fg", _fm(f("final_g"), 8))
        put("modb", _fm(f("mod_b")[0], 48))
        put("convw", f("conv_w")[0].T.reshape(16, 128, 4).transpose(1, 0, 2).reshape(128, 64))
        put("convb", _fm(f("conv_b")[0], 16))
        put("dtb", bc(f("dt_bias")[0]))
        put("alog", bc(f("a_log")[0]))
        put("dskip", bc(f("d_skip")[0]))
        put("sinks", bc(f("sinks")[0]))
        put("rbias", bc(f("router_bias")[0]))
        put("wog", _fm(np.concatenate([f("ssm_norm_g")[0], f("att_norm_g")[0]]), 16))
        put("flag", np.full((128, 1), float(hf), np.float32))
        put("cT", _fm(c[b], 8))
        xo = np.ascontiguousarray(x[b, hf * NT:(hf + 1) * NT, :].T)
        xp = np.ascontiguousarray(x[b, 0:NT, :].T) if hf == 1 else np.zeros((1024, NT), np.float32)
        maps.append(dict(xoT=xo, xpT=xp, vec=vec, modw=modw, cbT=cbT, w1=w1, w2=w2, wo=wo, wr=wr, biasT=biasT,
                         eg=eg, eu=eu, ed=ed))
    return maps


_NC_CACHE = {}


def run(inp, stage="full"):
    if stage not in _NC_CACHE:
        _NC_CACHE[stage] = build_nc(stage)
    nc = _NC_CACHE[stage]
    maps = make_in_maps(inp)
    res = run_bass_kernel_spmd(nc, maps, core_ids=list(range(8)))
    out = np.empty((4, 4096, 1024), np.float32)
    for core in range(8):
        b, hf = core // 2, core % 2
        out[b, hf * NT:(hf + 1) * NT, :] = res.results[core]["outT"].T
    return out


def kernel(**inputs):
    return run(inputs, "full")
```

```python
import numpy as np
from contextlib import ExitStack
import concourse.bass as bass
import concourse.mybir as mybir
from concourse.bass_utils import run_bass_kernel_spmd

F32, BF16 = mybir.dt.float32, mybir.dt.bfloat16
AF = mybir.ActivationFunctionType
ALU = mybir.AluOpType
AX = mybir.AxisListType
EPS = 1e-6
NT = 2048
NCH = NT // 128
NEXP = 65
NDUMMY = 16

_VOFF = {}
_o = 0
for _n, _w in (("n1g", 8), ("n2g", 8), ("fg", 8), ("modb", 48), ("convw", 64), ("convb", 16), ("dtb", 16), ("alog", 16),
               ("dskip", 16), ("sinks", 16), ("rbias", 64), ("wog", 16), ("flag", 1), ("cT", 8)):
    _VOFF[_n] = (_o, _w)
    _o += _w
NV = _o


class _Rec:
    def __init__(self):
        self.call = None

    def __getattr__(self, name):
        def f(*a, **k):
            self.call = (name, a, k)
            return self
        return f


class Prog:
    def __init__(self, nc, es):
        self.nc, self.es = nc, es
        self.names = ("pe", "dve", "act", "pool", "sp")
        self.semh, self.cnt = {}, {}
        for k in self.names:
            self.semh[k] = es.enter_context(nc.semaphore("c_" + k))
            self.cnt[k] = 0
        self.seen = {k: {} for k in self.names}
        self.lastw, self.readers = {}, {}
        self.ops = {k: [] for k in self.names}

    def _deps(self, eng, r, w):
        deps = {}

        def add(d):
            if d is None:
                return
            s, v = d
            if s == eng and eng in ("pe", "sp"):
                return
            if deps.get(s, 0) < v:
                deps[s] = v
        for k in r:
            add(self.lastw.get(k))
        for k in w:
            add(self.lastw.get(k))
            for s, v in self.readers.get(k, {}).items():
                add((s, v))
        out = []
        for s, v in deps.items():
            if self.seen[eng].get(s, 0) < v:
                self.seen[eng][s] = v
                out.append((s, v))
        return out

    def _mark(self, me, r, w):
        for k in r:
            self.readers.setdefault(k, {})[me[0]] = me[1]
        for k in w:
            self.lastw[k] = me
            self.readers[k] = {}

    def op(self, eng, fn, r=(), w=()):
        rec = _Rec()
        fn(rec)
        name_, a_, k_ = rec.call
        fn = (lambda e, name_=name_, a_=a_, k_=k_: getattr(e, name_)(*a_, **k_))
        waits = self._deps(eng, r, w)
        self.cnt[eng] += 1
        self._mark((eng, self.cnt[eng]), r, w)
        self.ops[eng].append((waits, fn, (eng, 1)))

    def dma(self, q, out, in_, r=(), w=(), sem="d"):
        if sem not in self.semh:
            self.semh[sem] = self.es.enter_context(self.nc.semaphore("d_" + sem))
            self.cnt[sem] = 0
        waits = self._deps(q, r, w)
        self.cnt[sem] += 16
        self._mark((sem, self.cnt[sem]), r, w)
        self.ops[q].append((waits, (lambda e, o=out, i=in_: e.dma_start(out=o, in_=i)), (sem, 16)))

    def dma_group(self, q, pairs, r=(), w=(), sem="d"):
        if sem not in self.semh:
            self.semh[sem] = self.es.enter_context(self.nc.semaphore("d_" + sem))
            self.cnt[sem] = 0
        waits = self._deps(q, r, w)
        for i, (o, i_) in enumerate(pairs):
            self.cnt[sem] += 16
            self.ops[q].append((waits if i == 0 else [], (lambda e, o=o, i_=i_: e.dma_start(out=o, in_=i_)), (sem, 16)))
        self._mark((sem, self.cnt[sem]), r, w)

    def barrier(self):
        tgt = dict(self.cnt)
        for eng in self.names:
            waits = []
            for s, v in tgt.items():
                if v > 0 and self.seen[eng].get(s, 0) < v and not (s == eng and eng in ("pe", "sp")):
                    self.seen[eng][s] = v
                    waits.append((s, v))
            if waits:
                self.ops[eng].append((waits, None, None))
        self.lastw, self.readers = {}, {}

    def final_wait(self, eng, sem):
        self.ops[eng].append(([(sem, self.cnt[sem])], None, None))

    def emit(self):
        block = self.es.enter_context(self.nc.Block())
        decs = dict(pe=block.tensor, dve=block.vector, act=block.scalar, pool=block.gpsimd, sp=block.sync)
        for name in self.names:
            ops = self.ops[name]

            def body(e, ops=ops):
                for waits, fn, inc in ops:
                    for ws, wv in waits:
                        e.wait_ge(self.semh[ws], wv)
                    if fn is not None:
                        fn(e).then_inc(self.semh[inc[0]], inc[1])
            decs[name](body)


def build_nc(stage="full"):
    nc = bass.Bass("TRN2", target_bir_lowering=False)
    D = lambda n, sh: nc.dram_tensor(n, sh, F32, kind="ExternalInput").ap()
    xoT = D("xoT", [1024, NT])
    xpT = D("xpT", [1024, NT])
    vec = D("vec", [128, NV])
    modw = D("modw", [128, 8, 6144])
    cbTd = D("cbT", [16, 128])
    w1d = D("w1", [128, 8, 3088])
    w2d = D("w2", [128, 8, 1792])
    wod = D("wo", [128, 16, 1024])
    wrd = D("wr", [128, 8, 64])
    biasd = D("biasT", [128, 16 * 256])
    egd = D("eg", [NEXP, 128, 8 * 256])
    eud = D("eu", [NEXP, 128, 8 * 256])
    edd = D("ed", [NEXP, 128, 2 * 1024])
    outT = nc.dram_tensor("outT", [1024, NT], F32, kind="ExternalOutput").ap()
    xo3 = xoT.rearrange("(c p) t -> p c t", p=128)
    xp3 = xpT.rearrange("(c p) t -> p c t", p=128)
    out3 = outT.rearrange("(c p) t -> p c t", p=128)

    with ExitStack() as es:
        P = Prog(nc, es)
        SB = lambda n, sh, dt, st=es: st.enter_context(nc.sbuf_tensor(n, sh, dt))
        PS = es.enter_context(nc.psum_tensor("PS", [128, 4096], F32))
        PSB = PS[:].bitcast(BF16)

        def bank(b, n=512, off=0):
            return PS[:, b * 512 + off: b * 512 + off + n]

        def bankb(b, n=1024):
            return PSB[:, b * 1024: b * 1024 + n]
        pk = lambda *bs: [("ps", b) for b in bs]

        R = SB("R", [128, 8, NT], F32)
        V = SB("V", [128, NV], F32)
        U = SB("U", [128, 128], F32)
        SL = SB("SL", [128, 128], F32)
        idf = SB("idf", [128, 128], F32)
        idb = SB("idb", [128, 128], BF16)
        onf = SB("onf", [128, 128], F32)
        onb = SB("onb", [128, 128], BF16)
        MOD = SB("MOD", [128, 48], F32)
        A1 = SB("A1", [128, 8], F32)
        A2 = SB("A2", [128, 8], F32)
        g1bc = SB("g1bc", [128, 1024], F32)
        Abc = SB("Abc", [128, 16], F32)
        esink = SB("esink", [128, 16], F32)
        vs = lambda n, i=None: (V[:, _VOFF[n][0]:_VOFF[n][0] + _VOFF[n][1]] if i is None
                                else V[:, _VOFF[n][0] + i:_VOFF[n][0] + i + 1])

        P.dma("sp", V[:], vec[:, :], w=["V"], sem="v")
        P.op("pool", lambda e: e.memset(U[:], 1.0), w=["U"])
        P.op("pool", lambda e: e.affine_select(out=U[:], in_=U[:], pattern=[[1, 128]], compare_op=ALU.is_ge,
                                               fill=0.0, base=0, channel_multiplier=-1), r=["U"], w=["U"])
        P.op("pool", lambda e: e.memset(SL[:], 1.0), w=["SL"])
        P.op("pool", lambda e: e.affine_select(out=SL[:], in_=SL[:], pattern=[[-1, 128]], compare_op=ALU.is_ge,
                                               fill=0.0, base=-1, channel_multiplier=1), r=["SL"], w=["SL"])
        P.op("pool", lambda e: e.memset(idf[:], 1.0), w=["idf"])
        P.op("pool", lambda e: e.affine_select(out=idf[:], in_=idf[:], pattern=[[1, 128]], compare_op=ALU.is_equal,
                                               fill=0.0, base=0, channel_multiplier=-1), r=["idf"], w=["idf"])
        P.op("pool", lambda e: e.tensor_copy(out=idb[:], in_=idf[:]), r=["idf"], w=["idb"])
        P.op("pool", lambda e: e.memset(onf[:], 1.0), w=["onf"])
        P.op("pool", lambda e: e.memset(onb[:], 1.0), w=["onb"])

        def norm_h(xsrc, xkey, hT, hkey, Ag, sh_off, wk, n, ssb):
            sq, rs, hn = wk["sq"], wk["rs"], wk["hn"]
            hk = list(xkey) if wk.get("inplace") else ["hn"]
            P.op("act", lambda e: e.activation(out=sq[:, :, :n], in_=xsrc, func=AF.Square), r=xkey, w=["sq"])
            if wk.get("inplace"):
                P.op("pool", lambda e: e.tensor_tensor(out=hn[:, :, :n], in0=hn[:, :, :n],
                                                       in1=Ag[:].unsqueeze(2).broadcast_to([128, 8, n]), op=ALU.mult),
                     r=hk + ["A1", "A2"], w=hk)
            for c in range(8):
                P.op("pe", lambda e, c=c: e.matmul(bank(ssb, n), onb[:], sq[:, c, :n], start=(c == 0), stop=(c == 7)),
                     r=["sq", "onb"], w=pk(ssb))
            P.op("act", lambda e: e.activation(out=rs[:, :n], in_=bank(ssb, n), func=AF.Ln, bias=wk["eps"][:, 0:1],
                                               scale=1.0 / 1024), r=pk(ssb) + ["eps"], w=["rs"])
            P.op("act", lambda e: e.activation(out=rs[:, :n], in_=rs[:, :n], func=AF.Exp, scale=-0.5), r=["rs"], w=["rs"])
            if wk.get("inplace"):
                P.op("dve", lambda e: e.tensor_tensor(out=hn[:, :, :n], in0=hn[:, :, :n],
                                                      in1=rs[:, :n].unsqueeze(1).broadcast_to([128, 8, n]), op=ALU.mult),
                     r=hk + ["rs"], w=hk)
            else:
                P.op("dve", lambda e: e.tensor_tensor(out=hn[:, :, :n], in0=xsrc,
                                                      in1=rs[:, :n].unsqueeze(1).broadcast_to([128, 8, n]), op=ALU.mult),
                     r=xkey + ["rs"], w=hk)
                P.op("pool", lambda e: e.tensor_tensor(out=hn[:, :, :n], in0=hn[:, :, :n],
                                                       in1=Ag[:].unsqueeze(2).broadcast_to([128, 8, n]), op=ALU.mult),
                     r=hk + ["A1", "A2"], w=hk)
            P.op("dve", lambda e: e.tensor_tensor(out=hT, in0=hn[:, :, :n],
                                                  in1=MOD[:, sh_off:sh_off + 8].unsqueeze(2).broadcast_to([128, 8, n]),
                                                  op=ALU.add), r=hk + ["MOD"], w=hkey)

        with ExitStack() as e1:
            w1 = SB("w1s", [128, 8, 3088], BF16, e1)
            wo1 = SB("wo1", [128, 8, 1024], BF16, e1)
            with ExitStack() as esA:
                sc = SB("sc", [128, 8], BF16, esA)
                mw = [SB("mw%d" % i, [128, 8, 1024], BF16, esA) for i in range(4)]
                dg = SB("dg", [128, 128], F32, esA)
                P.op("act", lambda e: e.activation(out=sc[:], in_=vs("cT"), func=AF.Silu), r=["V"], w=["sc"])

                def load_mw(m):
                    P.dma_group("pool", [(mw[m % 4][:, c, :], modw[:, c, m * 1024:(m + 1) * 1024]) for c in range(8)],
                                w=[("mw", m % 4)], sem="mw%d" % (m % 4))
                for m in range(4):
                    load_mw(m)
                for m in range(6):
                    s = m % 4
                    for j in range(8):
                        col = m * 8 + j
                        for c in range(8):
                            P.op("pe", lambda e, s=s, j=j, c=c, col=col: e.matmul(
                                bank(0, 1, col), mw[s][:, c, j * 128:(j + 1) * 128], sc[:, c:c + 1],
                                start=(c == 0), stop=(c == 7)), r=[("mw", s), "sc"], w=pk(0))
                    if m + 4 < 6:
                        load_mw(m + 4)
                    if m == 1:
                        P.dma_group("pool", [(w1[:, c, :], w1d[:, c, :]) for c in range(8)], w=["w1"], sem="w1")
                        P.dma("pool", wo1[:], wod[:, 0:8, :], w=["wo1"], sem="wo1")
                P.op("dve", lambda e: e.tensor_tensor(out=MOD[:], in0=bank(0, 48), in1=vs("modb"), op=ALU.add),
                     r=pk(0) + ["V"], w=["MOD"])
                P.op("dve", lambda e: e.scalar_tensor_tensor(out=A1[:], in0=MOD[:, 8:16], scalar=1.0, in1=vs("n1g"),
                                                             op0=ALU.add, op1=ALU.mult), r=["MOD", "V"], w=["A1"])
                P.op("dve", lambda e: e.scalar_tensor_tensor(out=A2[:], in0=MOD[:, 32:40], scalar=1.0, in1=vs("n2g"),
                                                             op0=ALU.add, op1=ALU.mult), r=["MOD", "V"], w=["A2"])
                for gi, (gb, o) in enumerate(((g1bc, 16),)):
                    for c in range(8):
                        P.op("dve", lambda e, c=c, o=o: e.tensor_scalar(out=dg[:], in0=idf[:], scalar1=MOD[:, o + c:o + c + 1],
                                                                        scalar2=None, op0=ALU.mult),
                             r=["MOD", "idf"], w=["dg"])
                        P.op("pe", lambda e, c=c, gi=gi: e.matmul(bank(1 + 2 * gi + c // 4, 128, (c % 4) * 128), onf[:], dg[:],
                                                                  start=True, stop=True),
                             r=["onf", "dg"], w=pk(1 + 2 * gi + c // 4))
                    P.op("dve", lambda e, gb=gb, gi=gi: e.tensor_copy(out=gb[:], in_=PS[:, (1 + 2 * gi) * 512:(3 + 2 * gi) * 512]),
                         r=pk(1 + 2 * gi, 2 + 2 * gi), w=["gbc%d" % gi])
                P.op("act", lambda e: e.activation(out=Abc[:], in_=vs("alog"), func=AF.Exp), r=["V"], w=["Abc"])
                P.op("dve", lambda e: e.tensor_scalar(out=Abc[:], in0=Abc[:], scalar1=-1.0, scalar2=None, op0=ALU.mult),
                     r=["Abc"], w=["Abc"])
                P.op("act", lambda e: e.activation(out=esink[:], in_=vs("sinks"), func=AF.Exp), r=["V"], w=["esink"])
                P.barrier()
            P.dma_group("sp", [(R[:, c, :], xo3[:, c, :]) for c in range(8)], w=[("R", c) for c in range(8)], sem="r")

            xc = [SB("xc%d" % i, [128, 8, 128], F32, e1) for i in range(2)]
            wk = dict(sq=SB("sq1", [128, 8, 128], BF16, e1), rs=SB("rs1", [128, 128], F32, e1),
                      eps=SB("eps1", [128, 1], F32, e1), inplace=True)
            hTs = [SB("hT1%d" % i, [128, 8, 128], BF16, e1) for i in range(2)]
            pre = SB("pre", [128, 16, 131], BF16, e1)
            cdiag = SB("cdiag", [128, 16, 4, 128], BF16, e1)
            cbT = SB("cbTs", [16, 128], BF16, e1)
            P.dma("pool", cbT[:], cbTd[:, :], w=["cbT"], sem="cbT")
            xbcT = SB("xbcT", [128, 16, 128], BF16, e1)
            sm = SB("sm1", [128, 16, 12], F32, e1)
            S32 = SB("S32", [128, 1024], F32, e1)
            Sb = SB("Sb", [128, 1024], BF16, e1)
            Xd = SB("Xd", [128, 16, 64], BF16, e1)
            Xdd = SB("Xdd", [128, 16, 64], BF16, e1)
            Btok = SB("Btok", [128, 4, 128], BF16, e1)
            rhsA = SB("rhsA", [128, 16, 128], F32, e1)
            Lt = SB("Lt", [128, 16, 128], BF16, e1)
            CBm = SB("CBm", [128, 4, 128], BF16, e1)
            _rf = rhsA[:].rearrange("p h l -> p (h l)")
            y1 = _rf[:, 0:1024].rearrange("p (h d) -> p h d", h=16)
            y2 = _rf[:, 1024:2048].rearrange("p (h d) -> p h d", h=16)
            szt = SB("szt", [128, 1024], BF16, e1)
            ssq = SB("ssq1", [128, 4], F32, e1)
            yn = SB("yn", [128, 1024], BF16, e1)
            ymT = SB("ymT", [128, 8, 128], BF16, e1)
            SMK = lambda i: ("sm", i)
            smv = lambda i: sm[:, :, i]

            P.op("pool", lambda e: e.memset(wk["eps"][:], EPS), w=["eps"])
            for c in range(8):
                P.op("dve", lambda e, c=c: e.scalar_tensor_tensor(out=wo1[:, c, :], in0=wo1[:, c, :], scalar=vs("wog", c),
                                                                  in1=g1bc[:], op0=ALU.mult, op1=ALU.mult),
                     r=["wo1", "V", "gbc0"], w=["wo1"])
            for tl in range(16):
                for k in range(4):
                    P.op("dve", lambda e, tl=tl, k=k: e.tensor_scalar(out=cdiag[:, tl, k, :], in0=idf[:],
                                                                     scalar1=vs("convw", tl * 4 + k), scalar2=None,
                                                                     op0=ALU.mult), r=["idf", "V"], w=["cdiag"])
            P.op("pool", lambda e: e.memset(S32[:], 0.0), w=["S32"])
            P.op("pool", lambda e: e.memset(Sb[:], 0.0), w=["Sb"])
            P.op("pool", lambda e: e.memset(pre[:], 0.0), w=["pre"])

            def LOADX(t_):
                src_ = xo3 if t_ >= 16 else xp3
                tc_ = (t_ % 16) * 128
                P.dma("sp", xc[t_ % 2][:], src_[:, :, tc_:tc_ + 128], w=[("xc", t_ % 2)], sem="xc%d" % (t_ % 2))

            def NORM(t_):
                wk["hn"] = xc[t_ % 2]
                norm_h(xc[t_ % 2][:], [("xc", t_ % 2)], hTs[t_ % 2][:], [("hT", t_ % 2)], A1, 0, wk, 128, 0)
            LOADX(0)
            NORM(0)
            for t in range(32):
                own = t >= 16
                tc = (t % 16) * 128
                par = t % 2
                src = xo3 if own else xp3
                if t + 1 < 32:
                    LOADX(t + 1)
                hT, HK = hTs[par], ("hT", par)
                for c in range(8):
                    P.op("pe", lambda e, c=c: e.matmul(bank(5, 16), hT[:, c, :], w1[:, c, 3072:3088],
                                                       start=(c == 0), stop=(c == 7)), r=[HK, "w1"], w=pk(5))
                P.op("dve", lambda e: e.tensor_tensor(out=smv(0), in0=bank(5, 16), in1=vs("dtb"), op=ALU.add),
                     r=pk(5) + ["V"], w=[SMK(0)])
                P.op("dve", lambda e: e.tensor_scalar(out=smv(1), in0=smv(0), scalar1=-1.0, scalar2=None, op0=ALU.mult),
                     r=[SMK(0)], w=[SMK(1)])
                P.op("dve", lambda e: e.tensor_tensor(out=smv(1), in0=smv(1), in1=smv(0), op=ALU.max),
                     r=[SMK(0), SMK(1)], w=[SMK(1)])
                P.op("act", lambda e: e.activation(out=smv(2), in_=smv(1), func=AF.Exp, scale=-1.0),
                     r=[SMK(1)], w=[SMK(2)])
                P.op("act", lambda e: e.activation(out=smv(3), in_=smv(2), func=AF.Ln, bias=onf[:, 0:1]),
                     r=[SMK(2), "onf"], w=[SMK(3)])
                P.op("dve", lambda e: e.scalar_tensor_tensor(out=smv(4), in0=smv(0), scalar=0.0, in1=smv(3),
                                                             op0=ALU.max, op1=ALU.add), r=[SMK(0), SMK(3)], w=[SMK(4)])
                P.op("dve", lambda e: e.tensor_tensor(out=smv(5), in0=smv(4), in1=Abc[:], op=ALU.mult),
                     r=[SMK(4), "Abc"], w=[SMK(5)])
                cur = pre
                if t == 16:
                    P.op("pool", lambda e: e.tensor_scalar(out=pre[:, :, 0:3], in0=pre[:, :, 128:131],
                                                           scalar1=vs("flag", 0), scalar2=None, op0=ALU.mult),
                         r=["pre", "V"], w=["pre"])
                else:
                    P.op("pool", lambda e: e.tensor_copy(out=pre[:, :, 0:3], in_=pre[:, :, 128:131]),
                         r=["pre"], w=["pre"])
                for bg in range(4 if t >= 15 else 3):
                    b = 1 + bg % 2
                    for ti in range(4):
                        tl = bg * 4 + ti
                        for c in range(8):
                            P.op("pe", lambda e, b=b, ti=ti, tl=tl, c=c: e.matmul(
                                bank(b, 128, ti * 128), w1[:, c, tl * 128:(tl + 1) * 128], hT[:, c, :],
                                start=(c == 0), stop=(c == 7)), r=["w1", HK], w=pk(b))
                    eng = "act" if bg % 2 == 0 else "dve"
                    if eng == "act":
                        P.op("act", lambda e, b=b, bg=bg, cur=cur: e.activation(
                            out=cur[:, bg * 4:(bg + 1) * 4, 3:131], in_=bank(b).rearrange("p (a n) -> p a n", a=4),
                            func=AF.Copy), r=pk(b), w=["pre"])
                    else:
                        P.op("dve", lambda e, b=b, bg=bg, cur=cur: e.tensor_copy(
                            out=cur[:, bg * 4:(bg + 1) * 4, 3:131], in_=bank(b).rearrange("p (a n) -> p a n", a=4)),
                            r=pk(b), w=["pre"])
                if t + 1 < 32:
                    NORM(t + 1)
                P.op("pe", lambda e: e.matmul(bank(5, 16, 16), U[:], smv(5), start=True, stop=True),
                     r=["U", SMK(5)], w=pk(5))
                P.op("pe", lambda e: e.matmul(bank(5, 16, 32), onf[:], smv(5), start=True, stop=True),
                     r=["onf", SMK(5)], w=pk(5))
                P.op("dve", lambda e: e.tensor_copy(out=smv(6), in_=bank(5, 16, 16)), r=pk(5), w=[SMK(6)])
                P.op("act", lambda e: e.activation(out=smv(7), in_=bank(5, 16, 16), func=AF.Exp), r=pk(5), w=[SMK(7)])
                P.op("dve", lambda e: e.tensor_tensor(out=smv(10), in0=bank(5, 16, 32), in1=smv(6), op=ALU.subtract),
                     r=pk(5) + [SMK(6)], w=[SMK(10)])
                P.op("act", lambda e: e.activation(out=smv(8), in_=smv(10), func=AF.Exp), r=[SMK(10)], w=[SMK(8)])
                P.op("act", lambda e: e.activation(out=smv(9), in_=bank(5, 16, 32), func=AF.Exp), r=pk(5), w=[SMK(9)])
                if own:
                    for hf in range(2):
                        for c in range(8):
                            P.op("pe", lambda e, hf=hf, c=c: e.matmul(bank(hf), hT[:, c, :],
                                                                      w1[:, c, 2048 + hf * 512: 2048 + (hf + 1) * 512],
                                                                      start=(c == 0), stop=(c == 7)), r=[HK, "w1"], w=pk(hf))
                    P.op("act", lambda e: e.activation(out=szt[:], in_=PS[:, 0:1024], func=AF.Silu), r=pk(0, 1), w=["szt"])
                ntl = 16 if t >= 15 else 12
                for bg in range(ntl // 4):
                    b = 3 + bg % 2
                    for ti in range(4):
                        tl = bg * 4 + ti
                        P.op("pe", lambda e, b=b, ti=ti, tl=tl: e.matmul(
                            bank(b, 128, ti * 128), cbT[:], idb[0:16, tl:tl + 1].broadcast_to([16, 128]),
                            start=True, stop=False), r=["cbT", "idb"], w=pk(b))
                        for k in range(4):
                            P.op("pe", lambda e, b=b, ti=ti, tl=tl, k=k: e.matmul(
                                bank(b, 128, ti * 128), cdiag[:, tl, k, :], pre[:, tl, k:k + 128],
                                start=False, stop=(k == 3)), r=["cdiag", "pre"], w=pk(b))
                    P.op("act", lambda e, b=b, bg=bg: e.activation(
                        out=xbcT[:, bg * 4:(bg + 1) * 4, :], in_=bank(b).rearrange("p (a n) -> p a n", a=4),
                        func=AF.Silu), r=pk(b), w=["xbcT"])
                for c in range(8):
                    P.op("pe", lambda e, c=c: e.transpose(out=bankb(6, 128 * (c + 1))[:, c * 128:(c + 1) * 128],
                                                          in_=xbcT[:, c, :], identity=idb[:]),
                         r=["xbcT", "idb"], w=pk(6))
                for g in range(4):
                    P.op("pe", lambda e, g=g: e.transpose(out=bankb(7, 128 * (g + 1))[:, g * 128:(g + 1) * 128],
                                                          in_=xbcT[:, 8 + g, :], identity=idb[:]),
                         r=["xbcT", "idb"], w=pk(7))
                P.op("dve", lambda e: e.tensor_tensor(out=Xd[:], in0=bankb(6).rearrange("p (h d) -> p h d", h=16),
                                                      in1=smv(4).unsqueeze(2).broadcast_to([128, 16, 64]), op=ALU.mult),
                     r=pk(6) + [SMK(4)], w=["Xd"])
                P.op("act", lambda e: e.activation(out=Btok[:], in_=bankb(7, 512).rearrange("p (g n) -> p g n", g=4),
                                                   func=AF.Copy), r=pk(7), w=["Btok"])
                P.op("pool", lambda e: e.tensor_tensor(out=Xdd[:], in0=Xd[:],
                                                       in1=smv(8).unsqueeze(2).broadcast_to([128, 16, 64]), op=ALU.mult),
                     r=["Xd", SMK(8)], w=["Xdd"])
                if own:
                    P.op("dve", lambda e: e.tensor_tensor(out=rhsA[:], in0=U[:].unsqueeze(1).broadcast_to([128, 16, 128]),
                                                          in1=smv(5).unsqueeze(2).broadcast_to([128, 16, 128]),
                                                          op=ALU.mult), r=["U", SMK(5)], w=["rhsA"])
                    for q4 in range(4):
                        P.op("pe", lambda e, q4=q4: e.matmul(bank(q4), SL[:], rhsA[:, q4 * 4:(q4 + 1) * 4, :],
                                                             start=True, stop=True), r=["SL", "rhsA"], w=pk(q4))
                    P.op("dve", lambda e: e.tensor_tensor(out=y2, in0=bankb(6).rearrange("p (h d) -> p h d", h=16),
                                                          in1=vs("dskip").unsqueeze(2).broadcast_to([128, 16, 64]),
                                                          op=ALU.mult), r=pk(6) + ["V"], w=["rhsA"])
                    P.op("act", lambda e: e.activation(out=Lt[:], in_=PS[:, 0:2048].rearrange("p (h l) -> p h l", h=16),
                                                       func=AF.Exp), r=pk(0, 1, 2, 3), w=["Lt"])
                    for g in range(4):
                        P.op("pe", lambda e, g=g: e.matmul(bank(7, 128, g * 128), xbcT[:, 8 + g, :], xbcT[:, 12 + g, :],
                                                           start=True, stop=True), r=["xbcT"], w=pk(7))
                    P.op("dve", lambda e: e.tensor_tensor(out=CBm[:], in0=bank(7).rearrange("p (g l) -> p g l", g=4),
                                                          in1=U[:].unsqueeze(1).broadcast_to([128, 4, 128]), op=ALU.mult),
                         r=pk(7) + ["U"], w=["CBm"])
                    P.op("dve", lambda e: e.tensor_tensor(
                        out=Lt[:].rearrange("p (g r) l -> p g r l", g=4), in0=Lt[:].rearrange("p (g r) l -> p g r l", g=4),
                        in1=CBm[:].unsqueeze(2).broadcast_to([128, 4, 4, 128]), op=ALU.mult), r=["Lt", "CBm"], w=["Lt"])
                    for h in range(16):
                        P.op("pe", lambda e, h=h: e.matmul(PS[:, 2048 + h * 64: 2048 + (h + 1) * 64], Lt[:, h, :], Xd[:, h, :],
                                                           start=True, stop=True), r=["Lt", "Xd"], w=pk(4 + h // 8))
                    for h in range(16):
                        P.op("pe", lambda e, h=h: e.matmul(PS[:, 3072 + h * 64: 3072 + (h + 1) * 64], xbcT[:, 12 + h // 4, :],
                                                           Sb[:, h * 64:(h + 1) * 64], start=True, stop=True),
                             r=["xbcT", "Sb"], w=pk(6 + h // 8))
                    for dmy in range(NDUMMY):
                        P.op("pe", lambda e, dmy=dmy: e.matmul(bank(dmy % 2), idb[:], w1[:, dmy % 8, 0:512], start=True, stop=True),
                             r=["idb", "w1"], w=pk(dmy % 2))
                    P.op("dve", lambda e: e.tensor_tensor(out=y1, in0=PS[:, 3072:4096].rearrange("p (h d) -> p h d", h=16),
                                                          in1=smv(7).unsqueeze(2).broadcast_to([128, 16, 64]), op=ALU.mult),
                         r=pk(6, 7) + [SMK(7)], w=["rhsA"])
                    P.op("dve", lambda e: e.tensor_tensor(out=y1, in0=PS[:, 2048:3072].rearrange("p (h d) -> p h d", h=16),
                                                          in1=y1, op=ALU.add), r=pk(4, 5) + ["rhsA"], w=["rhsA"])
                    P.op("dve", lambda e: e.tensor_tensor(out=y2, in0=y2, in1=y1, op=ALU.add),
                         r=["rhsA"], w=["rhsA"])
                    y1f = _rf[:, 0:1024]
                    P.op("dve", lambda e, y1f=y1f: e.tensor_tensor(out=y1f, in0=szt[:], in1=_rf[:, 1024:2048],
                                                                   op=ALU.mult), r=["szt", "rhsA"], w=["rhsA"])
                    P.op("pool", lambda e: e.memset(ssq[:], 0.0), w=["ssq"])
                    for g in range(4):
                        P.op("act", lambda e, g=g, y1f=y1f: e.activation(
                            out=Lt[:, 0:2, :].rearrange("p a l -> p (a l)"), in_=y1f[:, g * 256:(g + 1) * 256],
                            func=AF.Square, accum_out=ssq[:, g:g + 1]), r=["rhsA", "ssq"], w=["Lt", "ssq"])
                    P.op("act", lambda e: e.activation(out=ssq[:], in_=ssq[:], func=AF.Ln, bias=wk["eps"][:, 0:1],
                                                       scale=1.0 / 256), r=["ssq", "eps"], w=["ssq"])
                    P.op("act", lambda e: e.activation(out=ssq[:], in_=ssq[:], func=AF.Exp, scale=-0.5), r=["ssq"], w=["ssq"])
                    P.op("dve", lambda e: e.tensor_tensor(out=yn[:].rearrange("p (g f) -> p g f", g=4),
                                                          in0=y1f.rearrange("p (g f) -> p g f", g=4),
                                                          in1=ssq[:].unsqueeze(2).broadcast_to([128, 4, 256]), op=ALU.mult),
                         r=["rhsA", "ssq"], w=["yn"])
                    for c in range(8):
                        P.op("pe", lambda e, c=c: e.transpose(out=bankb(2, 128 * (c + 1))[:, c * 128:(c + 1) * 128],
                                                              in_=yn[:, c * 128:(c + 1) * 128], identity=idb[:]),
                             r=["yn", "idb"], w=pk(2))
                    P.op("act", lambda e: e.activation(out=ymT[:], in_=bankb(2).rearrange("p (c l) -> p c l", c=8),
                                                       func=AF.Copy), r=pk(2), w=["ymT"])
                    out_proj(P, bank, wo1, "wo1", ymT, "ymT", R, t - 16, (3, 4))
                if t < 31:
                    for h in range(16):
                        P.op("pe", lambda e, h=h: e.matmul(PS[:, 2560 + h * 64: 2560 + (h + 1) * 64], Btok[:, h // 4, :],
                                                           Xdd[:, h, :], start=True, stop=True),
                             r=["Btok", "Xdd"], w=pk(5 + h // 8))
                    P.op("pool", lambda e: e.tensor_tensor(out=S32[:].rearrange("p (h d) -> p h d", h=16),
                                                           in0=S32[:].rearrange("p (h d) -> p h d", h=16),
                                                           in1=smv(9).unsqueeze(2).broadcast_to([128, 16, 64]),
                                                           op=ALU.mult), r=["S32", SMK(9)], w=["S32"])
                    P.op("dve", lambda e: e.tensor_tensor(out=S32[:], in0=PS[:, 2560:3584], in1=S32[:], op=ALU.add),
                         r=pk(5, 6) + ["S32"], w=["S32"])
                    if t == 15:
                        P.op("dve", lambda e: e.tensor_scalar(out=S32[:], in0=S32[:], scalar1=vs("flag", 0), scalar2=None,
                                                              op0=ALU.mult), r=["S32", "V"], w=["S32"])
                    P.op("act", lambda e: e.activation(out=Sb[:], in_=S32[:], func=AF.Copy), r=["S32"], w=["Sb"])
            P.barrier()

        if stage == "ssd":
            return finish(nc, P, R, out3, es)

        with ExitStack() as eW:
            wg = [SB("wg%d" % i, [128, 8, 256], BF16, eW) for i in range(3)]
            wu = [SB("wu%d" % i, [128, 8, 256], BF16, eW) for i in range(3)]
            wd = [SB("wd%d" % i, [128, 2, 1024], BF16, eW) for i in range(3)]
            wr = SB("wrs", [128, 8, 64], F32, eW)

            def load_expert_dma(e_):
                s = e_ % 3
                P.dma_group("pool", [(wg[s][:].rearrange("p c n -> p (c n)"), egd[e_, :, :]),
                                     (wu[s][:].rearrange("p c n -> p (c n)"), eud[e_, :, :]),
                                     (wd[s][:].rearrange("p c n -> p (c n)"), edd[e_, :, :])],
                            w=[("wg", s), ("wu", s), ("wd", s)], sem="ew%d" % s)
            with ExitStack() as e2:
                w2 = SB("w2s", [128, 8, 1792], BF16, e2)
                wo2 = SB("wo2", [128, 8, 1024], BF16, e2)
                bT = SB("bT", [128, 16, 256], BF16, e2)
                xc = [SB("xd%d" % i, [128, 8, 128], F32, e2) for i in range(2)]
                wk = dict(sq=SB("sq2", [128, 8, 128], BF16, e2), rs=SB("rs2", [128, 128], F32, e2),
                          eps=SB("eps2", [128, 1], F32, e2), inplace=True)
                hT = SB("hT2", [128, 8, 128], BF16, e2)
                kT = [SB("kT%d" % i, [128, 4, 128], BF16, e2) for i in range(3)]
                qT = [SB("qT%d" % i, [128, 8, 128], BF16, e2) for i in range(2)]
                Va = [SB("Va%d" % i, [128, 4, 65], BF16, e2) for i in range(3)]
                PT = SB("PT", [128, 16, 2, 128], BF16, e2)
                den = SB("den", [128, 16], F32, e2)
                on = SB("on", [128, 16, 64], F32, e2)
                junk = SB("junk2", [128, 1024], F32, e2)
                ss1 = SB("ss1", [128, 1], F32, e2)
                ya = SB("ya", [128, 1024], BF16, e2)
                yaT = SB("yaT", [128, 8, 128], BF16, e2)

                P.op("pool", lambda e: e.memset(wk["eps"][:], EPS), w=["eps"])
                P.dma_group("pool", [(w2[:, c, :], w2d[:, c, :]) for c in range(8)], w=["w2"], sem="w2")
                P.dma("pool", wo2[:], wod[:, 8:16, :], w=["wo2"], sem="wo2")
                P.dma("pool", bT[:].rearrange("p h n -> p (h n)"), biasd[:, :], w=["bT"], sem="bT")
                load_expert_dma(0)
                load_expert_dma(1)
                P.dma("sp", wr[:], wrd[:, :, :], w=["wr"], sem="wr")
                for c in range(8):
                    P.op("dve", lambda e, c=c: e.scalar_tensor_tensor(out=wo2[:, c, :], in0=wo2[:, c, :], scalar=vs("wog", 8 + c),
                                                                      in1=g1bc[:], op0=ALU.mult, op1=ALU.mult),
                         r=["wo2", "V", "gbc0"], w=["wo2"])
                for i in range(3):
                    P.op("pool", lambda e, i=i: e.memset(Va[i][:], 1.0), w=[("Va", i)])

                def F(t):
                    own = t >= 0
                    par = t % 2
                    p3 = t % 3
                    if own:
                        P.dma("sp", xc[par][:], xo3[:, :, t * 128:(t + 1) * 128], w=[("xc", par)], sem="xc%d" % par)
                    else:
                        P.dma("sp", xc[par][:], xp3[:, :, NT - 128:NT], w=[("xc", par)], sem="xc%d" % par)
                    wk["hn"] = xc[par]
                    norm_h(xc[par][:], [("xc", par)], hT[:], ["hT"], A1, 0, wk, 128, 0)
                    for kh in range(4):
                        for c in range(8):
                            P.op("pe", lambda e, kh=kh, c=c: e.matmul(bank(1, 128, kh * 128), w2[:, c, 1024 + kh * 128:1024 + (kh + 1) * 128],
                                                                      hT[:, c, :], start=(c == 0), stop=(c == 7)),
                                 r=["w2", "hT"], w=pk(1))
                    P.op("act", lambda e, p3=p3: e.activation(out=kT[p3][:], in_=bank(1).rearrange("p (a n) -> p a n", a=4),
                                                                func=AF.Copy), r=pk(1), w=[("kT", p3)])
                    for c in range(8):
                        P.op("pe", lambda e, c=c: e.matmul(bank(0, 256, 128), hT[:, c, :], w2[:, c, 1536:1792],
                                                           start=(c == 0), stop=(c == 7)), r=["w2", "hT"], w=pk(0))
                    P.op("dve", lambda e, p3=p3: e.tensor_copy(out=Va[p3][:, :, 0:64],
                                                                in_=bank(0, 256, 128).rearrange("p (k d) -> p k d", k=4)),
                         r=pk(0), w=[("Va", p3)])
                    if not own:
                        return
                    for bg in range(2):
                        b = 2
                        for ti in range(4):
                            tl = bg * 4 + ti
                            for c in range(8):
                                P.op("pe", lambda e, b=b, ti=ti, tl=tl, c=c: e.matmul(
                                    bank(b, 128, ti * 128), w2[:, c, tl * 128:(tl + 1) * 128], hT[:, c, :],
                                    start=(c == 0), stop=(c == 7)), r=["w2", "hT"], w=pk(b))
                        P.op("act", lambda e, b=b, bg=bg, par=par: e.activation(out=qT[par][:, bg * 4:(bg + 1) * 4, :],
                                                                       in_=bank(b).rearrange("p (a n) -> p a n", a=4),
                                                                       func=AF.Copy, scale=0.125), r=pk(b), w=[("qT", par)])
                def B(t):
                    par = t % 2
                    p3, q3 = t % 3, (t - 1) % 3
                    for hp in range(8):
                        b = 5 + hp % 2
                        for hh in range(2):
                            h = 2 * hp + hh
                            kh = h // 4
                            lo, hi = hh * 64, (hh + 1) * 64
                            P.op("pe", lambda e, b=b, hh=hh, h=h: e.matmul(bank(b, 256, hh * 256), idb[:], bT[:, h, :],
                                                                           start=True, stop=False), r=["idb", "bT"], w=pk(b))
                            P.op("pe", lambda e, b=b, hh=hh, kh=kh, lo=lo, hi=hi, hp=hp: e.matmul(
                                bank(b, 128, hh * 256), kT[q3][lo:hi, kh, :], qT[par][lo:hi, hp, :], start=False, stop=False),
                                r=[("kT", q3), ("qT", par)], w=pk(b))
                            P.op("pe", lambda e, b=b, hh=hh, kh=kh, lo=lo, hi=hi, hp=hp: e.matmul(
                                bank(b, 128, hh * 256 + 128), kT[p3][lo:hi, kh, :], qT[par][lo:hi, hp, :], start=False, stop=True),
                                r=[("kT", p3), ("qT", par)], w=pk(b))
                        P.op("act", lambda e, b=b, hp=hp: e.activation(
                            out=PT[:, 2 * hp:2 * hp + 2, :, :].rearrange("p h b q -> p (h b q)"), in_=bank(b), func=AF.Exp),
                            r=pk(b), w=["PT"])
                    if t == 0:
                        P.op("pool", lambda e: e.tensor_scalar(out=PT[:, :, 0, :], in0=PT[:, :, 0, :], scalar1=vs("flag", 0),
                                                               scalar2=None, op0=ALU.mult), r=["PT", "V"], w=["PT"])
                    hb = ((0, 6, 3), (6, 12, 4), (12, 16, 7))
                    for h0, h1, b in hb:
                        for h in range(h0, h1):
                            kh = h // 4
                            o_ = (h - h0) * 65
                            P.op("pe", lambda e, b=b, o_=o_, h=h, kh=kh: e.matmul(bank(b, 65, o_), PT[:, h, 0, :], Va[q3][:, kh, :],
                                                                                  start=True, stop=False),
                                 r=["PT", ("Va", q3)], w=pk(b))
                            P.op("pe", lambda e, b=b, o_=o_, h=h, kh=kh: e.matmul(bank(b, 65, o_), PT[:, h, 1, :], Va[p3][:, kh, :],
                                                                                  start=False, stop=True),
                                 r=["PT", ("Va", p3)], w=pk(b))
                    for dmy in range(12):
                        P.op("pe", lambda e, dmy=dmy: e.matmul(bank(6), idb[:], w2[:, dmy % 8, 0:512], start=True, stop=True),
                             r=["idb", "w2"], w=pk(6))
                    for h0, h1, b in hb:
                        n = h1 - h0
                        pv = bank(b, n * 65).rearrange("p (h d) -> p h d", h=n)
                        P.op("dve", lambda e, pv=pv, h0=h0, h1=h1: e.tensor_tensor(out=den[:, h0:h1], in0=pv[:, :, 64],
                                                                                   in1=esink[:, h0:h1], op=ALU.add),
                             r=pk(b) + ["esink"], w=["den"])
                    P.op("dve", lambda e: e.reciprocal(out=den[:], in_=den[:]), r=["den"], w=["den"])
                    for h0, h1, b in hb:
                        n = h1 - h0
                        pv = bank(b, n * 65).rearrange("p (h d) -> p h d", h=n)
                        P.op("dve", lambda e, pv=pv, h0=h0, h1=h1, n=n: e.tensor_tensor(
                            out=on[:, h0:h1, :], in0=pv[:, :, 0:64], in1=den[:, h0:h1].unsqueeze(2).broadcast_to([128, n, 64]),
                            op=ALU.mult), r=pk(b) + ["den"], w=["on"])
                    P.op("pool", lambda e: e.memset(ss1[:], 0.0), w=["ss1"])
                    P.op("act", lambda e: e.activation(out=junk[:], in_=on[:].rearrange("p h d -> p (h d)"), func=AF.Square,
                                                       accum_out=ss1[:, 0:1]), r=["on", "ss1"], w=["junk", "ss1"])
                    P.op("act", lambda e: e.activation(out=ss1[:], in_=ss1[:], func=AF.Ln, bias=wk["eps"][:, 0:1],
                                                       scale=1.0 / 1024), r=["ss1", "eps"], w=["ss1"])
                    P.op("act", lambda e: e.activation(out=ss1[:], in_=ss1[:], func=AF.Exp, scale=-0.5), r=["ss1"], w=["ss1"])
                    P.op("dve", lambda e: e.tensor_scalar(out=ya[:], in0=on[:].rearrange("p h d -> p (h d)"), scalar1=ss1[:, 0:1],
                                                          scalar2=None, op0=ALU.mult), r=["on", "ss1"], w=["ya"])
                    for c in range(8):
                        P.op("pe", lambda e, c=c: e.transpose(out=bankb(5, 128 * (c + 1))[:, c * 128:(c + 1) * 128],
                                                              in_=ya[:, c * 128:(c + 1) * 128], identity=idb[:]),
                             r=["ya", "idb"], w=pk(5))
                    P.op("act", lambda e: e.activation(out=yaT[:], in_=bankb(5).rearrange("p (c l) -> p c l", c=8),
                                                       func=AF.Copy), r=pk(5), w=["yaT"])
                    out_proj(P, bank, wo2, "wo2", yaT, "yaT", R, t, (6, 7))
                F(-1)
                F(0)
                for t in range(16):
                    if t + 1 < 16:
                        F(t + 1)
                    B(t)
                P.barrier()

            if stage == "mixer":
                return finish(nc, P, R, out3, es)

            with ExitStack() as e3:
                h2T = SB("h2T", [128, 8, NT], BF16, e3)
                gT = SB("gT", [128, NT], BF16, e3)
                selE = SB("selE", [128, NEXP, 128], BF16, e3)
                wk = dict(sq=SB("sq3", [128, 8, 512], BF16, e3), rs=SB("rs3", [128, 512], F32, e3),
                          hn=SB("hn3", [128, 8, 512], F32, e3), eps=SB("eps3", [128, 1], F32, e3))
                rt = SB("rt", [128, 12, 64], F32, e3)
                gsb = SB("gsb", [128, 512], F32, e3)
                sg = [SB("sg%d" % i, [128, 512], BF16, e3) for i in range(2)]
                tt = [SB("tt%d" % i, [128, 512], BF16, e3) for i in range(2)]
                uu = [[SB("uu%d%d" % (i, f), [128, 512], BF16, e3) for f in range(2)] for i in range(2)]

                g2bc = SB("g2bc", [128, 1024], F32, e3)
                dg2 = SB("dg2", [128, 128], F32, e3)
                for c in range(8):
                    P.op("dve", lambda e, c=c: e.tensor_scalar(out=dg2[:], in0=idf[:], scalar1=MOD[:, 40 + c:41 + c],
                                                               scalar2=None, op0=ALU.mult), r=["MOD", "idf"], w=["dg2"])
                    P.op("pe", lambda e, c=c: e.matmul(bank(3 + c // 4, 128, (c % 4) * 128), onf[:], dg2[:], start=True, stop=True),
                         r=["onf", "dg2"], w=pk(3 + c // 4))
                P.op("dve", lambda e: e.tensor_copy(out=g2bc[:], in_=PS[:, 3 * 512:5 * 512]), r=pk(3, 4), w=["gbc1"])
                P.op("pool", lambda e: e.memset(wk["eps"][:], EPS), w=["eps"])
                P.op("pool", lambda e: e.memset(selE[:], 1.0), w=["selE"])
                P.op("pool", lambda e: e.affine_select(out=selE[:], in_=selE[:], pattern=[[1, NEXP], [0, 128]],
                                                       compare_op=ALU.is_equal, fill=0.0, base=0, channel_multiplier=-1),
                     r=["selE"], w=["selE"])
                P.op("pool", lambda e: e.memset(gT[:], 0.0), w=[("gT", g_) for g_ in range(4)])
                P.op("pool", lambda e: e.memset(gT[64:65, :], 1.0), w=[("gT", g_) for g_ in range(4)])

                def scale_expert(e_):
                    s = e_ % 3
                    P.op("pool", lambda e, s=s: e.tensor_tensor(out=wd[s][:], in0=wd[s][:],
                                                                in1=g2bc[:].unsqueeze(1).broadcast_to([128, 2, 1024]), op=ALU.mult),
                         r=[("wd", s), "gbc1"], w=[("wd", s)])

                def load_expert(e_):
                    load_expert_dma(e_)
                    scale_expert(e_)
                scale_expert(0)
                scale_expert(1)

                rv = lambda i, n=64: rt[:, i, 0:n]
                RK = lambda i: ("rt", i)
                def PRO(tg):
                    ts_ = slice(tg * 512, (tg + 1) * 512)
                    norm_h(R[:, :, ts_], [("R", c, tg) for c in range(8)], h2T[:, :, ts_], [("h2T", tg)], A2, 24, wk, 512, 5)
                    P.op("dve", lambda e: e.tensor_tensor(out=wk["hn"][:], in0=wk["hn"][:],
                                                          in1=MOD[:, 24:32].unsqueeze(2).broadcast_to([128, 8, 512]), op=ALU.add),
                         r=["hn", "MOD"], w=["hn"])
                    for tl in range(4):
                        for c in range(8):
                            P.op("pe", lambda e, tl=tl, c=c: e.matmul(bank(6, 64), wk["hn"][:, c, tl * 128:(tl + 1) * 128], wr[:, c, :],
                                                                      start=(c == 0), stop=(c == 7)), r=["hn", "wr"], w=pk(6))
                        P.op("act", lambda e: e.activation(out=rv(0), in_=bank(6, 64), func=AF.Sigmoid), r=pk(6), w=[RK(0)])
                        P.op("dve", lambda e: e.tensor_tensor(out=rv(1), in0=rv(0), in1=vs("rbias"), op=ALU.add),
                             r=[RK(0), "V"], w=[RK(1)])
                        g3 = lambda ap: ap.rearrange("p (g e) -> p g e", e=8)
                        b3 = lambda ap: ap.unsqueeze(2).broadcast_to([128, 8, 8])
                        P.op("dve", lambda e: e.tensor_reduce(out=rv(6, 8), in_=g3(rv(1)), axis=AX.X, op=ALU.max), r=[RK(1)], w=[RK(6)])
                        P.op("dve", lambda e: e.tensor_tensor(out=g3(rv(2)), in0=g3(rv(1)), in1=b3(rv(6, 8)), op=ALU.is_equal),
                             r=[RK(1), RK(6)], w=[RK(2)])
                        P.op("dve", lambda e: e.scalar_tensor_tensor(out=rv(2), in0=rv(2), scalar=-1e9, in1=rv(1), op0=ALU.mult,
                                                                     op1=ALU.add), r=[RK(2), RK(1)], w=[RK(2)])
                        P.op("dve", lambda e: e.tensor_reduce(out=rv(7, 8), in_=g3(rv(2)), axis=AX.X, op=ALU.max), r=[RK(2)], w=[RK(7)])
                        P.op("dve", lambda e: e.tensor_tensor(out=rv(7, 8), in0=rv(7, 8), in1=rv(6, 8), op=ALU.add),
                             r=[RK(7), RK(6)], w=[RK(7)])
                        P.op("dve", lambda e: e.max(out=rv(8, 8), in_=rv(7, 8)), r=[RK(7)], w=[RK(8)])
                        P.op("dve", lambda e: e.tensor_scalar(out=rv(9, 8), in0=rv(7, 8), scalar1=rt[:, 8, 3:4], scalar2=None,
                                                              op0=ALU.is_ge), r=[RK(7), RK(8)], w=[RK(9)])
                        P.op("dve", lambda e: e.scalar_tensor_tensor(out=g3(rv(3)), in0=g3(rv(1)), scalar=2.0, in1=b3(rv(9, 8)),
                                                                     op0=ALU.add, op1=ALU.mult), r=[RK(1), RK(9)], w=[RK(3)])
                        P.op("dve", lambda e: e.max(out=rv(10, 8), in_=rv(3)), r=[RK(3)], w=[RK(10)])
                        P.op("dve", lambda e: e.tensor_scalar(out=rv(4), in0=rv(3), scalar1=rt[:, 10, 7:8], scalar2=None,
                                                              op0=ALU.is_ge), r=[RK(3), RK(10)], w=[RK(4)])
                        P.op("dve", lambda e: e.tensor_tensor(out=rv(4), in0=rv(4), in1=rv(0), op=ALU.mult),
                             r=[RK(4), RK(0)], w=[RK(4)])
                        P.op("dve", lambda e: e.tensor_reduce(out=rv(11, 1), in_=rv(4), axis=AX.X, op=ALU.add), r=[RK(4)], w=[RK(11)])
                        P.op("dve", lambda e: e.reciprocal(out=rv(11, 1), in_=rv(11, 1)), r=[RK(11)], w=[RK(11)])
                        P.op("dve", lambda e: e.tensor_scalar(out=rv(5), in0=rv(4), scalar1=rt[:, 11, 0:1], scalar2=2.5,
                                                              op0=ALU.mult, op1=ALU.mult), r=[RK(4), RK(11)], w=[RK(5)])
                        P.op("pe", lambda e: e.transpose(out=bank(7, 128)[0:64, :], in_=rv(5), identity=idf[:]),
                             r=[RK(5), "idf"], w=pk(7))
                        col = tg * 512 + tl * 128
                        P.op("act", lambda e, col=col: e.activation(out=gT[0:64, col:col + 128], in_=bank(7, 128)[0:64, :],
                                                                    func=AF.Copy), r=pk(7), w=[("gT", tg)])

                PRO(0)
                if stage != "norouted":
                    experts = list(range(NEXP))
                else:
                    experts = [64]
                steps = [(e_, tg) for e_ in experts for tg in range(4)]

                def down(i):
                    e_, tg = steps[i]
                    s = e_ % 3
                    for dtl in range(8):
                        b = 5 + dtl % 3
                        for f in range(2):
                            P.op("pe", lambda e, b=b, s=s, f=f, dtl=dtl, i=i: e.matmul(
                                bank(b), wd[s][:, f, dtl * 128:(dtl + 1) * 128], uu[i % 2][f][:], start=(f == 0), stop=(f == 1)),
                                r=[("wd", s), ("uu", i % 2, f)], w=pk(b))
                        P.op("dve", lambda e, b=b, dtl=dtl, tg=tg: e.tensor_tensor(
                            out=R[:, dtl, tg * 512:(tg + 1) * 512], in0=bank(b), in1=R[:, dtl, tg * 512:(tg + 1) * 512], op=ALU.add),
                            r=pk(b) + [("R", dtl, tg)], w=[("R", dtl, tg)])

                for i, (e_, tg) in enumerate(steps):
                    s = e_ % 3
                    if stage == "norouted" and i == 0:
                        P.barrier()
                        load_expert(64)
                    P.op("pe", lambda e, e_=e_, tg=tg: e.matmul(bank(0), selE[:, e_, :], gT[:, tg * 512:(tg + 1) * 512],
                                                                start=True, stop=True), r=["selE", ("gT", tg)], w=pk(0))
                    P.op("act", lambda e: e.activation(out=gsb[:], in_=bank(0), func=AF.Copy), r=pk(0), w=["gsb"])
                    for f in range(2):
                        bG, bU = 1 + 2 * f, 2 + 2 * f
                        for c in range(8):
                            P.op("pe", lambda e, bG=bG, s=s, c=c, f=f, tg=tg: e.matmul(
                                bank(bG), wg[s][:, c, f * 128:(f + 1) * 128], h2T[:, c, tg * 512:(tg + 1) * 512],
                                start=(c == 0), stop=(c == 7)), r=[("wg", s), ("h2T", tg)], w=pk(bG))
                        for c in range(8):
                            P.op("pe", lambda e, bU=bU, s=s, c=c, f=f, tg=tg: e.matmul(
                                bank(bU), wu[s][:, c, f * 128:(f + 1) * 128], h2T[:, c, tg * 512:(tg + 1) * 512],
                                start=(c == 0), stop=(c == 7)), r=[("wu", s), ("h2T", tg)], w=pk(bU))
                        P.op("act", lambda e, bG=bG, f=f: e.activation(out=sg[f][:], in_=bank(bG), func=AF.Silu),
                             r=pk(bG), w=[("sg", f)])
                        P.op("dve", lambda e, bU=bU, f=f: e.tensor_tensor(out=tt[f][:], in0=bank(bU), in1=gsb[:], op=ALU.mult),
                             r=pk(bU) + ["gsb"], w=[("tt", f)])
                        P.op("dve", lambda e, f=f, i=i: e.tensor_tensor(out=uu[i % 2][f][:], in0=sg[f][:], in1=tt[f][:], op=ALU.mult),
                             r=[("sg", f), ("tt", f)], w=[("uu", i % 2, f)])
                    if i > 0:
                        down(i - 1)
                    if e_ == experts[0] and tg + 1 < 4:
                        PRO(tg + 1)
                    if tg == 0 and stage != "norouted" and e_ + 2 < NEXP:
                        load_expert(e_ + 2)
                down(len(steps) - 1)

                for tg in range(4):
                    ts_ = slice(tg * 512, (tg + 1) * 512)
                    sq, rs, hn = wk["sq"], wk["rs"], wk["hn"]
                    P.op("act", lambda e, ts_=ts_: e.activation(out=sq[:], in_=R[:, :, ts_], func=AF.Square),
                         r=[("R", c, tg) for c in range(8)], w=["sq"])
                    for c in range(8):
                        P.op("pe", lambda e, c=c: e.matmul(bank(0), onb[:], sq[:, c, :], start=(c == 0), stop=(c == 7)),
                             r=["sq", "onb"], w=pk(0))
                    P.op("act", lambda e: e.activation(out=rs[:], in_=bank(0), func=AF.Ln, bias=wk["eps"][:, 0:1], scale=1.0 / 1024),
                         r=pk(0) + ["eps"], w=["rs"])
                    P.op("act", lambda e: e.activation(out=rs[:], in_=rs[:], func=AF.Exp, scale=-0.5), r=["rs"], w=["rs"])
                    P.op("dve", lambda e, ts_=ts_: e.tensor_tensor(out=hn[:], in0=R[:, :, ts_],
                                                                   in1=rs[:].unsqueeze(1).broadcast_to([128, 8, 512]), op=ALU.mult),
                         r=[("R", c, tg) for c in range(8)] + ["rs"], w=["hn"])
                    P.op("pool", lambda e: e.tensor_tensor(out=hn[:], in0=hn[:], in1=vs("fg").unsqueeze(2).broadcast_to([128, 8, 512]),
                                                           op=ALU.mult), r=["hn", "V"], w=["hn"])
                    P.dma_group("sp", [(out3[:, c, ts_], hn[:, c, :]) for c in range(8)], r=["hn"], sem="out")
                P.final_wait("sp", "out")
                P.emit()
    return nc


def out_proj(P, bank, wo, wokey, yT, ykey, R, t, banks):
    for half in range(2):
        b = banks[half]
        for di in range(4):
            dtl = half * 4 + di
            for fc in range(8):
                P.op("pe", lambda e, b=b, di=di, dtl=dtl, fc=fc: e.matmul(
                    bank(b, 128, di * 128), wo[:, fc, dtl * 128:(dtl + 1) * 128], yT[:, fc, :],
                    start=(fc == 0), stop=(fc == 7)), r=[wokey, ykey], w=[("ps", b)])
        P.op("dve", lambda e, b=b, half=half: e.tensor_tensor(
            out=R[:, half * 4:(half + 1) * 4, t * 128:(t + 1) * 128], in0=bank(b).rearrange("p (a n) -> p a n", a=4),
            in1=R[:, half * 4:(half + 1) * 4, t * 128:(t + 1) * 128], op=ALU.add),
            r=[("ps", b)] + [("R", half * 4 + i) for i in range(4)], w=[("R", half * 4 + i) for i in range(4)])


def finish(nc, P, R, out3, es):
    for c in range(8):
        P.dma("sp", out3[:, c, :], R[:, c, :], r=[("R", c)], sem="out")
    P.final_wait("sp", "out")
    P.emit()
    return nc


def _fm(v, n):
    return np.ascontiguousarray(np.asarray(v, np.float32).reshape(n, 128).T)


def _wl(w):
    K, N = w.shape
    return np.ascontiguousarray(np.asarray(w, np.float32).reshape(K // 128, 128, N).transpose(1, 0, 2))


def _bucket_table():
    import math
    q = np.arange(128)[None, :]
    j = np.arange(128)[:, None]
    tabs, valid = [], []
    for blk in range(2):
        dist = q + 128 - (blk * 128 + j)
        ok = (dist >= 0) & (dist < 128)
        d = np.maximum(dist, 0)
        dd = np.maximum(d, 1).astype(np.float32)
        large = 16 + (np.log(dd / np.float32(16)) / np.float32(math.log(128 / 16)) * np.float32(16)).astype(np.int32)
        large = np.minimum(large, 31)
        tabs.append(np.where(d < 16, d, large))
        valid.append(ok)
    return np.stack(tabs, 1), np.stack(valid, 1)


def make_in_maps(inp):
    f = lambda k: np.asarray(inp[k], np.float32)
    x, c = f("x"), f("c")
    W = f("w_in")[0]
    z_, xbc_, dt_, q_, k_, v_ = (W[:, 0:1024], W[:, 1024:3072], W[:, 3072:3088], W[:, 3088:4112], W[:, 4112:4368],
                                 W[:, 4368:4624])
    w1 = _wl(np.concatenate([xbc_, z_, dt_], 1))
    kd = np.concatenate([np.concatenate([k_[:, h * 64:(h + 1) * 64]] * 2, 1) for h in range(4)], 1)
    w2 = _wl(np.concatenate([q_, kd, v_], 1))
    wo = _wl(f("w_out")[0])
    wr = _wl(f("router_w")[0])
    modw = _wl(f("mod_w")[0])
    tab, ok = _bucket_table()
    rb = f("rel_bias")
    bias = rb[tab]
    bias = np.where(ok[..., None], bias, np.float32(-30000.0)).transpose(0, 3, 1, 2)
    biasT = np.ascontiguousarray(bias.reshape(128, 16 * 256), np.float32)
    eg = np.concatenate([f("exp_w_gate")[0], f("sh_w_gate")], 0)
    eu = np.concatenate([f("exp_w_up")[0], f("sh_w_up")], 0)
    ed = np.concatenate([f("exp_w_down")[0], f("sh_w_down")], 0)
    eg = np.ascontiguousarray(eg.reshape(NEXP, 8, 128, 256).transpose(0, 2, 1, 3).reshape(NEXP, 128, 2048))
    eu = np.ascontiguousarray(eu.reshape(NEXP, 8, 128, 256).transpose(0, 2, 1, 3).reshape(NEXP, 128, 2048))
    ed = np.ascontiguousarray(ed.reshape(NEXP, 2, 128, 1024).transpose(0, 2, 1, 3).reshape(NEXP, 128, 2048))
    bc = lambda v: np.broadcast_to(np.asarray(v, np.float32).reshape(1, -1), (128, np.asarray(v).size))
    cbT = np.ascontiguousarray(f("conv_b")[0].reshape(16, 128))
    maps = []
    for core in range(8):
        b, hf = core // 2, core % 2
        vec = np.zeros((128, NV), np.float32)

        def put(n, a):
            o, w = _VOFF[n]
            vec[:, o:o + w] = a
        put("n1g", _fm(f("norm1_g")[0], 8))
        put("n2g", _fm(f("norm2_g")[0], 8))
        put("fg", _fm(f("final_g"), 8))
        put("modb", _fm(f("mod_b")[0], 48))
        put("convw", f("conv_w")[0].T.reshape(16, 128, 4).transpose(1, 0, 2).reshape(128, 64))
        put("convb", _fm(f("conv_b")[0], 16))
        put("dtb", bc(f("dt_bias")[0]))
        put("alog", bc(f("a_log")[0]))
        put("dskip", bc(f("d_skip")[0]))
        put("sinks", bc(f("sinks")[0]))
        put("rbias", bc(f("router_bias")[0]))
        put("wog", _fm(np.concatenate([f("ssm_norm_g")[0], f("att_norm_g")[0]]), 16))
        put("flag", np.full((128, 1), float(hf), np.float32))
        put("cT", _fm(c[b], 8))
        xo = np.ascontiguousarray(x[b, hf * NT:(hf + 1) * NT, :].T)
        xp = np.ascontiguousarray(x[b, 0:NT, :].T) if hf == 1 else np.zeros((1024, NT), np.float32)
        maps.append(dict(xoT=xo, xpT=xp, vec=vec, modw=modw, cbT=cbT, w1=w1, w2=w2, wo=wo, wr=wr, biasT=biasT,
                         eg=eg, eu=eu, ed=ed))
    return maps


_NC_CACHE = {}


def run(inp, stage="full"):
    if stage not in _NC_CACHE:
        _NC_CACHE[stage] = build_nc(stage)
    nc = _NC_CACHE[stage]
    maps = make_in_maps(inp)
    res = run_bass_kernel_spmd(nc, maps, core_ids=list(range(8)))
    out = np.empty((4, 4096, 1024), np.float32)
    for core in range(8):
        b, hf = core // 2, core % 2
        out[b, hf * NT:(hf + 1) * NT, :] = res.results[core]["outT"].T
    return out


def kernel(**inputs):
    return run(inputs, "full")
```
